# Optimizing a Trainium2 kernel written in Bass

```python
import math
import jax, jax.numpy as jnp
from jax import lax
import numpy as np

D_MODEL = 2048
BATCH = 2
SEQ = 4096
DEPTH = 2

N_MIXERS = 2
HEAD_DIM = 64
ROPE_DIM = HEAD_DIM // 4
ROPE_THETA = 500000.0
ATTN_BLOCK = 128
SWA_Q_HEADS = 32
SWA_KV_HEADS = 4
SWA_WINDOW = 128
NSA_Q_HEADS = 32
NSA_KV_HEADS = 4
CMP_BLOCK = 32
CMP_STRIDE = 16
SEL_BLOCK = 64
SEL_TOPK = 16
NSA_WINDOW = 512
CMP_HIDDEN = 256
NSA_QCHUNK = 64
FORCE_BONUS = 1e4
N_GROUPS = 4
EXPERTS_PER_GROUP = 4
N_EXPERTS = N_GROUPS * EXPERTS_PER_GROUP
TOPK_IN_GROUP = 2
D_EXPERT = D_MODEL // 4
ALPHA = (2 * DEPTH) ** 0.25
BETA = (8 * DEPTH) ** -0.25
LN_EPS = 1e-5
NEG_INF = -1e30
N_SWA_LAYERS = (DEPTH + 1) // 2
N_NSA_LAYERS = DEPTH // 2
SWA_QKV_COLS = (SWA_Q_HEADS + 2 * SWA_KV_HEADS) * HEAD_DIM
NSA_IN_COLS = NSA_Q_HEADS * HEAD_DIM + 6 * NSA_KV_HEADS * HEAD_DIM + 3 * NSA_Q_HEADS

kernel_name = "hybrid_swa_nsa_hmoe_deepnorm_adaln"


def layer_norm(x, g, b):
    xf = x.astype(jnp.float32)
    mu = jnp.mean(xf, -1, keepdims=True)
    var = jnp.mean(jnp.square(xf - mu), -1, keepdims=True)
    y = (xf - mu) * lax.rsqrt(var + LN_EPS)
    return (y * g.astype(jnp.float32) + b.astype(jnp.float32)).astype(x.dtype)


def rope_tables(positions):
    inv_freq = ROPE_THETA ** (-jnp.arange(0, ROPE_DIM, 2, dtype=jnp.float32) / ROPE_DIM)
    ang = positions.astype(jnp.float32)[..., None] * inv_freq
    return jnp.cos(ang)[:, :, None, :], jnp.sin(ang)[:, :, None, :]


def apply_partial_rope(x, cos, sin):
    half = ROPE_DIM // 2
    x1 = x[..., :half].astype(jnp.float32)
    x2 = x[..., half:ROPE_DIM].astype(jnp.float32)
    rot = jnp.concatenate([x1 * cos - x2 * sin, x2 * cos + x1 * sin], -1).astype(x.dtype)
    return jnp.concatenate([rot, x[..., ROPE_DIM:]], -1)


def banded_attention(q, k, v, window, sinks=None):
    B, S, Hkv, G, D = q.shape
    n_blocks = S // ATTN_BLOCK
    halo = -(-window // ATTN_BLOCK) * ATTN_BLOCK
    span = ATTN_BLOCK + halo
    pad = ((0, 0), (halo, 0), (0, 0), (0, 0))
    kp = jnp.pad(k, pad)
    vp = jnp.pad(v, pad)
    scale = D ** -0.5

    def one_block(n):
        start = n * ATTN_BLOCK
        qb = lax.dynamic_slice_in_dim(q, start, ATTN_BLOCK, axis=1)
        kb = lax.dynamic_slice_in_dim(kp, start, span, axis=1)
        vb = lax.dynamic_slice_in_dim(vp, start, span, axis=1)
        s = jnp.einsum('bqhgd,bkhd->bhgqk', qb, kb).astype(jnp.float32) * scale
        qpos = start + jnp.arange(ATTN_BLOCK)
        kpos = start - halo + jnp.arange(span)
        rel = qpos[:, None] - kpos[None, :]
        mask = (rel >= 0) & (rel < window) & (kpos[None, :] >= 0)
        s = jnp.where(mask, s, NEG_INF)
        if sinks is not None:
            sink = jnp.broadcast_to(sinks.astype(jnp.float32)[None, :, :, None, None], s.shape[:-1] + (1,))
            p = jax.nn.softmax(jnp.concatenate([s, sink], -1), -1)[..., :-1]
        else:
            p = jax.nn.softmax(s, -1)
        return jnp.einsum('bhgqk,bkhd->bqhgd', p.astype(vb.dtype), vb)

    out = lax.map(one_block, jnp.arange(n_blocks))
    return jnp.moveaxis(out, 0, 1).reshape(B, S, Hkv, G, D)


def swa_mixer(h, cos, sin, w_qkv, b_qkv, sinks, w_o):
    B, S, _ = h.shape
    G = SWA_Q_HEADS // SWA_KV_HEADS
    qkv = h @ w_qkv + b_qkv
    q, k, v = jnp.split(qkv, [SWA_Q_HEADS * HEAD_DIM, (SWA_Q_HEADS + SWA_KV_HEADS) * HEAD_DIM], -1)
    q = apply_partial_rope(q.reshape(B, S, SWA_Q_HEADS, HEAD_DIM), cos, sin)
    k = apply_partial_rope(k.reshape(B, S, SWA_KV_HEADS, HEAD_DIM), cos, sin)
    v = v.reshape(B, S, SWA_KV_HEADS, HEAD_DIM)
    o = banded_attention(q.reshape(B, S, SWA_KV_HEADS, G, HEAD_DIM), k, v, SWA_WINDOW,
                         sinks.reshape(SWA_KV_HEADS, G))
    return o.reshape(B, S, SWA_Q_HEADS * HEAD_DIM) @ w_o


def compress_blocks(x, pe, w1, w2):
    B, S, H, D = x.shape
    chunks = x.reshape(B, S // CMP_STRIDE, CMP_STRIDE, H, D)
    blocks = jnp.concatenate([chunks[:, :-1], chunks[:, 1:]], axis=2)
    blocks = blocks + pe[None, None, :, None, :]
    n_cmp = blocks.shape[1]
    flat = jnp.moveaxis(blocks, 3, 2).reshape(B, n_cmp, H, CMP_BLOCK * D)
    return jax.nn.silu(flat @ w1) @ w2


def cmp_sel_overlap(n_cmp, n_sel):
    c0 = jnp.arange(n_cmp)[:, None] * CMP_STRIDE
    s0 = jnp.arange(n_sel)[None, :] * SEL_BLOCK
    ov = jnp.clip(jnp.minimum(c0 + CMP_BLOCK, s0 + SEL_BLOCK) - jnp.maximum(c0, s0), 0)
    return ov.astype(jnp.float32) / CMP_BLOCK


def nsa_mixer(h, cos, sin, w_in, pe_k, pe_v, phi_k1, phi_k2, phi_v1, phi_v2, w_o):
    B, S, _ = h.shape
    HQ, HKV, D = NSA_Q_HEADS, NSA_KV_HEADS, HEAD_DIM
    G = HQ // HKV
    kvw = HKV * D
    proj = h @ w_in
    q, k_c, v_c, k_s, v_s, k_w, v_w, gates = jnp.split(proj, [HQ * D + i * kvw for i in range(7)], -1)
    q = q.reshape(B, S, HQ, D)
    q_raw = q.reshape(B, S, HKV, G, D)
    q_rot = apply_partial_rope(q, cos, sin).reshape(B, S, HKV, G, D)
    shp = (B, S, HKV, D)
    k_s = apply_partial_rope(k_s.reshape(shp), cos, sin)
    k_w = apply_partial_rope(k_w.reshape(shp), cos, sin)
    v_s = v_s.reshape(shp)
    v_w = v_w.reshape(shp)
    kc = compress_blocks(k_c.reshape(shp), pe_k, phi_k1, phi_k2)
    vc = compress_blocks(v_c.reshape(shp), pe_v, phi_v1, phi_v2)
    n_cmp = kc.shape[1]
    n_sel = S // SEL_BLOCK
    top_k = min(SEL_TOPK, n_sel)
    ks_blocks = k_s.reshape(B, n_sel, SEL_BLOCK, HKV, D).transpose(0, 3, 1, 2, 4)
    vs_blocks = v_s.reshape(B, n_sel, SEL_BLOCK, HKV, D).transpose(0, 3, 1, 2, 4)
    overlap = cmp_sel_overlap(n_cmp, n_sel)
    cmp_end = jnp.arange(n_cmp) * CMP_STRIDE + CMP_BLOCK - 1
    sel_idx = jnp.arange(n_sel)
    scale = D ** -0.5
    bi = jnp.arange(B)[:, None, None, None]
    hi = jnp.arange(HKV)[None, :, None, None]

    def one_chunk(n):
        start = n * NSA_QCHUNK
        t = start + jnp.arange(NSA_QCHUNK)
        qc = lax.dynamic_slice_in_dim(q_raw, start, NSA_QCHUNK, axis=1)
        qr = lax.dynamic_slice_in_dim(q_rot, start, NSA_QCHUNK, axis=1)
        s = jnp.einsum('bthgd,bnhd->bhgtn', qc, kc).astype(jnp.float32) * scale
        valid = cmp_end[None, :] <= t[:, None]
        p = jax.nn.softmax(jnp.where(valid, s, NEG_INF), -1) * valid
        o_cmp = jnp.einsum('bhgtn,bnhd->bthgd', p.astype(vc.dtype), vc)
        imp = jnp.einsum('bhgtn,ns->bhts', p, overlap)
        cur = t[:, None] // SEL_BLOCK
        causal = sel_idx[None, :] * SEL_BLOCK <= t[:, None]
        forced = (sel_idx[None, :] == 0) | (sel_idx[None, :] == cur) | (sel_idx[None, :] == cur - 1)
        score = jnp.where(causal, imp + jnp.where(forced, FORCE_BONUS, 0.0), NEG_INF)
        _, idx = lax.top_k(score, top_k)
        ksel = ks_blocks[bi, hi, idx].reshape(B, HKV, NSA_QCHUNK, top_k * SEL_BLOCK, D)
        vsel = vs_blocks[bi, hi, idx].reshape(B, HKV, NSA_QCHUNK, top_k * SEL_BLOCK, D)
        kpos = (idx[..., None] * SEL_BLOCK + jnp.arange(SEL_BLOCK)).reshape(B, HKV, NSA_QCHUNK, top_k * SEL_BLOCK)
        m2 = (kpos <= t[:, None])[:, :, None]
        s2 = jnp.einsum('bthgd,bhtkd->bhgtk', qr, ksel).astype(jnp.float32) * scale
        p2 = jax.nn.softmax(jnp.where(m2, s2, NEG_INF), -1)
        o_sel = jnp.einsum('bhgtk,bhtkd->bthgd', p2.astype(vsel.dtype), vsel)
        return o_cmp, o_sel

    o_cmp, o_sel = lax.map(one_chunk, jnp.arange(S // NSA_QCHUNK))
    o_cmp = jnp.moveaxis(o_cmp, 0, 1).reshape(B, S, HKV, G, D)
    o_sel = jnp.moveaxis(o_sel, 0, 1).reshape(B, S, HKV, G, D)
    o_win = banded_attention(q_rot, k_w, v_w, NSA_WINDOW)
    g = jax.nn.sigmoid(gates.astype(jnp.float32)).reshape(B, S, HKV, G, 3).astype(h.dtype)
    o = g[..., 0:1] * o_cmp + g[..., 1:2] * o_sel + g[..., 2:3] * o_win
    return o.reshape(B, S, HQ * D) @ w_o


def hier_moe(h, w_group, b_group, w_router, b_router, w1, w3, w2):
    B, S, Dm = h.shape
    T = B * S
    t = h.reshape(T, Dm)
    pg = jax.nn.softmax((t @ w_group + b_group).astype(jnp.float32), -1)
    g_prob, g_idx = lax.top_k(pg, 1)
    le = (t @ w_router + b_router).astype(jnp.float32).reshape(T, N_GROUPS, EXPERTS_PER_GROUP)
    g_onehot = jax.nn.one_hot(g_idx[:, 0], N_GROUPS, dtype=jnp.float32)
    le = jnp.sum(g_onehot[:, :, None] * le, axis=1)
    pe = jax.nn.softmax(le, -1)
    e_prob, e_idx = lax.top_k(pe, TOPK_IN_GROUP)
    e_prob = e_prob / jnp.sum(e_prob, -1, keepdims=True)
    expert_id = g_idx * EXPERTS_PER_GROUP + e_idx
    weight = g_prob * e_prob
    combine = jnp.sum(jax.nn.one_hot(expert_id, N_EXPERTS, dtype=jnp.float32) * weight[..., None], axis=1)
    combine = combine.astype(t.dtype)
    y = jnp.zeros_like(t)
    for e in range(N_EXPERTS):
        he = jax.nn.silu(t @ w1[e]) * (t @ w3[e])
        y = y + combine[:, e:e + 1] * (he @ w2[e])
    return y.reshape(B, S, Dm)


def setup_inputs(seed: int = 0) -> dict:
    key = jax.random.key(seed)
    ks = jax.random.split(key, 32)
    f32 = jnp.float32
    D = D_MODEL
    nrm = lambda k, shape, s: jax.random.normal(k, shape, f32) * s
    x = nrm(ks[0], (BATCH, SEQ, D), 1.0)
    c = nrm(ks[1], (BATCH, D), 1.0)
    positions = jnp.broadcast_to(jnp.arange(SEQ, dtype=jnp.int32)[None, :], (BATCH, SEQ))
    w_ada = nrm(ks[2], (DEPTH, D, 6 * D), D ** -0.5)
    b_ada = nrm(ks[3], (DEPTH, 6 * D), 0.01)
    swa_scale = jnp.concatenate([jnp.ones(((SWA_Q_HEADS + SWA_KV_HEADS) * HEAD_DIM,), f32),
                                 jnp.full((SWA_KV_HEADS * HEAD_DIM,), BETA, f32)])
    swa_w_qkv = nrm(ks[4], (N_SWA_LAYERS, D, SWA_QKV_COLS), D ** -0.5) * swa_scale
    swa_b_qkv = nrm(ks[5], (N_SWA_LAYERS, SWA_QKV_COLS), 0.01)
    swa_sinks = nrm(ks[6], (N_SWA_LAYERS, SWA_Q_HEADS), 1.0)
    swa_w_o = nrm(ks[7], (N_SWA_LAYERS, SWA_Q_HEADS * HEAD_DIM, D), (SWA_Q_HEADS * HEAD_DIM) ** -0.5 * BETA)
    kvw = NSA_KV_HEADS * HEAD_DIM
    ones_kv = jnp.ones((kvw,), f32)
    beta_kv = jnp.full((kvw,), BETA, f32)
    nsa_scale = jnp.concatenate([jnp.ones((NSA_Q_HEADS * HEAD_DIM,), f32),
                                 ones_kv, beta_kv, ones_kv, beta_kv, ones_kv, beta_kv,
                                 jnp.ones((3 * NSA_Q_HEADS,), f32)])
    nsa_w_in = nrm(ks[8], (N_NSA_LAYERS, D, NSA_IN_COLS), D ** -0.5) * nsa_scale
    nsa_pe_k = nrm(ks[9], (N_NSA_LAYERS, CMP_BLOCK, HEAD_DIM), 0.1)
    nsa_pe_v = nrm(ks[10], (N_NSA_LAYERS, CMP_BLOCK, HEAD_DIM), 0.1)
    fan = CMP_BLOCK * HEAD_DIM
    nsa_phi_k1 = nrm(ks[11], (N_NSA_LAYERS, fan, CMP_HIDDEN), fan ** -0.5)
    nsa_phi_k2 = nrm(ks[12], (N_NSA_LAYERS, CMP_HIDDEN, HEAD_DIM), CMP_HIDDEN ** -0.5)
    nsa_phi_v1 = nrm(ks[13], (N_NSA_LAYERS, fan, CMP_HIDDEN), fan ** -0.5)
    nsa_phi_v2 = nrm(ks[14], (N_NSA_LAYERS, CMP_HIDDEN, HEAD_DIM), CMP_HIDDEN ** -0.5)
    nsa_w_o = nrm(ks[15], (N_NSA_LAYERS, NSA_Q_HEADS * HEAD_DIM, D), (NSA_Q_HEADS * HEAD_DIM) ** -0.5 * BETA)
    moe_w_group = nrm(ks[16], (DEPTH, D, N_GROUPS), D ** -0.5)
    moe_b_group = nrm(ks[17], (DEPTH, N_GROUPS), 0.01)
    moe_w_router = nrm(ks[18], (DEPTH, D, N_EXPERTS), D ** -0.5)
    moe_b_router = nrm(ks[19], (DEPTH, N_EXPERTS), 0.01)
    moe_w1 = nrm(ks[20], (DEPTH, N_EXPERTS, D, D_EXPERT), D ** -0.5)
    moe_w3 = nrm(ks[21], (DEPTH, N_EXPERTS, D, D_EXPERT), D ** -0.5)
    moe_w2 = nrm(ks[22], (DEPTH, N_EXPERTS, D_EXPERT, D), D_EXPERT ** -0.5 * BETA)
    ln_t_g = 1.0 + nrm(ks[23], (DEPTH, D), 0.01)
    ln_t_b = nrm(ks[24], (DEPTH, D), 0.01)
    ln_c_g = 1.0 + nrm(ks[25], (DEPTH, D), 0.01)
    ln_c_b = nrm(ks[26], (DEPTH, D), 0.01)
    return {"x": x, "c": c, "positions": positions, "w_ada": w_ada, "b_ada": b_ada,
            "swa_w_qkv": swa_w_qkv, "swa_b_qkv": swa_b_qkv, "swa_sinks": swa_sinks, "swa_w_o": swa_w_o,
            "nsa_w_in": nsa_w_in, "nsa_pe_k": nsa_pe_k, "nsa_pe_v": nsa_pe_v,
            "nsa_phi_k1": nsa_phi_k1, "nsa_phi_k2": nsa_phi_k2, "nsa_phi_v1": nsa_phi_v1, "nsa_phi_v2": nsa_phi_v2,
            "nsa_w_o": nsa_w_o,
            "moe_w_group": moe_w_group, "moe_b_group": moe_b_group,
            "moe_w_router": moe_w_router, "moe_b_router": moe_b_router,
            "moe_w1": moe_w1, "moe_w3": moe_w3, "moe_w2": moe_w2,
            "ln_t_g": ln_t_g, "ln_t_b": ln_t_b, "ln_c_g": ln_c_g, "ln_c_b": ln_c_b}


def reference(x, c, positions, w_ada, b_ada, swa_w_qkv, swa_b_qkv, swa_sinks, swa_w_o,
              nsa_w_in, nsa_pe_k, nsa_pe_v, nsa_phi_k1, nsa_phi_k2, nsa_phi_v1, nsa_phi_v2, nsa_w_o,
              moe_w_group, moe_b_group, moe_w_router, moe_b_router, moe_w1, moe_w3, moe_w2,
              ln_t_g, ln_t_b, ln_c_g, ln_c_b):
    cos, sin = rope_tables(positions)
    c_act = jax.nn.silu(c)
    for i in range(DEPTH):
        mod = (c_act @ w_ada[i] + b_ada[i])[:, None, :]
        sh_t, sc_t, g_t, sh_c, sc_c, g_c = jnp.split(mod, 6, -1)
        h = x * (1.0 + sc_t) + sh_t
        j = i // N_MIXERS
        if i % N_MIXERS == 0:
            out = swa_mixer(h, cos, sin, swa_w_qkv[j], swa_b_qkv[j], swa_sinks[j], swa_w_o[j])
        else:
            out = nsa_mixer(h, cos, sin, nsa_w_in[j], nsa_pe_k[j], nsa_pe_v[j], nsa_phi_k1[j], nsa_phi_k2[j],
                            nsa_phi_v1[j], nsa_phi_v2[j], nsa_w_o[j])
        x = layer_norm(ALPHA * x + g_t * out, ln_t_g[i], ln_t_b[i])
        h = x * (1.0 + sc_c) + sh_c
        out = hier_moe(h, moe_w_group[i], moe_b_group[i], moe_w_router[i], moe_b_router[i],
                       moe_w1[i], moe_w3[i], moe_w2[i])
        x = layer_norm(ALPHA * x + g_c * out, ln_c_g[i], ln_c_b[i])
    return x
```

```python
import math
import numpy as np
import ml_dtypes
from contextlib import ExitStack
import concourse.bass as bass
import concourse.mybir as mybir
from concourse.bass_utils import run_bass_kernel_spmd

F32 = mybir.dt.float32
BF16 = mybir.dt.bfloat16
I32 = mybir.dt.int32
AF = mybir.ActivationFunctionType
ALU = mybir.AluOpType
AX = mybir.AxisListType

D = 2048
NCH = 16
NT = 1024
HALO0 = 128
ALPHA = 4 ** 0.25
LN_EPS = 1e-5
EPS_EFF = LN_EPS / (ALPHA * ALPHA)
ROPE_THETA = 500000.0
SCALE = 64 ** -0.5
WB = 256


class Tok:
    __slots__ = ("name", "w", "r")

    def __init__(self, name=""):
        self.name = name
        self.w = None
        self.r = {}


class K:
    ND = 6

    def __init__(self, nc, stack):
        self.nc = nc
        self.stack = stack
        self.eng = {"pe": nc.tensor, "act": nc.scalar, "dve": nc.vector, "pool": nc.gpsimd, "sp": nc.sync}
        self.sem = {}
        self.cnt = {}
        self.pend = {}
        for e in ("pe", "act", "dve", "pool"):
            self.sem[e] = stack.enter_context(nc.semaphore("s_" + e))
            self.cnt[e] = 0
            self.pend[e] = False
        self.dsem = {}
        self.dcnt = {}
        self.drr = {}
        self.dwaited = {}
        for q in ("sp", "act", "pool"):
            self.dsem[q] = [stack.enter_context(nc.semaphore("d_%s%d" % (q, i))) for i in range(self.ND)]
            self.dcnt[q] = [0] * self.ND
            self.dwaited[q] = [0] * self.ND
            self.drr[q] = 0
        self.waited = {e: {} for e in self.eng}
        self.ninst = 0
        self._n = 0

    def sb(self, name, shape, dt):
        return self.stack.enter_context(self.nc.sbuf_tensor("sb_" + name, list(shape), dt))

    def ps(self, name, shape, dt):
        return self.stack.enter_context(self.nc.psum_tensor("ps_" + name, list(shape), dt))

    def _wait(self, e, sem, val):
        w = self.waited[e]
        kk = id(sem)
        if w.get(kk, 0) >= val:
            return
        self.eng[e].wait_ge(sem, val)
        w[kk] = val

    def _deps(self, e, reads, writes):
        own = self.sem.get(e)
        pe = (e == "pe")
        for t in reads:
            if t.w is not None and not (pe and t.w[0] is own):
                self._wait(e, *t.w)
        for t in writes:
            if t.w is not None and not (pe and t.w[0] is own):
                self._wait(e, *t.w)
            for (s, v) in t.r.values():
                if pe and s is own:
                    continue
                self._wait(e, s, v)

    def _mark(self, sem, val, reads, writes):
        for t in writes:
            t.w = (sem, val)
            t.r = {}
        for t in reads:
            t.r[id(sem)] = (sem, val)

    def op(self, e, ins_fn, reads=(), writes=(), inc=True):
        self._deps(e, reads, writes)
        ins = ins_fn(self.eng[e])
        if inc:
            self.cnt[e] += 1
            ins.then_inc(self.sem[e], 1)
            self.pend[e] = False
            self._mark(self.sem[e], self.cnt[e], reads, writes)
        else:
            self.pend[e] = True
            self._mark(self.sem[e], self.cnt[e] + 1, reads, writes)
        self.ninst += 1
        return ins

    def dma(self, q, out, in_, reads=(), writes=(), **kw):
        i = self.drr[q]
        self.drr[q] = (i + 1) % self.ND
        sem = self.dsem[q][i]
        if self.dwaited[q][i] < self.dcnt[q][i]:
            self._wait(q, sem, self.dcnt[q][i])
            self.dwaited[q][i] = self.dcnt[q][i]
        self._deps(q, reads, writes)
        ins = self.eng[q].dma_start(out=out, in_=in_, **kw)
        self.dcnt[q][i] += 16
        ins.then_inc(sem, 16)
        self._mark(sem, self.dcnt[q][i], reads, writes)
        self.ninst += 1
        return ins

    def finish(self, toks, e="sp"):
        for t in toks:
            if t.w is not None:
                self._wait(e, *t.w)


class Ctx:
    pass


def setup_ctx(k, consts_ap, hoff=HALO0):
    C = Ctx()
    C.k = k
    C.pb = [k.ps("pb%d" % i, [128, 512], F32) for i in range(8)]
    C.t_pb = [Tok("pb%d" % i) for i in range(8)]
    C.pbi = 0
    C.xT = k.sb("xT", [128, NCH, NT], F32)
    C.t_x = [[Tok() for _ in range(2)] for _ in range(NCH)]
    C.HOFF = hoff
    C.hT = k.sb("hT", [128, NCH, NT + hoff], BF16)
    C.t_h = [[Tok() for _ in range(3)] for _ in range(NCH)]
    C.big = k.sb("big", [128, NCH, NT], BF16)
    C.t_big = [[Tok() for _ in range(2)] for _ in range(NCH)]
    C.NWB = 3
    C.wb = [k.sb("wb%d" % i, [128, NCH, WB], BF16) for i in range(C.NWB)]
    C.t_wb = [Tok() for _ in range(C.NWB)]
    C.wbi = 0
    C.ident_bf = k.sb("ident_bf", [128, 128], BF16)
    C.ident_f = k.sb("ident_f", [128, 128], F32)
    C.ones_f = k.sb("ones_f", [128, 128], F32)
    C.ones_bf = k.sb("ones_bf", [128, 128], BF16)
    C.t_const = Tok("const")
    k.dma("sp", C.ident_f[:], consts_ap["ident_f"], writes=[C.t_const])
    k.dma("pool", C.ident_bf[:], consts_ap["ident_f"], writes=[C.t_const])
    k.op("dve", lambda e: e.memset(C.ones_f[:], 1.0), writes=[C.t_const])
    k.op("dve", lambda e: e.memset(C.ones_bf[:], 1.0), writes=[C.t_const])
    return C


def next_pb(C):
    i = C.pbi
    C.pbi = (i + 1) % 8
    return C.pb[i], C.t_pb[i]


def next_wb(C):
    wl = getattr(C, 'wb_cur', None) or C.wb
    tl = getattr(C, 't_wb_cur', None) or C.t_wb
    i = C.wbi % len(wl)
    C.wbi = (i + 1) % len(wl)
    return wl[i], tl[i]


def tsl(th):
    return slice(th * 512, (th + 1) * 512)


def load_wblock(C, srcs, q="pool"):
    k = C.k
    buf, tok = next_wb(C)
    for (ap, off) in srcs:
        w = ap.shape[1]
        k.dma(q, buf[:, :, off:off + w], ap.rearrange("(kc p) n -> p kc n", p=128), writes=[tok])
    return buf, tok


def mm_group(C, out_ap, t_out, pairs, reads, first_in_bank=True):
    k = C.k
    n = len(pairs)
    for i, (l, r) in enumerate(pairs):
        k.op("pe", lambda e: e.matmul(out_ap, lhsT=l, rhs=r, start=(i == 0 and first_in_bank), stop=(i == n - 1),
                                      skip_group_check=True),
             reads=reads, writes=[t_out], inc=(i == n - 1))


def load_x_transposed(C, x_dram, n_tiles, dst_fn, stage, t_stage):
    k = C.k
    for t in range(n_tiles):
        s = t % 2
        k.dma("sp", stage[s][:], x_dram[t * 128:(t + 1) * 128, :], writes=[t_stage[s]])
        for c0 in range(0, NCH, 4):
            pb, tp = next_pb(C)
            for j in range(4):
                c = c0 + j
                k.op("pe", lambda e: e.transpose(pb[:, j * 128:(j + 1) * 128], stage[s][:, c * 128:(c + 1) * 128], C.ident_f[:]),
                     reads=[t_stage[s], C.t_const], writes=[tp], inc=(j == 3))
            dst_fn(t, c0, pb[:].rearrange("p (j n) -> p j n", j=4), tp)


def layer_norm_mod(C, vec, g_col, b_col, A_col, B_col, tmp, t_tmp, stat, t_stat, h_halo=False):
    k = C.k
    if hasattr(C, "t_wb4"):
        for e_ in ("act", "dve", "pool"):
            k._deps(e_, (), [C.t_wb4])
    for th in range(2):
        ts = tsl(th)
        pb_s, tp_s = next_pb(C)
        pb_q, tp_q = next_pb(C)
        for c in range(NCH):
            k.op("pe", lambda e: e.matmul(pb_s[:], lhsT=C.ones_f[:], rhs=C.xT[:, c, ts], start=(c == 0), stop=(c == NCH - 1)),
                 reads=[C.t_x[c][th], C.t_const], writes=[tp_s], inc=(c == NCH - 1))
        for c in range(NCH):
            s = c % 2
            k.op("act", lambda e: e.activation(out=tmp[s][:], in_=C.xT[:, c, ts], func=AF.Square),
                 reads=[C.t_x[c][th]], writes=[t_tmp[s]])
            k.op("pe", lambda e: e.matmul(pb_q[:], lhsT=C.ones_f[:], rhs=tmp[s][:], start=(c == 0), stop=(c == NCH - 1)),
                 reads=[t_tmp[s], C.t_const], writes=[tp_q])
        mean, rstd, msq = stat[0], stat[1], stat[2]
        k.op("act", lambda e: e.mul(out=mean[:], in_=pb_s[:], mul=1.0 / D), reads=[tp_s], writes=[t_stat[0]])
        k.op("dve", lambda e: e.tensor_tensor(out=msq[:], in0=mean[:], in1=mean[:], op=ALU.mult), reads=[t_stat[0]], writes=[t_stat[2]])
        k.op("dve", lambda e: e.scalar_tensor_tensor(out=msq[:], in0=pb_q[:], scalar=1.0 / D, in1=msq[:], op0=ALU.mult, op1=ALU.subtract),
             reads=[tp_q, t_stat[2]], writes=[t_stat[2]])
        k.op("dve", lambda e: e.tensor_scalar(out=msq[:], in0=msq[:], scalar1=EPS_EFF, scalar2=None, op0=ALU.add),
             reads=[t_stat[2]], writes=[t_stat[2]])
        k.op("act", lambda e: e.sqrt(out=msq[:], in_=msq[:]), reads=[t_stat[2]], writes=[t_stat[2]])
        k.op("dve", lambda e: e.reciprocal(out=rstd[:], in_=msq[:]), reads=[t_stat[2]], writes=[t_stat[1]])
        for c in range(NCH):
            s = c % 2
            k.op("dve", lambda e: e.tensor_tensor(out=tmp[s][:], in0=C.xT[:, c, ts], in1=mean[:], op=ALU.subtract),
                 reads=[C.t_x[c][th], t_stat[0]], writes=[t_tmp[s]])
            k.op("dve", lambda e: e.tensor_tensor(out=tmp[s][:], in0=tmp[s][:], in1=rstd[:], op=ALU.mult),
                 reads=[t_tmp[s], t_stat[1]], writes=[t_tmp[s]])
            k.op("act", lambda e: e.activation(out=C.xT[:, c, ts], in_=tmp[s][:], func=AF.Identity,
                                               scale=vec[:, g_col + c:g_col + c + 1], bias=vec[:, b_col + c:b_col + c + 1]),
                 reads=[t_tmp[s], C.t_vec], writes=[C.t_x[c][th]])
            if A_col is not None:
                k.op("pool", lambda e: e.tensor_scalar(out=C.hT[:, c, C.HOFF + th * 512:C.HOFF + (th + 1) * 512], in0=tmp[s][:],
                                                       scalar1=vec[:, A_col + c:A_col + c + 1], scalar2=vec[:, B_col + c:B_col + c + 1],
                                                       op0=ALU.mult, op1=ALU.add),
                     reads=[t_tmp[s], C.t_vec], writes=[C.t_h[c][1 + th]])


def linear_fm(C, w_blocks, rhs_fn, rhs_toks_fn, n_tok_pieces, evac_fn):
    k = C.k
    for bi, (srcs, nm) in enumerate(w_blocks):
        buf, tw = load_wblock(C, srcs)
        for m in range(nm):
            for pc in range(n_tok_pieces):
                pb, tp = next_pb(C)
                rts = rhs_toks_fn(pc)
                for kc in range(NCH):
                    r = rhs_fn(kc, pc)
                    k.op("pe", lambda e: e.matmul(pb[:, 0:r.shape[-1]] if len(r.shape) == 2 else pb[:], lhsT=buf[:, kc, m * 128:(m + 1) * 128], rhs=r,
                                                  start=(kc == 0), stop=(kc == NCH - 1)),
                         reads=[tw] + rts(kc), writes=[tp], inc=(kc == NCH - 1))
                evac_fn(bi, m, pc, pb, tp)


def moe_layer(C, L, vec, W):
    k = C.k
    if not hasattr(C, "t_wb4"):
        C.t_wb4 = Tok()
    wb4 = C.bfv(20480, 4096).rearrange("p (kc n) -> p kc n", kc=16)
    k._deps("pool", (), C.t_tmp + C.t_stat + [C.t_wb4])
    C.wb_cur = C.wb + [wb4]
    C.t_wb_cur = C.t_wb + [C.t_wb4]
    pbr, tpr = next_pb(C)
    for c in range(NCH):
        s = c % 2
        k.op("dve", lambda e: e.tensor_scalar(out=C.h32[s][:], in0=C.xT[:, c, :], scalar1=vec[:, W["sc1"] + c:W["sc1"] + c + 1],
                                              scalar2=vec[:, W["sh"] + c:W["sh"] + c + 1], op0=ALU.mult, op1=ALU.add),
             reads=[C.t_x[c][0], C.t_x[c][1], C.t_vec], writes=[C.t_h32[s]])
        for t in range(8):
            k.op("pe", lambda e: e.matmul(pbr[:, t * 20:(t + 1) * 20], lhsT=C.h32[s][:, t * 128:(t + 1) * 128], rhs=C.wr[:, c, :],
                                          start=(c == 0 and t == 0), stop=(c == NCH - 1), skip_group_check=True),
                 reads=[C.t_h32[s], C.t_wr], writes=[tpr], inc=(t == 7))
    lg = C.lg
    tl = C.t_lg
    k.op("dve", lambda e: e.tensor_tensor(out=lg[:], in0=pbr[:, 0:160].rearrange("p (t n) -> p t n", t=8),
                                          in1=C.brow[:].unsqueeze(1).to_broadcast([128, 8, 20]), op=ALU.add),
         reads=[tpr, C.t_wr], writes=[tl])
    R = C.rt
    tr = C.t_rt
    for t in range(8):
        gl = lg[:, t, 0:4]
        rl = lg[:, t, 4:20].rearrange("p (g j) -> p g j", g=4)
        k.op("dve", lambda e: e.tensor_reduce(out=R["gmax"][:], in_=gl, axis=AX.X, op=ALU.max), reads=[tl], writes=[tr])
        k.op("dve", lambda e: e.tensor_scalar(out=R["ngmax"][:], in0=R["gmax"][:], scalar1=-1.0, scalar2=None, op0=ALU.mult), reads=[tr], writes=[tr])
        k.op("act", lambda e: e.activation(out=R["ge"][:], in_=gl, func=AF.Exp, bias=R["ngmax"][:], scale=1.0, accum_out=R["gsum"][:]),
             reads=[tl, tr], writes=[tr])
        k.op("dve", lambda e: e.reciprocal(out=R["gprob"][:], in_=R["gsum"][:]), reads=[tr], writes=[tr])
        k.op("dve", lambda e: e.tensor_scalar(out=R["gw"][:], in0=gl, scalar1=R["gmax"][:], scalar2=R["gprob"][:], op0=ALU.is_equal, op1=ALU.mult),
             reads=[tl, tr], writes=[tr])
        k.op("dve", lambda e: e.tensor_reduce(out=R["m1"][:], in_=rl, axis=AX.X, op=ALU.max), reads=[tl], writes=[tr])
        m1b = R["m1"][:].unsqueeze(2).to_broadcast([128, 4, 4])
        k.op("dve", lambda e: e.tensor_tensor(out=R["eq1"][:], in0=rl, in1=m1b, op=ALU.is_equal), reads=[tl, tr], writes=[tr])
        k.op("dve", lambda e: e.scalar_tensor_tensor(out=R["rl2"][:], in0=R["eq1"][:], scalar=-1e30, in1=rl, op0=ALU.mult, op1=ALU.add),
             reads=[tl, tr], writes=[tr])
        k.op("dve", lambda e: e.tensor_reduce(out=R["m2"][:], in_=R["rl2"][:], axis=AX.X, op=ALU.max), reads=[tr], writes=[tr])
        m2b = R["m2"][:].unsqueeze(2).to_broadcast([128, 4, 4])
        k.op("dve", lambda e: e.tensor_tensor(out=R["top2"][:], in0=rl, in1=m2b, op=ALU.is_ge), reads=[tl, tr], writes=[tr])
        k.op("dve", lambda e: e.tensor_tensor(out=R["dd"][:], in0=rl, in1=m1b, op=ALU.subtract), reads=[tl, tr], writes=[tr])
        k.op("act", lambda e: e.activation(out=R["ee"][:], in_=R["dd"][:], func=AF.Exp), reads=[tr], writes=[tr])
        k.op("dve", lambda e: e.tensor_tensor(out=R["ee"][:], in0=R["ee"][:], in1=R["top2"][:], op=ALU.mult), reads=[tr], writes=[tr])
        k.op("dve", lambda e: e.tensor_reduce(out=R["den"][:], in_=R["ee"][:], axis=AX.X, op=ALU.add), reads=[tr], writes=[tr])
        k.op("dve", lambda e: e.reciprocal(out=R["den"][:], in_=R["den"][:]), reads=[tr], writes=[tr])
        k.op("dve", lambda e: e.tensor_tensor(out=R["den"][:], in0=R["den"][:], in1=R["gw"][:], op=ALU.mult), reads=[tr], writes=[tr])
        k.op("dve", lambda e: e.tensor_tensor(out=C.comb[:, t, :].rearrange("p (g j) -> p g j", g=4), in0=R["ee"][:],
                                              in1=R["den"][:].unsqueeze(2).to_broadcast([128, 4, 4]), op=ALU.mult),
             reads=[tr], writes=[C.t_comb])
    w1, w3, w2 = W["w1"], W["w3"], W["w2"]
    for gi in range(4):
        for j in range(4):
            ecol = gi * 4 + j
            for half in range(2):
                pb, tp = next_pb(C)
                for tt in range(4):
                    t = half * 4 + tt
                    s = (tt % 2)
                    k.op("dve", lambda e: e.tensor_scalar(out=C.diag[s][:], in0=C.ident_bf[:], scalar1=C.comb[:, t, ecol:ecol + 1], scalar2=None, op0=ALU.mult),
                         reads=[C.t_comb, C.t_const], writes=[C.t_diag[s]])
                    k.op("pe", lambda e: e.matmul(pb[:, tt * 128:(tt + 1) * 128], lhsT=C.ones_bf[:], rhs=C.diag[s][:], start=(tt == 0), stop=(tt == 3), skip_group_check=True),
                         reads=[C.t_diag[s], C.t_const], writes=[tp])
                k.op("act", lambda e: e.copy(out=C.cmb[:, j, half * 512:(half + 1) * 512], in_=pb[:]), reads=[tp], writes=[C.t_cmb[j][half]])
        for hb in range(8):
            j = hb // 2
            e_id = gi * 4 + j
            cols = slice((hb % 2) * 256, (hb % 2) * 256 + 256)
            b1, tw1 = load_wblock(C, [(w1[e_id][:, cols], 0)])
            b3, tw3 = load_wblock(C, [(w3[e_id][:, cols], 0)])
            for m in range(2):
                hc = hb * 2 + m
                for th in range(2):
                    hs = slice(C.HOFF + th * 512, C.HOFF + (th + 1) * 512)
                    pa, tpa = next_pb(C)
                    pbb, tpb = next_pb(C)
                    for kc in range(NCH):
                        k.op("pe", lambda e: e.matmul(pa[:], lhsT=b1[:, kc, m * 128:(m + 1) * 128], rhs=C.hT[:, kc, hs], start=(kc == 0), stop=(kc == NCH - 1)),
                             reads=[tw1, C.t_h[kc][1 + th]], writes=[tpa], inc=(kc == NCH - 1))
                    for kc in range(NCH):
                        k.op("pe", lambda e: e.matmul(pbb[:], lhsT=b3[:, kc, m * 128:(m + 1) * 128], rhs=C.hT[:, kc, hs], start=(kc == 0), stop=(kc == NCH - 1)),
                             reads=[tw3, C.t_h[kc][1 + th]], writes=[tpb], inc=(kc == NCH - 1))
                    s = (hc * 2 + th) % 2
                    k.op("act", lambda e: e.activation(out=C.sa[s][:], in_=pa[:], func=AF.Silu), reads=[tpa], writes=[C.t_sa[s]])
                    k.op("dve", lambda e: e.tensor_tensor(out=C.sa[s][:], in0=C.sa[s][:], in1=pbb[:], op=ALU.mult), reads=[C.t_sa[s], tpb], writes=[C.t_sa[s]])
                    k.op("dve", lambda e: e.tensor_tensor(out=C.big[:, hc, tsl(th)], in0=C.sa[s][:], in1=C.cmb[:, j, tsl(th)], op=ALU.mult),
                         reads=[C.t_sa[s], C.t_cmb[j][th]], writes=[C.t_big[hc][th]])
        w2g = w2[gi * 4:(gi + 1) * 4].rearrange("e h n -> (e h) n")
        for ob in range(8):
            bw, tw = load_wblock(C, [(w2g[:, ob * 256:(ob + 1) * 256], 0)])
            for m in range(2):
                c = ob * 2 + m
                for th in range(2):
                    pb, tp = next_pb(C)
                    for kc in range(NCH):
                        k.op("pe", lambda e: e.matmul(pb[:], lhsT=bw[:, kc, m * 128:(m + 1) * 128], rhs=C.big[:, kc, tsl(th)], start=(kc == 0), stop=(kc == NCH - 1)),
                             reads=[tw, C.t_big[kc][th]], writes=[tp], inc=(kc == NCH - 1))
                    k.op("dve", lambda e: e.scalar_tensor_tensor(out=C.xT[:, c, tsl(th)], in0=pb[:], scalar=vec[:, W["gp"] + c:W["gp"] + c + 1],
                                                                 in1=C.xT[:, c, tsl(th)], op0=ALU.mult, op1=ALU.add),
                         reads=[tp, C.t_vec, C.t_x[c][th]], writes=[C.t_x[c][th]])
    C.wb_cur = None
    C.t_wb_cur = None
    C.wbi = 0


V_MODT = 0
V_SH_T, V_SC_T, V_G_T, V_SH_C, V_SC_C, V_G_C = 0, 16, 32, 48, 64, 80
V_LN_T_G, V_LN_T_B, V_LN_C_G, V_LN_C_B = 96, 112, 128, 144
V_BQ, V_BK = 160, 176
V_INVF, V_M16, V_OM16, V_SGN = 180, 181, 182, 183
V_SC1_T, V_GP_T, V_SC1_C, V_GP_C, V_A_C, V_B_C = 184, 200, 216, 232, 248, 264
V_A_N, V_B_N = 280, 296
NV = 312
RBYTES = 34816


def setup_region(C):
    k = C.k
    C.R = k.sb("R", [128, RBYTES // 2], BF16)

    def f32v(b0, n):
        return C.R[:, b0 // 2:b0 // 2 + 2 * n].bitcast(F32)

    def bfv(b0, n):
        return C.R[:, b0 // 2:b0 // 2 + n]
    C.f32v, C.bfv = f32v, bfv
    C.stage = [f32v(0, 2048), f32v(8192, 2048)]
    C.t_stage = [Tok(), Tok()]
    C.h32 = [f32v(0, 1024), f32v(4096, 1024)]
    C.t_h32 = [Tok(), Tok()]
    C.cmb = bfv(8192, 4096).rearrange("p (j t) -> p j t", j=4)
    C.t_cmb = [[Tok(), Tok()] for _ in range(4)]
    C.sa = [f32v(16384, 512), f32v(18432, 512)]
    C.t_sa = [Tok(), Tok()]
    C.tmp = [f32v(20480, 512), f32v(22528, 512)]
    C.t_tmp = [Tok(), Tok()]
    C.stat = [f32v(24576, 512), f32v(26624, 512), f32v(28672, 512)]
    C.t_stat = [Tok(), Tok(), Tok()]
    C.vec = k.sb("vec", [128, NV], F32)
    C.t_vec = Tok("vec")
    C.wr = k.sb("wr", [128, NCH, 20], F32)
    C.brow = k.sb("brow", [128, 20], F32)
    C.t_wr = Tok()
    C.lg = k.sb("lg", [128, 8, 20], F32)
    C.t_lg = Tok()
    C.comb = k.sb("comb", [128, 8, 16], F32)
    C.t_comb = Tok()
    C.rt = {}
    for nm, shp in (("gmax", [128, 1]), ("ngmax", [128, 1]), ("gsum", [128, 1]), ("gprob", [128, 1]), ("ge", [128, 4]), ("gw", [128, 4]),
                    ("m1", [128, 4]), ("m2", [128, 4]), ("den", [128, 4]), ("eq1", [128, 4, 4]), ("rl2", [128, 4, 4]),
                    ("top2", [128, 4, 4]), ("dd", [128, 4, 4]), ("ee", [128, 4, 4])):
        C.rt[nm] = k.sb("rt_" + nm, shp, F32)
    C.t_rt = Tok()
    C.diag = [k.sb("diag%d" % i, [128, 128], BF16) for i in range(2)]
    C.t_diag = [Tok(), Tok()]
    C.E = [k.sb("E%d" % i, [128, 512], BF16) for i in range(4)]
    C.t_E = [Tok() for _ in range(4)]
    C.Ei = 0


def derive_vec(C):
    k = C.k
    v = C.vec
    tv = C.t_vec

    def ts(out_c, in_c, s1, s2, o0, o1=None):
        if o1 is None:
            k.op("dve", lambda e: e.tensor_scalar(out=v[:, out_c:out_c + 16], in0=v[:, in_c:in_c + 16], scalar1=s1, scalar2=None, op0=o0), reads=[tv], writes=[tv])
        else:
            k.op("dve", lambda e: e.tensor_scalar(out=v[:, out_c:out_c + 16], in0=v[:, in_c:in_c + 16], scalar1=s1, scalar2=s2, op0=o0, op1=o1), reads=[tv], writes=[tv])
    ts(V_SC1_T, V_SC_T, 1.0, None, ALU.add)
    ts(V_SC1_C, V_SC_C, 1.0, None, ALU.add)
    ts(V_GP_T, V_G_T, 1.0 / ALPHA, None, ALU.mult)
    ts(V_GP_C, V_G_C, 1.0 / ALPHA, None, ALU.mult)
    k.op("dve", lambda e: e.tensor_tensor(out=v[:, V_A_C:V_A_C + 16], in0=v[:, V_LN_T_G:V_LN_T_G + 16], in1=v[:, V_SC1_C:V_SC1_C + 16], op=ALU.mult), reads=[tv], writes=[tv])
    k.op("dve", lambda e: e.tensor_tensor(out=v[:, V_B_C:V_B_C + 16], in0=v[:, V_LN_T_B:V_LN_T_B + 16], in1=v[:, V_SC1_C:V_SC1_C + 16], op=ALU.mult), reads=[tv], writes=[tv])
    k.op("dve", lambda e: e.tensor_tensor(out=v[:, V_B_C:V_B_C + 16], in0=v[:, V_B_C:V_B_C + 16], in1=v[:, V_SH_C:V_SH_C + 16], op=ALU.add), reads=[tv], writes=[tv])


def rope_tables(C, pos_dram, ntok, Tcos, Tsin, t_trig, wk_i, wk_f, wk_a, pre=()):
    k = C.k
    v = C.vec
    tw = Tok()
    TWO_PI = 2.0 * math.pi
    k.dma("sp", wk_i, pos_dram[0:1, :].to_broadcast([128, ntok]), writes=[tw, t_trig] + list(pre))
    k.op("dve", lambda e: e.tensor_copy(out=wk_f, in_=wk_i), reads=[tw], writes=[tw])
    k.op("dve", lambda e: e.tensor_scalar(out=wk_f, in0=wk_f, scalar1=v[:, V_INVF:V_INVF + 1], scalar2=None, op0=ALU.mult), reads=[tw, C.t_vec], writes=[tw])
    for (off, dst, s1, s2) in ((0.0, Tsin, V_SGN, None), (0.5 * math.pi, Tcos, V_M16, V_OM16)):
        ta = Tok()
        k.op("dve", lambda e: e.tensor_scalar(out=wk_a, in0=wk_f, scalar1=off, scalar2=None, op0=ALU.add), reads=[tw], writes=[ta])
        k.op("dve", lambda e: e.tensor_scalar(out=wk_i, in0=wk_a, scalar1=1.0 / TWO_PI, scalar2=None, op0=ALU.mult), reads=[ta, tw], writes=[tw])
        k.op("dve", lambda e: e.tensor_copy(out=dst, in_=wk_i), reads=[tw], writes=[t_trig])
        k.op("dve", lambda e: e.scalar_tensor_tensor(out=wk_a, in0=dst, scalar=-TWO_PI, in1=wk_a, op0=ALU.mult, op1=ALU.add), reads=[t_trig, ta], writes=[ta])
        k.op("dve", lambda e: e.tensor_scalar(out=dst, in0=wk_a, scalar1=math.pi, scalar2=-TWO_PI, op0=ALU.is_gt, op1=ALU.mult), reads=[ta], writes=[t_trig])
        k.op("dve", lambda e: e.tensor_tensor(out=wk_a, in0=wk_a, in1=dst, op=ALU.add), reads=[ta, t_trig], writes=[ta])
        k.op("dve", lambda e: e.tensor_scalar(out=dst, in0=wk_a, scalar1=-math.pi, scalar2=TWO_PI, op0=ALU.is_lt, op1=ALU.mult), reads=[ta], writes=[t_trig])
        k.op("dve", lambda e: e.tensor_tensor(out=wk_a, in0=wk_a, in1=dst, op=ALU.add), reads=[ta, t_trig], writes=[ta])
        k.op("dve", lambda e: e.tensor_scalar(out=wk_a, in0=wk_a, scalar1=-math.pi, scalar2=math.pi, op0=ALU.max, op1=ALU.min), reads=[ta], writes=[ta])
        k.op("act", lambda e: e.activation(out=dst, in_=wk_a, func=AF.Sin), reads=[ta], writes=[t_trig])
        if s2 is None:
            k.op("dve", lambda e: e.tensor_scalar(out=dst, in0=dst, scalar1=v[:, s1:s1 + 1], scalar2=None, op0=ALU.mult), reads=[t_trig, C.t_vec], writes=[t_trig])
        else:
            k.op("dve", lambda e: e.tensor_scalar(out=dst, in0=dst, scalar1=v[:, s1:s1 + 1], scalar2=v[:, s2:s2 + 1], op0=ALU.mult, op1=ALU.add),
                 reads=[t_trig, C.t_vec], writes=[t_trig])


def rope_evac(C, pb, tp, n, bias_ap, Tcos_s, Tsin_s, t_trig, dst, t_dst, wk):
    k = C.k
    i = wk["i"]
    wk["i"] = (i + 1) % 2
    qb, t1, t2 = wk["qb"][i][:, 0:n], wk["t1"][i][:, 0:n], wk["t2"][i][:, 0:n]
    tq, tt1, tt2 = wk["tq"][i], wk["tt1"][i], wk["tt2"][i]
    k.op("act", lambda e: e.activation(out=qb, in_=pb[:, 0:n], func=AF.Identity, bias=bias_ap, scale=1.0), reads=[tp, C.t_vec], writes=[tq])
    pr, tpr = next_pb(C)
    k.op("pe", lambda e: e.matmul(pr[:, 0:n], lhsT=C.rperm[:], rhs=qb, start=True, stop=True), reads=[tq, C.t_const], writes=[tpr])
    k.op("dve", lambda e: e.tensor_tensor(out=t2, in0=pr[:, 0:n], in1=Tsin_s, op=ALU.mult), reads=[tpr, t_trig], writes=[tt2])
    k.op("pool", lambda e: e.tensor_tensor(out=t1, in0=qb, in1=Tcos_s, op=ALU.mult), reads=[tq, t_trig], writes=[tt1])
    k.op("dve", lambda e: e.tensor_tensor(out=dst, in0=t1, in1=t2, op=ALU.add), reads=[tt1, tt2], writes=[t_dst])


def swa_layer(C, W):
    k = C.k
    v = C.vec
    NTOK = NT + HALO0
    f32v, bfv = C.f32v, C.bfv
    Vaug = bfv(0, 9 * 4 * 65).rearrange("p (t g d) -> p t g d", t=9, g=4)
    t_V = [Tok() for _ in range(9)]
    wk = {"i": 0,
          "qb": [bfv(4736, 512), bfv(5760, 512)], "tq": [Tok(), Tok()],
          "t1": [f32v(6784, 512), f32v(8832, 512)], "tt1": [Tok(), Tok()],
          "t2": [f32v(10880, 512), f32v(12928, 512)], "tt2": [Tok(), Tok()]}
    otok = [bfv(6784, 2048), bfv(10880, 2048)]
    t_otok = [Tok(), Tok()]
    Tcos = f32v(16384, NTOK)
    Tsin = f32v(16384 + 4608, NTOK)
    t_trig = Tok()
    KT = bfv(25600, 4 * NTOK).rearrange("p (g t) -> p g t", g=4)
    t_K = [[Tok() for _ in range(3)] for _ in range(4)]
    wk_i = C.big[:, 0:3, :].rearrange("p a t -> p (a t)")[:, 0:2 * NTOK].bitcast(I32)
    wk_f = C.big[:, 3:6, :].rearrange("p a t -> p (a t)")[:, 0:2 * NTOK].bitcast(F32)
    wk_a = C.big[:, 6:9, :].rearrange("p a t -> p (a t)")[:, 0:2 * NTOK].bitcast(F32)
    rope_tables(C, W["pos"], NTOK, Tcos, Tsin, t_trig, wk_i, wk_f, wk_a, pre=[C.t_big[c__][th__] for c__ in range(9) for th__ in range(2)])
    for c in range(9):
        for th in range(2):
            C.t_big[c][th].w = t_trig.w
    wq = W["w_qkv"]
    blocks = [([(wq[:, b * WB:(b + 1) * WB], 0)], 2) for b in range(8)]

    def q_evac(bi, m, th, pb, tp):
        c = bi * 2 + m
        cs = slice(HALO0 + th * 512, HALO0 + (th + 1) * 512)
        rope_evac(C, pb, tp, 512, v[:, V_BQ + c:V_BQ + c + 1], Tcos[:, cs], Tsin[:, cs], t_trig, C.big[:, c, tsl(th)], C.t_big[c][th], wk)
    linear_fm(C, blocks, lambda kc, th: C.hT[:, kc, HALO0 + th * 512:HALO0 + (th + 1) * 512],
              lambda th: (lambda kc: [C.t_h[kc][1 + th]]), 2, q_evac)
    kblocks = []
    for b in range(2):
        srcs = []
        for m in range(2):
            g = b * 2 + m
            col = 2048 + g * 64
            srcs.append((wq[:, col:col + 64], m * 128))
            srcs.append((wq[:, col:col + 64], m * 128 + 64))
        kblocks.append((srcs, 2))

    def k_evac(bi, m, pc, pb, tp):
        g = bi * 2 + m
        cs = slice(pc * 384, (pc + 1) * 384)
        rope_evac(C, pb, tp, 384, v[:, V_BK + g:V_BK + g + 1], Tcos[:, cs], Tsin[:, cs], t_trig, KT[:, g, cs], t_K[g][pc], wk)

    def k_rt(pc):
        def f(kc):
            return [C.t_h[kc][0], C.t_h[kc][1], C.t_h[kc][2]]
        return f
    linear_fm(C, kblocks, lambda kc, pc: C.hT[:, kc, pc * 384:(pc + 1) * 384], k_rt, 3, k_evac)
    tvones = Tok()
    k.op("pool", lambda e: e.memset(Vaug[:, :, :, 64:65], 1.0), writes=t_V)
    vbuf, tvb = load_wblock(C, [(wq[:, 2304:2560], 0)])
    for t in range(9):
        pb, tp = next_pb(C)
        for kc in range(NCH):
            k.op("pe", lambda e: e.matmul(pb[:, 0:256], lhsT=C.hT[:, kc, t * 128:(t + 1) * 128], rhs=vbuf[:, kc, :], start=(kc == 0), stop=(kc == NCH - 1)),
                 reads=[tvb, C.t_h[kc][0], C.t_h[kc][1], C.t_h[kc][2]], writes=[tp], inc=(kc == NCH - 1))
        k.op("dve", lambda e: e.tensor_tensor(out=Vaug[:, t, :, 0:64], in0=pb[:, 0:256].rearrange("p (g d) -> p g d", g=4),
                                              in1=C.bvrow[:].rearrange("p (g d) -> p g d", g=4), op=ALU.add),
             reads=[tp, C.t_wr], writes=[t_V[t]])
    esink = C.esink[:].rearrange("p (i two) -> p i two", two=2)
    for j in range(8):
        so = j % 2
        ot = otok[so]
        otv = ot.rearrange("p (i two d) -> p i two d", two=2, d=64)
        for g in range(4):
            for par in range(2):
                ps_ = slice(par * 64, (par + 1) * 64)
                Es = []
                for (kt, mask) in ((j, C.mprev0 if j == 0 else C.mprev), (j + 1, C.mdiag)):
                    pb, tp = next_pb(C)
                    kcs = slice(kt * 128, (kt + 1) * 128)
                    k.op("pe", lambda e: e.matmul(pb[:].rearrange("p (h q) -> p h q", h=4), lhsT=KT[ps_, g, kcs],
                                                  rhs=C.big[ps_, 4 * g:4 * g + 4, j * 128:(j + 1) * 128], start=True, stop=True),
                         reads=[t_K[g][kt // 3], C.t_big[4 * g][j // 4], C.t_big[4 * g + 1][j // 4], C.t_big[4 * g + 2][j // 4], C.t_big[4 * g + 3][j // 4]],
                         writes=[tp])
                    ei = C.Ei
                    C.Ei = (ei + 1) % 4
                    E, tE = C.E[ei], C.t_E[ei]
                    k.op("act", lambda e: e.activation(out=E[:], in_=pb[:], func=AF.Exp, scale=SCALE), reads=[tp], writes=[tE])
                    k.op("dve", lambda e: e.tensor_tensor(out=E[:].rearrange("p (h q) -> p h q", h=4), in0=E[:].rearrange("p (h q) -> p h q", h=4),
                                                          in1=mask[:].unsqueeze(1).to_broadcast([128, 4, 128]), op=ALU.mult),
                         reads=[tE, C.t_const], writes=[tE])
                    Es.append((E, tE, kt))
                po, tpo = next_pb(C)
                first = True
                for hh in range(4):
                    for ii, (E, tE, kt) in enumerate(Es):
                        k.op("pe", lambda e: e.matmul(po[:, hh * 65:(hh + 1) * 65], lhsT=E[:, hh * 128:(hh + 1) * 128], rhs=Vaug[:, kt, g, :],
                                                      start=first, stop=(ii == 1), skip_group_check=True),
                             reads=[tE, t_V[kt]], writes=[tpo], inc=(hh == 3 and ii == 1))
                        first = False
                pov = po[:, 0:260].rearrange("p (h d) -> p h d", d=65)
                den, tden = C.rt["den"], C.t_rt
                k.op("dve", lambda e: e.tensor_tensor(out=den[:], in0=pov[:, :, 64], in1=esink[:, 4 * g:4 * g + 4, par], op=ALU.add),
                     reads=[tpo, C.t_wr], writes=[tden])
                k.op("dve", lambda e: e.reciprocal(out=den[:], in_=den[:]), reads=[tden], writes=[tden])
                k.op("dve", lambda e: e.tensor_tensor(out=otv[:, 4 * g:4 * g + 4, par, :], in0=pov[:, :, 0:64],
                                                      in1=den[:].unsqueeze(2).to_broadcast([128, 4, 64]), op=ALU.mult),
                     reads=[tpo, tden], writes=[t_otok[so]])
        for c0 in range(0, NCH, 4):
            pb, tp = next_pb(C)
            pbv = pb[:].bitcast(BF16)
            for jj in range(4):
                c = c0 + jj
                k.op("pe", lambda e: e.transpose(pbv[:, jj * 128:(jj + 1) * 128], ot[:, c * 128:(c + 1) * 128], C.ident_bf[:]),
                     reads=[t_otok[so], C.t_const], writes=[tp], inc=(jj == 3))
            th = j // 4
            k.op("act", lambda e: e.copy(out=C.hT[:, c0:c0 + 4, HALO0 + j * 128:HALO0 + (j + 1) * 128], in_=pbv[:, 0:512].rearrange("p (c q) -> p c q", c=4)),
                 reads=[tp], writes=[C.t_h[c0 + i][1 + th] for i in range(4)])
    wo = W["w_o"]
    blocks = [([(wo[:, b * WB:(b + 1) * WB], 0)], 2) for b in range(8)]

    def o_evac(bi, m, th, pb, tp):
        c = bi * 2 + m
        k.op("dve", lambda e: e.scalar_tensor_tensor(out=C.xT[:, c, tsl(th)], in0=pb[:], scalar=v[:, V_GP_T + c:V_GP_T + c + 1],
                                                     in1=C.xT[:, c, tsl(th)], op0=ALU.mult, op1=ALU.add),
             reads=[tp, C.t_vec, C.t_x[c][th]], writes=[C.t_x[c][th]])
    linear_fm(C, blocks, lambda kc, th: C.hT[:, kc, HALO0 + th * 512:HALO0 + (th + 1) * 512],
              lambda th: (lambda kc: [C.t_h[kc][1 + th]]), 2, o_evac)


def store_x(C, out_dram):
    k = C.k
    for t in range(8):
        s = t % 2
        for c0 in range(0, NCH, 4):
            pb, tp = next_pb(C)
            for jj in range(4):
                c = c0 + jj
                k.op("pe", lambda e: e.transpose(pb[:, jj * 128:(jj + 1) * 128], C.xT[:, c, t * 128:(t + 1) * 128], C.ident_f[:]),
                     reads=[C.t_x[c][t // 4], C.t_const], writes=[tp], inc=(jj == 3))
            k.op("act" if (c0 // 4) % 2 == 0 else "dve",
                 (lambda e: e.copy(out=C.stage[s][:, c0 * 128:(c0 + 4) * 128], in_=pb[:])) if (c0 // 4) % 2 == 0 else
                 (lambda e: e.tensor_copy(out=C.stage[s][:, c0 * 128:(c0 + 4) * 128], in_=pb[:])),
                 reads=[tp], writes=[C.t_stage[s]])
        k.dma("sp", out_dram[t * 128:(t + 1) * 128, :], C.stage[s], reads=[C.t_stage[s]], writes=[C.t_out])


def load_x(C, x_dram, has_halo):
    k = C.k
    v = C.vec
    n_tiles = 9 if has_halo else 8

    def dst(t, c0, pv, tp):
        if has_halo and t == 0:
            for jj in range(4):
                c = c0 + jj
                k.op("act", lambda e: e.activation(out=C.hT[:, c, 0:128], in_=pv[:, jj, :], func=AF.Identity,
                                                   scale=v[:, V_SC1_T + c:V_SC1_T + c + 1], bias=v[:, V_SH_T + c:V_SH_T + c + 1]),
                     reads=[tp, C.t_vec], writes=[C.t_h[c][0]])
        else:
            to = t - 1 if has_halo else t
            k.op("act", lambda e: e.copy(out=C.xT[:, c0:c0 + 4, to * 128:(to + 1) * 128], in_=pv),
                 reads=[tp], writes=[C.t_x[c0 + i][to // 4] for i in range(4)])
    load_x_transposed(C, x_dram, n_tiles, dst, C.stage, C.t_stage)
    for c in range(NCH):
        for th in range(2):
            k.op("dve" if c % 2 == 0 else "pool",
                 lambda e: e.tensor_scalar(out=C.hT[:, c, C.HOFF + th * 512:C.HOFF + (th + 1) * 512], in0=C.xT[:, c, tsl(th)],
                                           scalar1=v[:, V_SC1_T + c:V_SC1_T + c + 1], scalar2=v[:, V_SH_T + c:V_SH_T + c + 1], op0=ALU.mult, op1=ALU.add),
                 reads=[C.t_x[c][th], C.t_vec], writes=[C.t_h[c][1 + th]])


def build_layer0():
    nc = bass.Bass("TRN2", target_bir_lowering=False)

    def din(name, shape, dt=F32):
        return nc.dram_tensor(name, list(shape), dt, kind="ExternalInput").ap()
    xin = din("xin", [NT + HALO0, D])
    pos = din("pos", [1, NT + HALO0], I32)
    vec_d = din("vec", [128, NV])
    ident = din("ident_f", [128, 128])
    masks = din("masks", [128, 4, 128])
    rows = din("rows", [128, 256 + 32 + 20])
    w_qkv = din("w_qkv", [D, 2560])
    w_o = din("w_o", [D, D])
    wr_d = din("wr", [D, 20])
    w1 = din("w1", [16, D, 512])
    w3 = din("w3", [16, D, 512])
    w2 = din("w2", [16, 512, D])
    xout = nc.dram_tensor("xout", [NT, D], F32, kind="ExternalOutput").ap()
    with ExitStack() as st:
        k = K(nc, st)
        C = setup_ctx(k, {"ident_f": ident})
        setup_region(C)
        C.t_out = Tok()
        C.mk = k.sb("mk", [128, 4, 128], BF16)
        k.dma("pool", C.mk[:], masks, writes=[C.t_const])
        C.mdiag, C.mprev, C.mprev0, C.rperm = C.mk[:, 0, :], C.mk[:, 1, :], C.mk[:, 2, :], C.mk[:, 3, :]
        C.rowsb = k.sb("rowsb", [128, 308], F32)
        C.bvrow = C.rowsb[:, 0:256]
        C.esink = k.sb("esink", [128, 32], F32)
        C.brow = C.rowsb[:, 288:308]
        k.dma("sp", C.rowsb[:], rows, writes=[C.t_wr])
        k.op("act", lambda e: e.activation(out=C.esink[:], in_=C.rowsb[:, 256:288], func=AF.Exp), reads=[C.t_wr], writes=[C.t_wr])
        k.dma("sp", C.wr[:], wr_d.rearrange("(kc p) n -> p kc n", p=128), writes=[C.t_wr])
        k.dma("sp", C.vec[:], vec_d, writes=[C.t_vec])
        derive_vec(C)
        load_x(C, xin, True)
        swa_layer(C, {"pos": pos, "w_qkv": w_qkv, "w_o": w_o})
        layer_norm_mod(C, C.vec, V_LN_T_G, V_LN_T_B, V_A_C, V_B_C, C.tmp, C.t_tmp, C.stat, C.t_stat)
        moe_layer(C, 0, C.vec, {"sc1": V_SC1_C, "sh": V_SH_C, "gp": V_GP_C, "w1": w1, "w3": w3, "w2": w2})
        layer_norm_mod(C, C.vec, V_LN_C_G, V_LN_C_B, None, None, C.tmp, C.t_tmp, C.stat, C.t_stat)
        store_x(C, xout)
        k.finish([C.t_out])
        print("layer0 program: %d instructions" % k.ninst, k.cnt)
    return nc


def fm(vv):
    return np.ascontiguousarray(np.asarray(vv, np.float32).reshape(16, 128).T)


def rope_consts():
    p = np.arange(128)
    d = p % 64
    invf = np.where(d < 16, ROPE_THETA ** (-(2.0 * (d % 8)) / 16.0), 0.0).astype(np.float32)
    m16 = (d < 16).astype(np.float32)
    om16 = 1.0 - m16
    sgn = np.where(d < 8, -1.0, np.where(d < 16, 1.0, 0.0)).astype(np.float32)
    return invf, m16, om16, sgn


def const_masks(first_quarter):
    kk = np.arange(128)[:, None]
    qq = np.arange(128)[None, :]
    mdiag = (kk <= qq).astype(np.float32)
    mprev = (kk > qq).astype(np.float32)
    mprev0 = np.zeros_like(mprev) if first_quarter else mprev
    rperm = np.zeros((128, 128), np.float32)
    for m in range(128):
        d = m % 64
        if d < 8:
            rperm[m + 8, m] = 1.0
        elif d < 16:
            rperm[m - 8, m] = 1.0
    return np.ascontiguousarray(np.stack([mdiag, mprev, mprev0, rperm], axis=1))


def layer0_inputs(inp, modT, core):
    b, r = core // 4, core % 4
    i = 0
    t0 = r * NT
    x = inp["x"][b]
    halo = x[t0 - HALO0:t0] if r > 0 else np.zeros((HALO0, D), np.float32)
    xin = np.ascontiguousarray(np.concatenate([halo, x[t0:t0 + NT]], axis=0))
    p = inp["positions"][b]
    ph = p[t0 - HALO0:t0] if r > 0 else np.zeros((HALO0,), np.int32)
    pos = np.ascontiguousarray(np.concatenate([ph, p[t0:t0 + NT]])[None, :].astype(np.int32))
    vec = np.zeros((128, NV), np.float32)
    vec[:, 0:96] = modT
    vec[:, V_LN_T_G:V_LN_T_G + 16] = fm(inp["ln_t_g"][i])
    vec[:, V_LN_T_B:V_LN_T_B + 16] = fm(inp["ln_t_b"][i])
    vec[:, V_LN_C_G:V_LN_C_G + 16] = fm(inp["ln_c_g"][i])
    vec[:, V_LN_C_B:V_LN_C_B + 16] = fm(inp["ln_c_b"][i])
    bq = inp["swa_b_qkv"][0]
    vec[:, V_BQ:V_BQ + 16] = fm(bq[0:2048])
    for g in range(4):
        bk = bq[2048 + g * 64:2048 + (g + 1) * 64]
        vec[:, V_BK + g] = np.concatenate([bk, bk])
    invf, m16, om16, sgn = rope_consts()
    vec[:, V_INVF], vec[:, V_M16], vec[:, V_OM16], vec[:, V_SGN] = invf, m16, om16, sgn
    rows = np.zeros((128, 308), np.float32)
    rows[:, 0:256] = bq[2304:2560][None, :]
    rows[:, 256:288] = inp["swa_sinks"][0][None, :]
    rows[:, 288:292] = inp["moe_b_group"][i][None, :]
    rows[:, 292:308] = inp["moe_b_router"][i][None, :]
    wr = np.ascontiguousarray(np.concatenate([inp["moe_w_group"][i], inp["moe_w_router"][i]], axis=1))
    return {"xin": xin, "pos": pos, "vec": vec, "ident_f": np.eye(128, dtype=np.float32), "masks": const_masks(r == 0),
            "rows": rows, "w_qkv": inp["swa_w_qkv"][0], "w_o": inp["swa_w_o"][0], "wr": wr,
            "w1": inp["moe_w1"][i], "w3": inp["moe_w3"][i], "w2": inp["moe_w2"][i]}


NSA_Q = 2048
KVW = 256
HALO1 = 512


def build_layer1a():
    nc = bass.Bass("TRN2", target_bir_lowering=False)

    def din(name, shape, dt=F32):
        return nc.dram_tensor(name, list(shape), dt, kind="ExternalInput").ap()
    xin = din("xin", [NT, D])
    pos = din("pos", [1, NT], I32)
    vec_d = din("vec", [128, NV])
    ident = din("ident_f", [128, 128])
    masks = din("masks", [128, 4, 128])
    w_in = din("w_in", [D, 3680])
    outs = {}
    for nm in ("kc_T", "vc_T", "ks_T", "kw_T"):
        outs[nm] = nc.dram_tensor(nm, [128, 4, NT], BF16, kind="ExternalOutput").ap()
    for nm in ("vs", "vw"):
        outs[nm] = nc.dram_tensor(nm, [128, 8, 256], BF16, kind="ExternalOutput").ap()
    with ExitStack() as st:
        k = K(nc, st)
        C = setup_ctx(k, {"ident_f": ident}, hoff=0)
        setup_region(C)
        C.t_out = Tok()
        C.mk = k.sb("mk", [128, 4, 128], BF16)
        k.dma("pool", C.mk[:], masks, writes=[C.t_const])
        C.rperm = C.mk[:, 3, :]
        k.dma("sp", C.vec[:], vec_d, writes=[C.t_vec])
        derive_vec(C)
        load_x(C, xin, False)
        v = C.vec
        f32v, bfv = C.f32v, C.bfv
        wk = {"i": 0,
              "qb": [bfv(4736, 512), bfv(5760, 512)], "tq": [Tok(), Tok()],
              "t1": [f32v(6784, 512), f32v(8832, 512)], "tt1": [Tok(), Tok()],
              "t2": [f32v(10880, 512), f32v(12928, 512)], "tt2": [Tok(), Tok()]}
        Tcos = f32v(16384, NT)
        Tsin = f32v(16384 + 4096, NT)
        t_trig = Tok()
        wk_i = C.big[:, 0:2, :].rearrange("p a t -> p (a t)").bitcast(I32)
        wk_f = C.big[:, 2:4, :].rearrange("p a t -> p (a t)").bitcast(F32)
        wk_a = C.big[:, 4:6, :].rearrange("p a t -> p (a t)").bitcast(F32)
        rope_tables(C, pos, NT, Tcos, Tsin, t_trig, wk_i, wk_f, wk_a)
        KO = {"kc_T": 0, "vc_T": 1, "ks_T": 2, "kw_T": 3}
        col0 = {"kc_T": 2048, "vc_T": 2304, "ks_T": 2560, "kw_T": 3072}
        stg = C.big[:, 0:16, :].rearrange("p (a g) t -> p a g t", g=4)
        t_stg = [[Tok() for _ in range(4)] for _ in range(4)]
        for a in range(4):
            for g in range(4):
                t_stg[a][g].w = t_trig.w
        kblocks = []
        order = []
        for nm in ("kc_T", "vc_T", "ks_T", "kw_T"):
            for b in range(2):
                srcs = []
                for m in range(2):
                    g = b * 2 + m
                    col = col0[nm] + g * 64
                    srcs.append((w_in[:, col:col + 64], m * 128))
                    srcs.append((w_in[:, col:col + 64], m * 128 + 64))
                kblocks.append((srcs, 2))
                order.append((nm, b))
        C.zero1 = k.sb("zero1", [128, 1], F32)
        k.op("dve", lambda e: e.memset(C.zero1[:], 0.0), writes=[C.t_vec])

        def evac(bi, m, th, pb, tp):
            nm, b = order[bi]
            g = b * 2 + m
            a = KO[nm]
            dst = stg[:, a, g, tsl(th)]
            if nm in ("ks_T", "kw_T"):
                cs = tsl(th)
                rope_evac(C, pb, tp, 512, C.zero1[:, 0:1], Tcos[:, cs], Tsin[:, cs], t_trig, dst, t_stg[a][g], wk)
            else:
                k.op("act", lambda e: e.copy(out=dst, in_=pb[:]), reads=[tp], writes=[t_stg[a][g]])
        linear_fm(C, kblocks, lambda kc, th: C.hT[:, kc, C.HOFF + th * 512:C.HOFF + (th + 1) * 512],
                  lambda th: (lambda kc: [C.t_h[kc][1 + th]]), 2, evac)
        for nm in ("kc_T", "vc_T", "ks_T", "kw_T"):
            a = KO[nm]
            k.dma("sp", outs[nm], stg[:, a, :, :], reads=t_stg[a], writes=[C.t_out])
        vst = [C.stage[0].bitcast(BF16)[:, 0:2048].rearrange("p (t n) -> p t n", t=8), C.stage[1].bitcast(BF16)[:, 0:2048].rearrange("p (t n) -> p t n", t=8)]
        for vi, (nm, col) in enumerate((("vs", 2816), ("vw", 3328))):
            vbuf, tvb = load_wblock(C, [(w_in[:, col:col + 256], 0)])
            for t in range(8):
                pb, tp = next_pb(C)
                for kc in range(NCH):
                    k.op("pe", lambda e: e.matmul(pb[:, 0:256], lhsT=C.hT[:, kc, C.HOFF + t * 128:C.HOFF + (t + 1) * 128], rhs=vbuf[:, kc, :], start=(kc == 0), stop=(kc == NCH - 1)),
                         reads=[tvb, C.t_h[kc][1], C.t_h[kc][2]], writes=[tp], inc=(kc == NCH - 1))
                k.op("act", lambda e: e.copy(out=vst[vi][:, t, :], in_=pb[:, 0:256]), reads=[tp], writes=[C.t_stage[vi]])
            k.dma("sp", outs[nm], vst[vi], reads=[C.t_stage[vi]], writes=[C.t_out])
        k.finish([C.t_out])
        print("layer1a program: %d instructions" % k.ninst, k.cnt)
    return nc


def layer1a_inputs(inp, modT, x1_core, core):
    b, r = core // 4, core % 4
    t0 = r * NT
    pos = np.ascontiguousarray(inp["positions"][b][t0:t0 + NT][None, :].astype(np.int32))
    vec = np.zeros((128, NV), np.float32)
    vec[:, 0:96] = modT
    invf, m16, om16, sgn = rope_consts()
    vec[:, V_INVF], vec[:, V_M16], vec[:, V_OM16], vec[:, V_SGN] = invf, m16, om16, sgn
    return {"xin": np.ascontiguousarray(x1_core), "pos": pos, "vec": vec, "ident_f": np.eye(128, dtype=np.float32),
            "masks": const_masks(False), "w_in": inp["nsa_w_in"][0]}


DBG = {'compress': True, 'cmp': True, 'sel': True, 'win': True, 'moe': True, 'glim': 4, 'jlim': 8}


def nsa_core(nc, k, C, A):
    v = C.vec
    pos, w_in, w_o, w1, w3, w2 = A.pos, A.w_in, A.w_o, A.w1, A.w3, A.w2
    phi_k1, phi_v1, causal, cvalid, sbias_d, xspill, xout = A.phi_k1, A.phi_v1, A.causal, A.cvalid, A.sbias_d, A.xspill, A.xout
    phi2_sb, peT_sb, ovl_sb, t_nc, kcT_all, vc_all, t_cmpkv = A.phi2_sb, A.peT_sb, A.ovl_sb, A.t_nc, A.kcT_all, A.vc_all, A.t_cmpkv
    kc_full = vc_full = None
    t_spill = Tok()
    allx = [C.t_x[c][th] for c in range(NCH) for th in range(2)]
    k.dma("sp", xspill, C.xT[:].rearrange("p c t -> p (c t)"), reads=allx, writes=[t_spill] + list(getattr(A, 'pre_toks', [])))
    X = C.xT[:].rearrange("p c t -> p (c t)").bitcast(BF16)

    def xbf(b0, n):
        return X[:, b0 // 2:b0 // 2 + n]

    def xf32(b0, n):
        return X[:, b0 // 2:b0 // 2 + 2 * n].bitcast(F32)
    xs_toks = []

    def xtok():
        t = Tok()
        t.w = t_spill.w
        xs_toks.append(t)
        return t
    q_raw = xbf(0, 4096).rearrange("p (c t) -> p c t", c=4)
    q_rot = xbf(8192, 4096).rearrange("p (c t) -> p c t", c=4)
    t_qraw = [[xtok(), xtok()] for _ in range(4)]
    t_qrot = [[xtok(), xtok()] for _ in range(4)]
    kvA = xbf(16384, 4096)
    t_kvA = xtok()
    Vs = xbf(24576, 32 * 65).rearrange("p (t d) -> p t d", d=65)
    t_Vs = xtok()
    Kw = xbf(28736, NT + HALO1)
    t_Kw = xtok()
    Vw = xbf(31808, 12 * 65).rearrange("p (t d) -> p t d", d=65)
    t_Vw = xtok()
    Tcos = xf32(34144, NT)
    Tsin = xf32(38240, NT)
    t_trig = xtok()
    wk = {"i": 0,
          "qb": [xbf(42336, 512), xbf(43360, 512)], "tq": [xtok(), xtok()],
          "t1": [xf32(44384, 512), xf32(46432, 512)], "tt1": [xtok(), xtok()],
          "t2": [xf32(48480, 512), xf32(50528, 512)], "tt2": [xtok(), xtok()]}
    selc = xbf(52576, 4096)
    t_selc = xtok()
    hid = xbf(60768, 512).rearrange("p (c n) -> p c n", c=2)
    t_hid = xtok()
    if kcT_all is None:
        kcT_all = xbf(61792, 1024).rearrange("p (g n) -> p g n", g=4)
        vc_all = xbf(63840, 520).rearrange("p (g nt d) -> p g nt d", g=4, nt=2)
        t_cmpkv = xtok()
    f32v, bfv = C.f32v, C.bfv
    wj = bfv(0, 4096)
    t_wj = Tok()
    otok = [bfv(8192, 512), bfv(9216, 512)]
    t_otok = [Tok(), Tok()]
    gates = f32v(16384, 768).rearrange("p (t n) -> p t n", t=8)
    t_gates = Tok()
    sbias = f32v(19456, 512).rearrange("p (j s) -> p j s", j=8)
    t_sb = Tok()
    cval = bfv(21504, 256).rearrange("p (nt t) -> p nt t", nt=2)
    t_cval = Tok()
    score = f32v(22016, 64)
    score2 = f32v(22272, 64)
    mx8 = f32v(22528, 8)
    selm = bfv(22592, 64)
    t_sc = Tok()
    acc = f32v(23552, 256).rearrange("p (h d) -> p h d", h=4)
    tacc = f32v(24576, 256).rearrange("p (h d) -> p h d", h=4)
    t_acc = Tok()
    fac = f32v(25600, 4)
    rcs = f32v(25632, 8)
    t_fac = Tok()
    impt = f32v(25664, 64)
    C.accP = [f32v(26624, 256).rearrange('p (h d) -> p h d', h=4), f32v(27648, 256).rearrange('p (h d) -> p h d', h=4)]
    C.t_accP = [Tok(), Tok()]
    oTt = [f32v(28672, 512), f32v(28672, 512)]
    t_o1 = Tok()
    t_oTt = [t_o1, t_o1]
    E6 = list(C.E) + [bfv(30720, 512), bfv(31744, 512)]
    tE6 = list(C.t_E) + [Tok(), Tok()]
    for t_ in C.t_accP + [t_o1] + tE6[4:]:
        t_.w = t_spill.w
    st6 = {"i": 0}

    def nextE():
        i_ = st6["i"]
        st6["i"] = (i_ + 1) % 6
        return E6[i_], tE6[i_]

    def back_to_token_major(pacc, tpacc, par):
        k.op("act", lambda e: e.copy(out=oTt[par][0:65, :], in_=pacc[0:65, :]), reads=[tpacc], writes=[t_oTt[par]])
        pt, tpt = npb()
        for hh in range(4):
            k.op("pe", lambda e: e.transpose(pt[:, hh * 65:(hh + 1) * 65], oTt[par][0:65, hh * 128:(hh + 1) * 128], C.ident_f[0:65, 0:65]),
                 reads=[t_oTt[par], C.t_const], writes=[tpt], inc=(hh == 3))
        return pt, tpt
    for t_ in (t_wj, t_otok[0], t_otok[1], t_gates, t_sb, t_cval, t_sc, t_acc, t_fac):
        t_.w = t_spill.w
    k.dma("sp", sbias, sbias_d.rearrange("p (j s) -> p j s", j=8), writes=[t_sb])
    rope_tables(C, pos, NT, Tcos, Tsin, t_trig, C.big[:, 0:2, :].rearrange("p a t -> p (a t)").bitcast(I32), C.big[:, 2:4, :].rearrange("p a t -> p (a t)").bitcast(F32), C.big[:, 4:6, :].rearrange("p a t -> p (a t)").bitcast(F32), pre=[C.t_big[c__][th__] for c__ in range(6) for th__ in range(2)])
    for c_ in range(6):
        for th_ in range(2):
            C.t_big[c_][th_].w = t_trig.w
    for tl_ in t_qraw + t_qrot:
        for t_ in tl_:
            t_.w = t_trig.w
    t_kvA.w = t_trig.w
    C.NPB = 6

    def npb():
        i = C.pbi % 4
        C.pbi = (i + 1) % 4
        return C.pb[i], C.t_pb[i]
    C.pbi = 0
    gbuf, tgb = load_wblock(C, [(w_in[:, 3584:3680], 0)])
    for t in range(8):
        pb, tp = npb()
        for kc in range(NCH):
            k.op("pe", lambda e: e.matmul(pb[:, 0:96], lhsT=C.hT[:, kc, C.HOFF + t * 128:C.HOFF + (t + 1) * 128], rhs=gbuf[:, kc, 0:96], start=(kc == 0), stop=(kc == NCH - 1)),
                 reads=[tgb, C.t_h[kc][1], C.t_h[kc][2]], writes=[tp], inc=(kc == NCH - 1))
        k.op("act", lambda e: e.activation(out=gates[:, t, :], in_=pb[:, 0:96], func=AF.Sigmoid), reads=[tp], writes=[t_gates])
    k.op("pool", lambda e: e.memset(kcT_all[:], 0.0), writes=[t_cmpkv])
    k.op("pool", lambda e: e.memset(vc_all[:], 1.0), writes=[t_cmpkv])
    k.op("pool", lambda e: e.memset(hid[:], 0.0), writes=[t_hid])
    kvP = kvA[:, 0:16 * 255].rearrange("p (jj n) -> p jj n", jj=16)
    for kv, (phi1_d, src_full, pecol) in enumerate(((phi_k1, None, 0), (phi_v1, None, 16)) if DBG['compress'] else ()):
        pbuf, tpw = load_wblock(C, [(phi1_d, 0)])
        for g in range(4):
            A.load_cmp_src(g, kv, kvA, t_kvA, selc, t_selc)
            if A.cmp_prepped:
                k.op("dve", lambda e: e.tensor_tensor(out=kvP, in0=kvP, in1=peT_sb[:, pecol:pecol + 16].unsqueeze(2).to_broadcast([128, 16, 255]), op=ALU.add),
                     reads=[t_kvA, t_nc], writes=[t_kvA])
            for hc in range(2):
                pb, tp = npb()
                for jj in range(16):
                    k.op("pe", lambda e: e.matmul(pb[:, 0:255], lhsT=pbuf[:, jj, hc * 128:(hc + 1) * 128], rhs=kvP[:, jj, :], start=(jj == 0), stop=(jj == 15)),
                         reads=[tpw, t_kvA], writes=[tp], inc=(jj == 15))
                k.op("act", lambda e: e.activation(out=hid[:, hc, 0:255], in_=pb[:, 0:255], func=AF.Silu), reads=[tp], writes=[t_hid])
            if kv == 0:
                pb, tp = npb()
                for hc in range(2):
                    k.op("pe", lambda e: e.matmul(pb[:, 0:255], lhsT=phi2_sb[:, hc, 0:128], rhs=hid[:, hc, 0:255], start=(hc == 0), stop=(hc == 1)),
                         reads=[t_nc, t_hid], writes=[tp], inc=(hc == 1))
                k.op("act", lambda e: e.copy(out=kcT_all[:, g, 0:255], in_=pb[:, 0:255]), reads=[tp], writes=[t_cmpkv])
            else:
                for nt in range(2):
                    pb, tp = npb()
                    for hc in range(2):
                        k.op("pe", lambda e: e.matmul(pb[:, 0:64], lhsT=hid[:, hc, nt * 128:(nt + 1) * 128], rhs=phi2_sb[:, hc, 128:192], start=(hc == 0), stop=(hc == 1)),
                             reads=[t_nc, t_hid], writes=[tp], inc=(hc == 1))
                    k.op("act", lambda e: e.copy(out=vc_all[:, g, nt, 0:64], in_=pb[:, 0:64]), reads=[tp], writes=[t_cmpkv])
    NEG = -1e30
    for g in range(DBG['glim']):
        blocks = [([(w_in[:, (4 * g + 2 * b) * 128:(4 * g + 2 * b + 2) * 128], 0)], 2) for b in range(2)]

        def q_evac(bi, m, th, pb, tp):
            cc = bi * 2 + m
            cs = tsl(th)
            qb = q_raw[:, cc, cs]
            tq = t_qraw[cc][th]
            k.op("act", lambda e: e.copy(out=qb, in_=pb[:]), reads=[tp], writes=[tq])
            pr, tpr = npb()
            k.op("pe", lambda e: e.matmul(pr[:], lhsT=C.rperm[:], rhs=qb, start=True, stop=True), reads=[tq, C.t_const], writes=[tpr])
            i = wk["i"]
            wk["i"] = (i + 1) % 2
            t1, t2 = wk["t1"][i], wk["t2"][i]
            k.op("dve", lambda e: e.tensor_tensor(out=t2, in0=pr[:], in1=Tsin[:, cs], op=ALU.mult), reads=[tpr, t_trig], writes=[wk["tt2"][i]])
            k.op("pool", lambda e: e.tensor_tensor(out=t1, in0=qb, in1=Tcos[:, cs], op=ALU.mult), reads=[tq, t_trig], writes=[wk["tt1"][i]])
            k.op("dve", lambda e: e.tensor_tensor(out=q_rot[:, cc, cs], in0=t1, in1=t2, op=ALU.add), reads=[wk["tt1"][i], wk["tt2"][i]], writes=[t_qrot[cc][th]])
        for bi, (srcs, nm_) in enumerate(blocks):
            buf, tw = load_wblock(C, srcs)
            for m in range(nm_):
                for th in range(2):
                    pb, tp = npb()
                    for kc in range(NCH):
                        k.op("pe", lambda e: e.matmul(pb[:], lhsT=buf[:, kc, m * 128:(m + 1) * 128], rhs=C.hT[:, kc, C.HOFF + th * 512:C.HOFF + (th + 1) * 512],
                                                      start=(kc == 0), stop=(kc == NCH - 1)),
                             reads=[tw, C.t_h[kc][1 + th]], writes=[tp], inc=(kc == NCH - 1))
                    q_evac(bi, m, th, pb, tp)
        A.load_kv(g, kvA, t_kvA, Vs, t_Vs, Kw, t_Kw, Vw, t_Vw)
        for j in range(DBG['jlim']):
            th = j // 4
            qs = slice(j * 128, (j + 1) * 128)
            if g == 0 or True:
                pass
            k.dma("sp", wj, causal[j], writes=[t_wj])
            k.dma("sp", cval, cvalid[j].rearrange("p (nt t) -> p nt t", nt=2), writes=[t_cval])
            so = (g * 8 + j) % 2
            ot = otok[so]
            otv = ot.rearrange("p (i two d) -> p i two d", two=2, d=64)
            gv = gates[:, j, :].rearrange("p (hh two i) -> p hh two i", two=2, i=3)
            pU, tpU = C.pb[5], C.t_pb[5]
            Ecs = {}
            for par in (range(2) if DBG['cmp'] else ()):
                ps_ = slice(par * 64, (par + 1) * 64)
                for nt in range(2):
                    pb, tp = npb()
                    k.op("pe", lambda e: e.matmul(pb[:].rearrange("p (h q) -> p h q", h=4), lhsT=kcT_all[ps_, g, nt * 128:(nt + 1) * 128],
                                                  rhs=q_raw[ps_, :, qs], start=True, stop=True),
                         reads=[t_cmpkv] + [t_qraw[c_][th] for c_ in range(4)], writes=[tp])
                    ei = C.Ei
                    C.Ei = (ei + 1) % 4
                    E, tE = C.E[ei], C.t_E[ei]
                    k.op("act", lambda e: e.activation(out=E[:], in_=pb[:], func=AF.Exp, scale=SCALE), reads=[tp], writes=[tE])
                    k.op("dve", lambda e: e.tensor_tensor(out=E[:].rearrange("p (h q) -> p h q", h=4), in0=E[:].rearrange("p (h q) -> p h q", h=4),
                                                          in1=cval[:, nt, :].unsqueeze(1).to_broadcast([128, 4, 128]), op=ALU.mult),
                         reads=[tE, t_cval], writes=[tE])
                    Ecs[(par, nt)] = (E, tE)
                po, tpo = npb()
                first = True
                for hh in range(4):
                    for nt in range(2):
                        E, tE = Ecs[(par, nt)]
                        k.op("pe", lambda e: e.matmul(po[:, hh * 65:(hh + 1) * 65], lhsT=E[:, hh * 128:(hh + 1) * 128], rhs=vc_all[:, g, nt, :],
                                                      start=first, stop=(nt == 1), skip_group_check=True),
                             reads=[tE, t_cmpkv], writes=[tpo], inc=(hh == 3 and nt == 1))
                        first = False
                for hh in range(4):
                    for nt in range(2):
                        E, tE = Ecs[(par, nt)]
                        col = (par * 4 + hh) * 64
                        k.op("pe", lambda e: e.matmul(pU[:, col:col + 64], lhsT=E[:, hh * 128:(hh + 1) * 128], rhs=ovl_sb[:, nt, :],
                                                      start=(par == 0 and hh == 0 and nt == 0), stop=(nt == 1), skip_group_check=True),
                             reads=[tE, t_nc], writes=[tpU], inc=(hh == 3 and nt == 1))
                pov = po[:, 0:260].rearrange("p (h d) -> p h d", d=65)
                rc = rcs[:, par * 4:par * 4 + 4]
                k.op("dve", lambda e: e.tensor_scalar(out=rc, in0=pov[:, :, 64], scalar1=1e-30, scalar2=None, op0=ALU.max), reads=[tpo], writes=[t_fac])
                k.op("dve", lambda e: e.reciprocal(out=rc, in_=rc), reads=[t_fac], writes=[t_fac])
                k.op("dve", lambda e: e.tensor_tensor(out=fac, in0=rc, in1=gv[:, 4 * g:4 * g + 4, par, 0], op=ALU.mult), reads=[t_fac, t_gates], writes=[t_fac])
                k.op("dve", lambda e: e.tensor_tensor(out=C.accP[par], in0=pov[:, :, 0:64], in1=fac.unsqueeze(2).to_broadcast([128, 4, 64]), op=ALU.mult),
                     reads=[tpo, t_fac], writes=[C.t_accP[par]])
            for h8 in (range(8) if DBG['cmp'] else ()):
                if h8 == 0:
                    k.op("dve", lambda e: e.tensor_scalar(out=impt, in0=pU[:, 0:64], scalar1=rcs[:, 0:1], scalar2=None, op0=ALU.mult), reads=[tpU, t_fac], writes=[t_sc])
                else:
                    k.op("dve", lambda e: e.scalar_tensor_tensor(out=impt, in0=pU[:, h8 * 64:(h8 + 1) * 64], scalar=rcs[:, h8:h8 + 1], in1=impt, op0=ALU.mult, op1=ALU.add),
                         reads=[tpU, t_fac, t_sc], writes=[t_sc])
            if DBG['cmp']:
              k.op("dve", lambda e: e.tensor_tensor(out=score, in0=impt, in1=sbias[:, j, :], op=ALU.add), reads=[t_sc, t_sb], writes=[t_sc])
            k.op("dve", lambda e: e.max(out=mx8, in_=score), reads=[t_sc], writes=[t_sc])
            k.op("dve", lambda e: e.match_replace(out=score2, in_to_replace=mx8, in_values=score, imm_value=-3e38), reads=[t_sc], writes=[t_sc])
            k.op("dve", lambda e: e.max(out=mx8, in_=score2), reads=[t_sc], writes=[t_sc])
            k.op("dve", lambda e: e.tensor_scalar(out=selm, in0=score, scalar1=mx8[:, 7:8], scalar2=None, op0=ALU.is_ge), reads=[t_sc], writes=[t_sc])
            k.op("dve", lambda e: e.tensor_tensor(out=selc.rearrange("p (s q) -> p s q", s=64), in0=wj.rearrange("p (s q) -> p s q", s=64),
                                                  in1=selm.unsqueeze(2).to_broadcast([128, 64, 64]), op=ALU.mult),
                 reads=[t_sc, t_wj], writes=[t_selc])
            pow_ = [(C.pb[6], C.t_pb[6]), (C.pb[7], C.t_pb[7])]
            pendw = []

            def wstage1(i5):
                kt = j + i5
                banks = [npb(), npb()]
                outs = []
                for par in range(2):
                    ps_ = slice(par * 64, (par + 1) * 64)
                    pb, tp = banks[par]
                    k.op("pe", lambda e: e.matmul(pb[:].rearrange("p (h q) -> p h q", h=4), lhsT=Kw[ps_, kt * 128:(kt + 1) * 128],
                                                  rhs=q_rot[ps_, :, qs], start=True, stop=True),
                         reads=[t_Kw] + [t_qrot[c_][th] for c_ in range(4)], writes=[tp])
                for par in range(2):
                    pb, tp = banks[par]
                    E, tE = nextE()
                    k.op("act", lambda e: e.activation(out=E[:], in_=pb[:], func=AF.Exp, scale=SCALE), reads=[tp], writes=[tE])
                    mask = C.mprev if i5 == 0 else (C.mdiag if i5 == 4 else None)
                    if mask is not None:
                        k.op("dve", lambda e: e.tensor_tensor(out=E[:].rearrange("p (h q) -> p h q", h=4), in0=E[:].rearrange("p (h q) -> p h q", h=4),
                                                              in1=mask.unsqueeze(1).to_broadcast([128, 4, 128]), op=ALU.mult),
                             reads=[tE, C.t_const], writes=[tE])
                    outs.append((E, tE, par, kt, i5))
                return outs

            def wstage2(outs):
                for (E, tE, par, kt, i5) in outs:
                    po, tpo = pow_[par]
                    k.op("pe", lambda e: e.matmul(po[0:65, :], lhsT=Vw[:, kt, :], rhs=E[:], start=(i5 == 0), stop=(i5 == 4)),
                         reads=[tE, t_Vw], writes=[tpo])
            for n_ in range(5 + 1):
                if n_ < 5:
                    pendw.append(wstage1(n_))
                if n_ >= 1:
                    wstage2(pendw[n_ - 1])
            for par in range(2):
                po, tpo = back_to_token_major(pow_[par][0], pow_[par][1], par)
                pov = po[:, 0:260].rearrange("p (h d) -> p h d", d=65)
                k.op("dve", lambda e: e.reciprocal(out=fac, in_=pov[:, :, 64]), reads=[tpo], writes=[t_fac])
                k.op("dve", lambda e: e.tensor_tensor(out=fac, in0=fac, in1=gv[:, 4 * g:4 * g + 4, par, 2], op=ALU.mult), reads=[t_fac, t_gates], writes=[t_fac])
                k.op("dve", lambda e: e.tensor_tensor(out=tacc, in0=pov[:, :, 0:64], in1=fac.unsqueeze(2).to_broadcast([128, 4, 64]), op=ALU.mult),
                     reads=[tpo, t_fac], writes=[t_acc])
                k.op("dve", lambda e: e.tensor_tensor(out=C.accP[par], in0=C.accP[par], in1=tacc, op=ALU.add), reads=[t_acc, C.t_accP[par]], writes=[C.t_accP[par]])
            posel = [C.pb[6], C.pb[7]]
            tposel = [C.t_pb[6], C.t_pb[7]]
            firsts = [True, True]
            LOOK = 2
            kg_max = (24 + j) // 4
            last_kt = kg_max * 4 + 3
            blocks = [(kg, kk) for kg in range(kg_max + 1) for kk in range(4)]
            pend = []
            pm_state = {}

            def stage1(kg, kk):
                if kk == 0:
                    bi_ = 4 + (kg % 2)
                    pm, tpm = C.pb[bi_], C.t_pb[bi_]
                    pmv_ = pm[:].bitcast(BF16)
                    for k4 in range(4):
                        kt_ = kg * 4 + k4
                        k.op("pe", lambda e: e.transpose(pmv_[:, k4 * 128:(k4 + 1) * 128], selc[:, kt_ * 128:(kt_ + 1) * 128], C.ident_bf[:]),
                             reads=[t_selc, C.t_const], writes=[tpm], inc=(k4 == 3))
                    pm_state[kg] = (pmv_, tpm)
                pmv, tpm = pm_state[kg]
                kt = kg * 4 + kk
                outs = []
                banks = [npb(), npb()]
                for par in range(2):
                    ps_ = slice(par * 64, (par + 1) * 64)
                    pb, tp = banks[par]
                    k.op("pe", lambda e: e.matmul(pb[:].rearrange("p (h q) -> p h q", h=4), lhsT=kvA[ps_, kt * 128:(kt + 1) * 128],
                                                  rhs=q_rot[ps_, :, qs], start=True, stop=True),
                         reads=[t_kvA] + [t_qrot[c_][th] for c_ in range(4)], writes=[tp])
                for par in range(2):
                    pb, tp = banks[par]
                    E, tE = nextE()
                    k.op("act", lambda e: e.activation(out=E[:], in_=pb[:], func=AF.Exp, scale=SCALE), reads=[tp], writes=[tE])
                    k.op("dve", lambda e: e.tensor_tensor(out=E[:].rearrange("p (h q) -> p h q", h=4), in0=E[:].rearrange("p (h q) -> p h q", h=4),
                                                          in1=pmv[:, kk * 128:(kk + 1) * 128].unsqueeze(1).to_broadcast([128, 4, 128]), op=ALU.mult),
                         reads=[tE, tpm], writes=[tE])
                    outs.append((E, tE, par, kt))
                return outs

            def stage2(outs):
                for (E, tE, par, kt) in outs:
                    k.op("pe", lambda e: e.matmul(posel[par][0:65, :], lhsT=Vs[:, kt, :], rhs=E[:], start=firsts[par], stop=(kt == last_kt)),
                         reads=[tE, t_Vs], writes=[tposel[par]])
                    firsts[par] = False
            for n_ in range(len(blocks) + LOOK):
                if n_ < len(blocks):
                    pend.append(stage1(*blocks[n_]))
                if n_ >= LOOK:
                    stage2(pend[n_ - LOOK])
            for par in range(2):
                pt, tpt = back_to_token_major(posel[par], tposel[par], par)
                pov = pt[:, 0:260].rearrange("p (h d) -> p h d", d=65)
                k.op("dve", lambda e: e.reciprocal(out=fac, in_=pov[:, :, 64]), reads=[tpt], writes=[t_fac])
                k.op("dve", lambda e: e.tensor_tensor(out=fac, in0=fac, in1=gv[:, 4 * g:4 * g + 4, par, 1], op=ALU.mult), reads=[t_fac, t_gates], writes=[t_fac])
                k.op("dve", lambda e: e.tensor_tensor(out=tacc, in0=pov[:, :, 0:64], in1=fac.unsqueeze(2).to_broadcast([128, 4, 64]), op=ALU.mult),
                     reads=[tpt, t_fac], writes=[t_acc])
                k.op("dve", lambda e: e.tensor_tensor(out=otv[:, :, par, :], in0=C.accP[par], in1=tacc, op=ALU.add),
                     reads=[t_acc, C.t_accP[par]], writes=[t_otok[so]])
            pb, tp = npb()
            pbv = pb[:].bitcast(BF16)
            for jj in range(4):
                k.op("pe", lambda e: e.transpose(pbv[:, jj * 128:(jj + 1) * 128], ot[:, jj * 128:(jj + 1) * 128], C.ident_bf[:]),
                     reads=[t_otok[so], C.t_const], writes=[tp], inc=(jj == 3))
            k.op("act", lambda e: e.copy(out=C.big[:, 4 * g:4 * g + 4, qs], in_=pbv[:, 0:512].rearrange("p (c q) -> p c q", c=4)),
                 reads=[tp], writes=[C.t_big[4 * g + i][th] for i in range(4)])
    if DBG.get('dump'):
        dbg_o = nc.dram_tensor('dbg_oT', [128, NCH * NT], BF16, kind='ExternalOutput').ap()
        k.dma('sp', dbg_o, C.big[:].rearrange('p c t -> p (c t)'), reads=[C.t_big[c_][th_] for c_ in range(NCH) for th_ in range(2)], writes=[C.t_out])
    k.dma("sp", C.xT[:].rearrange("p c t -> p (c t)"), xspill, reads=[t_spill], writes=xs_toks + allx + [t_spill])
    C.pbi = 0
    blocks = [([(w_o[:, b * WB:(b + 1) * WB], 0)], 2) for b in range(8)]

    def o_evac(bi, m, th, pb, tp):
        c = bi * 2 + m
        k.op("dve", lambda e: e.scalar_tensor_tensor(out=C.xT[:, c, tsl(th)], in0=pb[:], scalar=v[:, V_GP_T + c:V_GP_T + c + 1],
                                                     in1=C.xT[:, c, tsl(th)], op0=ALU.mult, op1=ALU.add),
             reads=[tp, C.t_vec, C.t_x[c][th]], writes=[C.t_x[c][th]])
    linear_fm(C, blocks, lambda kc, th: C.big[:, kc, tsl(th)], lambda th: (lambda kc: [C.t_big[kc][th]]), 2, o_evac)
    layer_norm_mod(C, C.vec, V_LN_T_G, V_LN_T_B, V_A_C, V_B_C, C.tmp, C.t_tmp, C.stat, C.t_stat)
    if DBG['moe']:
        moe_layer(C, 1, C.vec, {"sc1": V_SC1_C, "sh": V_SH_C, "gp": V_GP_C, "w1": w1, "w3": w3, "w2": w2})
    layer_norm_mod(C, C.vec, V_LN_C_G, V_LN_C_B, None, None, C.tmp, C.t_tmp, C.stat, C.t_stat)
    store_x(C, xout)


def build_layer1b():
    nc = bass.Bass("TRN2", target_bir_lowering=False)

    def din(name, shape, dt=F32):
        return nc.dram_tensor(name, list(shape), dt, kind="ExternalInput").ap()
    xin = din("xin", [NT, D])
    pos = din("pos", [1, NT], I32)
    vec_d = din("vec", [128, NV])
    ident = din("ident_f", [128, 128])
    masks = din("masks", [128, 4, 128])
    rows = din("rows", [128, 308])
    w_in = din("w_in", [D, 3680])
    w_o = din("w_o", [D, D])
    wr_d = din("wr", [D, 20])
    w1 = din("w1", [16, D, 512])
    w3 = din("w3", [16, D, 512])
    w2 = din("w2", [16, 512, D])
    kc_full = din("kc_full", [4, 128, 16 * 255], BF16)
    vc_full = din("vc_full", [4, 128, 16 * 255], BF16)
    ks_full = din("ks_full", [4, 128, 4096], BF16)
    vs_aug = din("vs_aug", [4, 128, 32 * 65], BF16)
    kw_core = din("kw_core", [4, 128, NT + HALO1], BF16)
    vw_aug = din("vw_aug", [4, 128, 12 * 65], BF16)
    phi_k1 = din("phi_k1", [D, 256])
    phi_v1 = din("phi_v1", [D, 256])
    phi2 = din("phi2", [128, 2, 192])
    peT = din("peT", [128, 32])
    causal = din("causal", [8, 128, 4096], BF16)
    cvalid = din("cvalid", [8, 128, 256], BF16)
    sbias_d = din("sbias", [128, 8 * 64])
    ovl = din("ovl", [128, 2, 64])
    xspill = nc.dram_tensor("xspill", [128, NCH * NT], F32, kind="Internal").ap()
    xout = nc.dram_tensor("xout", [NT, D], F32, kind="ExternalOutput").ap()
    with ExitStack() as st:
        k = K(nc, st)
        C = setup_ctx(k, {"ident_f": ident}, hoff=0)
        setup_region(C)
        C.t_out = Tok()
        C.mk = k.sb("mk", [128, 4, 128], BF16)
        k.dma("pool", C.mk[:], masks, writes=[C.t_const])
        C.mdiag, C.mprev, C.rperm = C.mk[:, 0, :], C.mk[:, 1, :], C.mk[:, 3, :]
        C.rowsb = k.sb("rowsb", [128, 308], F32)
        C.brow = C.rowsb[:, 288:308]
        k.dma("sp", C.rowsb[:], rows, writes=[C.t_wr])
        k.dma("sp", C.wr[:], wr_d.rearrange("(kc p) n -> p kc n", p=128), writes=[C.t_wr])
        k.dma("sp", C.vec[:], vec_d, writes=[C.t_vec])
        derive_vec(C)
        C.zero1 = k.sb("zero1", [128, 1], F32)
        k.op("dve", lambda e: e.memset(C.zero1[:], 0.0), writes=[C.t_vec])
        phi2_sb = k.sb("phi2_sb", [128, 2, 192], BF16)
        peT_sb = k.sb("peT_sb", [128, 32], BF16)
        ovl_sb = k.sb("ovl_sb", [128, 2, 64], BF16)
        t_nc = Tok()
        k.dma("pool", phi2_sb[:], phi2, writes=[t_nc])
        k.dma("pool", peT_sb[:], peT, writes=[t_nc])
        k.dma("pool", ovl_sb[:], ovl, writes=[t_nc])
        kcT_all = k.sb("kcT_all", [128, 4, 256], BF16)
        vc_all = k.sb("vc_all", [128, 4, 2, 65], BF16)
        t_cmpkv = Tok()
        v = C.vec
        load_x(C, xin, False)
        A = Ctx()
        A.pos, A.w_in, A.w_o, A.w1, A.w3, A.w2 = pos, w_in, w_o, w1, w3, w2
        A.phi_k1, A.phi_v1, A.causal, A.cvalid, A.sbias_d, A.xspill, A.xout = phi_k1, phi_v1, causal, cvalid, sbias_d, xspill, xout
        A.phi2_sb, A.peT_sb, A.ovl_sb, A.t_nc, A.kcT_all, A.vc_all, A.t_cmpkv = phi2_sb, peT_sb, ovl_sb, t_nc, kcT_all, vc_all, t_cmpkv

        def load_cmp_src(g, kv, kvA, t_kvA, selc, t_selc):
            k.dma("sp", kvA[:, 0:16 * 255], (kc_full, vc_full)[kv][g], writes=[t_kvA])

        def load_kv(g, kvA, t_kvA, Vs, t_Vs, Kw, t_Kw, Vw, t_Vw):
            k.dma("sp", kvA, ks_full[g], writes=[t_kvA])
            k.dma("sp", Vs.rearrange("p t d -> p (t d)"), vs_aug[g], writes=[t_Vs])
            k.dma("sp", Kw, kw_core[g], writes=[t_Kw])
            k.dma("sp", Vw.rearrange("p t d -> p (t d)"), vw_aug[g], writes=[t_Vw])
        A.load_cmp_src, A.load_kv = load_cmp_src, load_kv
        A.cmp_prepped = True
        nsa_core(nc, k, C, A)
        k.finish([C.t_out])
        print("layer1b program: %d instructions" % k.ninst, k.cnt)
    return nc


BF = ml_dtypes.bfloat16


def layer1b_consts(r):
    tg = (1024 * r + np.arange(1024)).reshape(8, 128)
    keys = np.arange(4096)
    causal = (keys[None, None, :] <= tg[:, :, None]).astype(BF)
    n = np.arange(256)
    cend = 16 * n + 31
    valid = (cend[None, :, None] <= tg[:, None, :])
    valid[:, 255, :] = False
    cvalid = np.ascontiguousarray(valid.reshape(8, 2, 128, 128).transpose(0, 2, 1, 3).reshape(8, 128, 256)).astype(BF)
    s = np.arange(64)
    cur = tg // 64
    caus = (s[None, None, :] * 64 <= tg[:, :, None])
    forced = (s[None, None, :] == 0) | (s[None, None, :] == cur[:, :, None]) | (s[None, None, :] == cur[:, :, None] - 1)
    sb = np.where(caus, np.where(forced, 1e4, 0.0), -1e30).astype(np.float32)
    sbias = np.ascontiguousarray(sb.transpose(1, 0, 2).reshape(128, 512))
    c0 = n[:, None] * 16
    s0 = s[None, :] * 64
    ov = np.clip(np.minimum(c0 + 32, s0 + 64) - np.maximum(c0, s0), 0, None).astype(np.float32) / 32.0
    ov[255] = 0.0
    ovl = np.ascontiguousarray(ov.reshape(2, 128, 64).transpose(1, 0, 2))
    return causal, cvalid, sbias, ovl


def layer1b_inputs(inp, modT, x1_core, core, kvb):
    b, r = core // 4, core % 4
    i = 1
    t0 = r * NT
    pos = np.ascontiguousarray(inp["positions"][b][t0:t0 + NT][None, :].astype(np.int32))
    vec = np.zeros((128, NV), np.float32)
    vec[:, 0:96] = modT
    vec[:, V_LN_T_G:V_LN_T_G + 16] = fm(inp["ln_t_g"][i])
    vec[:, V_LN_T_B:V_LN_T_B + 16] = fm(inp["ln_t_b"][i])
    vec[:, V_LN_C_G:V_LN_C_G + 16] = fm(inp["ln_c_g"][i])
    vec[:, V_LN_C_B:V_LN_C_B + 16] = fm(inp["ln_c_b"][i])
    invf, m16, om16, sgn = rope_consts()
    vec[:, V_INVF], vec[:, V_M16], vec[:, V_OM16], vec[:, V_SGN] = invf, m16, om16, sgn
    rows = np.zeros((128, 308), np.float32)
    rows[:, 288:292] = inp["moe_b_group"][i][None, :]
    rows[:, 292:308] = inp["moe_b_router"][i][None, :]
    wr = np.ascontiguousarray(np.concatenate([inp["moe_w_group"][i], inp["moe_w_router"][i]], axis=1))
    causal, cvalid, sbias, ovl = layer1b_consts(r)
    kw_full = kvb["kw_full"]
    kw_core = np.zeros((4, 128, NT + HALO1), BF)
    lo = t0 - HALO1
    if lo >= 0:
        kw_core[:] = kw_full[:, :, lo:t0 + NT]
    else:
        kw_core[:, :, -lo:] = kw_full[:, :, 0:t0 + NT]
    vw_full = kvb["vw_aug_full"]
    vw = np.zeros((4, 128, 12, 65), BF)
    tlo = 8 * r - 4
    if tlo >= 0:
        vw[:] = vw_full[:, :, tlo:tlo + 12, :]
    else:
        vw[:, :, -tlo:, :] = vw_full[:, :, 0:tlo + 12, :]
    k2, v2 = inp["nsa_phi_k2"][0], inp["nsa_phi_v2"][0]
    phi2 = np.zeros((128, 2, 192), np.float32)
    for hc in range(2):
        phi2[:, hc, 0:64] = k2[hc * 128:(hc + 1) * 128]
        phi2[:, hc, 64:128] = k2[hc * 128:(hc + 1) * 128]
        phi2[:, hc, 128:192] = v2[hc * 128:(hc + 1) * 128]
    peT = np.zeros((128, 32), np.float32)
    pk, pv = inp["nsa_pe_k"][0], inp["nsa_pe_v"][0]
    peT[0:64, 0:16] = pk[0::2].T
    peT[64:128, 0:16] = pk[1::2].T
    peT[0:64, 16:32] = pv[0::2].T
    peT[64:128, 16:32] = pv[1::2].T
    return {"xin": np.ascontiguousarray(x1_core), "pos": pos, "vec": vec, "ident_f": np.eye(128, dtype=np.float32),
            "masks": const_masks(False), "rows": rows, "w_in": inp["nsa_w_in"][0], "w_o": inp["nsa_w_o"][0], "wr": wr,
            "w1": inp["moe_w1"][i], "w3": inp["moe_w3"][i], "w2": inp["moe_w2"][i],
            "kc_full": kvb["kc_full"], "vc_full": kvb["vc_full"], "ks_full": kvb["ks_full"],
            "vs_aug": np.ascontiguousarray(kvb["vs_aug_full"].reshape(4, 128, 32 * 65)),
            "kw_core": kw_core, "vw_aug": np.ascontiguousarray(vw.reshape(4, 128, 12 * 65)),
            "phi_k1": inp["nsa_phi_k1"][0], "phi_v1": inp["nsa_phi_v1"][0], "phi2": phi2, "peT": peT,
            "causal": causal, "cvalid": cvalid, "sbias": sbias, "ovl": ovl}


def assemble_kv(resA, b):
    cores = [resA[b * 4 + r] for r in range(4)]

    def catT(nm):
        full = np.concatenate([np.asarray(c[nm]) for c in cores], axis=2)
        return np.ascontiguousarray(full.transpose(1, 0, 2))

    def vaug(nm):
        full = np.concatenate([np.asarray(c[nm]) for c in cores], axis=1)
        full = full.reshape(128, 32, 4, 64)
        aug = np.ones((128, 32, 4, 65), BF)
        aug[:, :, :, 0:64] = full
        return np.ascontiguousarray(aug.transpose(2, 0, 1, 3))
    def perm(a):
        out = np.zeros((4, 128, 16, 255), BF)
        n16 = 16 * np.arange(255)
        for jj in range(16):
            out[:, 0:64, jj, :] = a[:, 0:64, :][:, :, n16 + 2 * jj]
            out[:, 64:128, jj, :] = a[:, 64:128, :][:, :, n16 + 2 * jj + 1]
        return np.ascontiguousarray(out.reshape(4, 128, 16 * 255))
    return {"kc_full": perm(catT("kc_T")), "vc_full": perm(catT("vc_T")), "ks_full": catT("ks_T"), "kw_full": catT("kw_T"),
            "vs_aug_full": vaug("vs"), "vw_aug_full": vaug("vw")}


def build_mod():
    nc = bass.Bass("TRN2", target_bir_lowering=False)
    cT = nc.dram_tensor("cT", [128, 16, 2], F32, kind="ExternalInput").ap()
    w = nc.dram_tensor("w", [2048, 3072], F32, kind="ExternalInput").ap()
    b = nc.dram_tensor("b", [1, 3072], F32, kind="ExternalInput").ap()
    out = nc.dram_tensor("out", [2, 3072], F32, kind="ExternalOutput").ap()
    with ExitStack() as st:
        k = K(nc, st)
        c_sb = k.sb("c_sb", [128, 16, 2], F32)
        t_c = Tok()
        ca = k.sb("ca", [128, 16, 2], F32)
        t_ca = Tok()
        wb = [k.sb("wb%d" % i, [128, 16, 512], F32) for i in range(2)]
        t_wb = [Tok(), Tok()]
        bb = k.sb("bb", [1, 3072], F32)
        t_bb = Tok()
        ones = k.sb("ones", [1, 2], F32)
        t_ones = Tok()
        res = k.sb("res", [2, 3072], F32)
        t_res = Tok()
        pb = [k.ps("pb%d" % i, [128, 512], F32) for i in range(2)]
        t_pb = [Tok(), Tok()]
        k.dma("sp", c_sb[:], cT, writes=[t_c])
        k.dma("sp", bb[:], b, writes=[t_bb])
        k.op("dve", lambda e: e.memset(ones[:], 1.0), writes=[t_ones])
        k.op("act", lambda e: e.activation(out=ca[:], in_=c_sb[:], func=AF.Silu), reads=[t_c], writes=[t_ca])
        wv = w.rearrange("(kc p) n -> p kc n", p=128)
        for j in range(6):
            i = j % 2
            k.dma("sp", wb[i][:], wv[:, :, j * 512:(j + 1) * 512], writes=[t_wb[i]])
            for kc in range(16):
                k.op("pe", lambda e: e.matmul(pb[i][0:2, :], lhsT=ca[:, kc, :], rhs=wb[i][:, kc, :], start=(kc == 0), stop=False),
                     reads=[t_ca, t_wb[i]], writes=[t_pb[i]])
            k.op("pe", lambda e: e.matmul(pb[i][0:2, :], lhsT=ones[:], rhs=bb[:, j * 512:(j + 1) * 512], start=False, stop=True),
                 reads=[t_ones, t_bb], writes=[t_pb[i]])
            k.op("dve", lambda e: e.tensor_copy(out=res[:, j * 512:(j + 1) * 512], in_=pb[i][0:2, :]), reads=[t_pb[i]], writes=[t_res])
        k.dma("sp", out, res[:], reads=[t_res], writes=[t_res])
        k.finish([t_res])
    return nc


def kernel_unfused(**inp):
    inp = {kk: np.asarray(vv) for kk, vv in inp.items()}
    cores = list(range(8))
    c = inp["c"].astype(np.float32)
    wcat = np.concatenate([inp["w_ada"][0], inp["w_ada"][1]], axis=1)
    bcat = np.concatenate([inp["b_ada"][0], inp["b_ada"][1]], axis=0)
    cT = np.ascontiguousarray(c.T.reshape(16, 128, 2).transpose(1, 0, 2))
    in_maps = [{"cT": cT, "w": np.ascontiguousarray(wcat[:, i * 3072:(i + 1) * 3072]),
                "b": np.ascontiguousarray(bcat[None, i * 3072:(i + 1) * 3072])} for i in cores]
    res = run_bass_kernel_spmd(build_mod(), in_maps, core_ids=cores)
    mod = np.concatenate([np.asarray(r["out"]) for r in res.results], axis=1)

    def modT(b, layer):
        return np.ascontiguousarray(mod[b, layer * 12288:(layer + 1) * 12288].reshape(96, 128).T)
    in_maps = [layer0_inputs(inp, modT(cc // 4, 0), cc) for cc in cores]
    res = run_bass_kernel_spmd(build_layer0(), in_maps, core_ids=cores)
    x1 = [np.asarray(r["xout"]) for r in res.results]
    in_maps = [layer1a_inputs(inp, modT(cc // 4, 1), x1[cc], cc) for cc in cores]
    res = run_bass_kernel_spmd(build_layer1a(), in_maps, core_ids=cores)
    resA = [{kk: np.asarray(vv) for kk, vv in r.items()} for r in res.results]
    kvbs = [assemble_kv(resA, b) for b in range(2)]
    in_maps = [layer1b_inputs(inp, modT(cc // 4, 1), x1[cc], cc, kvbs[cc // 4]) for cc in cores]
    res = run_bass_kernel_spmd(build_layer1b(), in_maps, core_ids=cores)
    x2 = np.stack([np.asarray(r["xout"]) for r in res.results])
    return np.ascontiguousarray(x2.reshape(2, 4096, 2048)).astype(np.float32)


GW = 16384 + 2 * 2080
GROUPS = [[0, 1, 2, 3], [4, 5, 6, 7]]


def emit_collective(k, nc, kind, src, dst, reads, writes, name):
    k._deps("pool", reads, writes)
    ins = nc.gpsimd.collective_compute(kind, ALU.bypass, replica_groups=GROUPS, ins=[src], outs=[dst])
    sem = k.stack.enter_context(nc.semaphore(name))
    ins.then_inc(sem)
    k._mark(sem, 1, reads, writes)
    k.ninst += 1


def build_fused():
    nc = bass.Bass("TRN2", target_bir_lowering=False)

    def din(name, shape, dt=F32):
        return nc.dram_tensor(name, list(shape), dt, kind="ExternalInput").ap()
    xin = din("xin", [NT + HALO0, D])
    pos = din("pos", [1, NT + HALO0], I32)
    vec0_d = din("vec0", [128, NV])
    vec1_d = din("vec1", [128, NV])
    ident = din("ident_f", [128, 128])
    masks = din("masks", [128, 4, 128])
    rows0 = din("rows0", [128, 308])
    rows1 = din("rows1", [128, 308])
    cT = din("cT", [128, 16, 2])
    wada = din("wada", [D, 6144])
    bada = din("bada", [1, 6144])
    w_qkv = din("w_qkv", [D, 2560])
    w_o0 = din("w_o0", [D, D])
    wr0 = din("wr0", [D, 20])
    wr1 = din("wr1", [D, 20])
    w1 = din("w1", [2, 16, D, 512])
    w3 = din("w3", [2, 16, D, 512])
    w2 = din("w2", [2, 16, 512, D])
    w_in = din("w_in", [D, 3680])
    w_o1 = din("w_o1", [D, D])
    phi_k1 = din("phi_k1", [D, 256])
    phi_v1 = din("phi_v1", [D, 256])
    phi2 = din("phi2", [128, 2, 192])
    peT = din("peT", [128, 32])
    causal = din("causal", [8, 128, 4096], BF16)
    cvalid = din("cvalid", [8, 128, 256], BF16)
    sbias_d = din("sbias", [128, 8 * 64])
    ovl = din("ovl", [128, 2, 64])
    oneh_d = din("oneh", [128, 8])
    xspill = nc.dram_tensor("xspill", [128, NCH * NT], F32).ap()
    mod_src = nc.dram_tensor("mod_src", [2, 6144], F32).ap()
    mod_dst = nc.dram_tensor("mod_dst", [8, 6144], F32).ap()
    kv_src = [nc.dram_tensor("kv_src%d" % i, [128, 2048 if i < 8 else 1040], BF16).ap() for i in range(12)]
    kv_dst = [nc.dram_tensor("kv_dst%d" % i, [512, 2048 if i < 8 else 1040], BF16).ap() for i in range(12)]
    xout = nc.dram_tensor("xout", [NT, D], F32, kind="ExternalOutput").ap()
    with ExitStack() as st:
        k = K(nc, st)
        C = setup_ctx(k, {"ident_f": ident})
        setup_region(C)
        C.t_out = Tok()
        vec0 = C.vec
        vec1 = k.sb("vec1", [128, NV], F32)
        C.mk = k.sb("mk", [128, 4, 128], BF16)
        k.dma("pool", C.mk[:], masks, writes=[C.t_const])
        C.mdiag, C.mprev, C.mprev0, C.rperm = C.mk[:, 0, :], C.mk[:, 1, :], C.mk[:, 2, :], C.mk[:, 3, :]
        C.rowsb = k.sb("rowsb", [128, 308], F32)
        C.bvrow = C.rowsb[:, 0:256]
        C.esink = k.sb("esink", [128, 32], F32)
        C.brow = C.rowsb[:, 288:308]
        k.dma("sp", C.rowsb[:], rows0, writes=[C.t_wr])
        k.op("act", lambda e: e.activation(out=C.esink[:], in_=C.rowsb[:, 256:288], func=AF.Exp), reads=[C.t_wr], writes=[C.t_wr])
        k.dma("sp", C.wr[:], wr0.rearrange("(kc p) n -> p kc n", p=128), writes=[C.t_wr])
        k.dma("sp", vec0[:], vec0_d, writes=[C.t_vec])
        k.dma("sp", vec1[:], vec1_d, writes=[C.t_vec])
        C.zero1 = k.sb("zero1", [128, 1], F32)
        k.op("dve", lambda e: e.memset(C.zero1[:], 0.0), writes=[C.t_vec])
        oneh = k.sb("oneh", [128, 8], F32)
        phi2_sb = k.sb("phi2_sb", [128, 2, 192], BF16)
        peT_sb = k.sb("peT_sb", [128, 32], F32)
        ovl_sb = k.sb("ovl_sb", [128, 2, 64], BF16)
        t_nc = Tok()
        k.dma("sp", oneh[:], oneh_d, writes=[t_nc])
        k.dma("pool", phi2_sb[:], phi2, writes=[t_nc])
        k.dma("sp", peT_sb[:], peT, writes=[t_nc])
        k.dma("pool", ovl_sb[:], ovl, writes=[t_nc])
        kcT_all = vc_all = t_cmpkv = None
        c_sb = k.sb("c_sb", [128, 16, 2], F32)
        t_c = Tok()
        k.dma("sp", c_sb[:], cT, writes=[t_c])
        k.op("act", lambda e: e.activation(out=c_sb[:], in_=c_sb[:], func=AF.Silu), reads=[t_c], writes=[t_c])
        wst = [C.big[:].rearrange("p c t -> p (c t)").bitcast(F32).rearrange("p (kc n) -> p kc n", kc=16),
               C.hT[:].rearrange("p c t -> p (c t)").bitcast(F32)[:, 0:8192].rearrange("p (kc n) -> p kc n", kc=16)]
        t_wst = [Tok(), Tok()]
        brow_m = C.stage[0][0:1, :]
        bsb = C.xT[0:1, 8:14, :].rearrange("p c t -> p (c t)")
        t_bsb = Tok()
        k.dma("sp", bsb, bada, writes=[t_bsb])
        ones2 = k.sb("ones2", [1, 2], F32)
        k.op("dve", lambda e: e.memset(ones2[:], 1.0), writes=[t_c])
        mres = C.xT[0:2, 0:6, :].rearrange("p c t -> p (c t)")
        t_mres = Tok()
        wv = wada.rearrange("(kc p) n -> p kc n", p=128)
        for jb in range(12):
            i = jb % 2
            k.dma("sp", wst[i], wv[:, :, jb * 512:(jb + 1) * 512], writes=[t_wst[i]])
            pb, tp = next_pb(C)
            for kc in range(16):
                k.op("pe", lambda e: e.matmul(pb[0:2, :], lhsT=c_sb[:, kc, :], rhs=wst[i][:, kc, :], start=(kc == 0), stop=False),
                     reads=[t_c, t_wst[i]], writes=[tp], inc=False)
            k.op("pe", lambda e: e.matmul(pb[0:2, :], lhsT=ones2[:], rhs=bsb[:, jb * 512:(jb + 1) * 512], start=False, stop=True),
                 reads=[t_c, t_bsb], writes=[tp])
            k.op("dve", lambda e: e.tensor_copy(out=mres[:, jb * 512:(jb + 1) * 512], in_=pb[0:2, :]), reads=[tp], writes=[t_mres])
        t_msrc, t_mdst = Tok(), Tok()
        k.dma("sp", mod_src, mres, reads=[t_mres], writes=[t_msrc])
        emit_collective(k, nc, "AllGather", mod_src, mod_dst, [t_msrc], [t_mdst], "cc_mod")
        mt = [C.stage[0][:, 0:128], C.stage[0][0:64, 128:256]]
        for r_ in range(4):
            rowsrc = mod_dst[2 * r_:2 * r_ + 1, :].rearrange("o (j p) -> (o j) p", p=128)
            lo = 48 * r_
            for (a0, a1) in ((lo, min(lo + 48, 128)), (max(lo, 128), lo + 48)):
                if a1 <= a0:
                    continue
                if a0 < 128:
                    k.dma("sp", mt[0][a0:a1, :], rowsrc[a0 - lo:a1 - lo, :], reads=[t_mdst], writes=[C.t_stage[0]])
                else:
                    k.dma("sp", mt[1][a0 - 128:a1 - 128, :], rowsrc[a0 - lo:a1 - lo, :], reads=[t_mdst], writes=[C.t_stage[0]])
        pb, tp = next_pb(C)
        k.op("pe", lambda e: e.transpose(pb[:, 0:128], mt[0], C.ident_f[:]), reads=[C.t_stage[0], C.t_const], writes=[tp])
        k.op("pe", lambda e: e.transpose(pb[:, 128:192], mt[1], C.ident_f[0:64, 0:64]), reads=[C.t_stage[0], C.t_const], writes=[tp])
        k.op("dve", lambda e: e.tensor_copy(out=vec0[:, 0:96], in_=pb[:, 0:96]), reads=[tp, C.t_vec], writes=[C.t_vec])
        k.op("dve", lambda e: e.tensor_copy(out=vec1[:, 0:96], in_=pb[:, 96:192]), reads=[tp, C.t_vec], writes=[C.t_vec])
        for c_ in range(NCH):
            for t_ in C.t_x[c_]:
                t_.w = t_msrc.w
        last_pe = (k.sem["pe"], k.cnt["pe"])
        for c_ in range(NCH):
            for t_ in C.t_big[c_] + C.t_h[c_]:
                t_.w = last_pe
        for t_ in C.t_stage + C.t_h32 + C.t_sa + C.t_tmp + C.t_stat + [tt for l_ in C.t_cmb for tt in l_]:
            t_.w = (k.sem["dve"], k.cnt["dve"])
        derive_vec(C)
        C.vec = vec1
        derive_vec(C)
        C.vec = vec0
        tv = C.t_vec
        k.op("dve", lambda e: e.tensor_tensor(out=vec0[:, V_A_N:V_A_N + 16], in0=vec0[:, V_LN_C_G:V_LN_C_G + 16], in1=vec1[:, V_SC1_T:V_SC1_T + 16], op=ALU.mult), reads=[tv], writes=[tv])
        k.op("dve", lambda e: e.tensor_tensor(out=vec0[:, V_B_N:V_B_N + 16], in0=vec0[:, V_LN_C_B:V_LN_C_B + 16], in1=vec1[:, V_SC1_T:V_SC1_T + 16], op=ALU.mult), reads=[tv], writes=[tv])
        k.op("dve", lambda e: e.tensor_tensor(out=vec0[:, V_B_N:V_B_N + 16], in0=vec0[:, V_B_N:V_B_N + 16], in1=vec1[:, V_SH_T:V_SH_T + 16], op=ALU.add), reads=[tv], writes=[tv])
        load_x(C, xin, True)
        swa_layer(C, {"pos": pos, "w_qkv": w_qkv, "w_o": w_o0})
        layer_norm_mod(C, vec0, V_LN_T_G, V_LN_T_B, V_A_C, V_B_C, C.tmp, C.t_tmp, C.stat, C.t_stat)
        moe_layer(C, 0, vec0, {"sc1": V_SC1_C, "sh": V_SH_C, "gp": V_GP_C, "w1": w1[0], "w3": w3[0], "w2": w2[0]})
        layer_norm_mod(C, vec0, V_LN_C_G, V_LN_C_B, V_A_N, V_B_N, C.tmp, C.t_tmp, C.stat, C.t_stat)
        if DBG.get('fdump'):
            d1 = nc.dram_tensor('dbg_vec', [128, 2 * NV], F32, kind='ExternalOutput').ap()
            k.dma('sp', d1[:, 0:NV], vec0[:], reads=[C.t_vec], writes=[C.t_out])
            k.dma('sp', d1[:, NV:2 * NV], vec1[:], reads=[C.t_vec], writes=[C.t_out])
            d2 = nc.dram_tensor('dbg_x1', [128, NCH * NT], F32, kind='ExternalOutput').ap()
            k.dma('sp', d2, C.xT[:].rearrange('p c t -> p (c t)'), reads=[C.t_x[c_][th_] for c_ in range(NCH) for th_ in range(2)], writes=[C.t_out])
            d3 = nc.dram_tensor('dbg_h1', [128, NCH * (NT + HALO0)], BF16, kind='ExternalOutput').ap()
            k.dma('sp', d3, C.hT[:].rearrange('p c t -> p (c t)'), reads=[C.t_h[c_][th_] for c_ in range(NCH) for th_ in range(3)], writes=[C.t_out])
        C.vec = vec1
        v = vec1
        k.dma("sp", C.rowsb[:], rows1, writes=[C.t_wr])
        k.dma("sp", C.wr[:], wr1.rearrange("(kc p) n -> p kc n", p=128), writes=[C.t_wr])
        f32v, bfv = C.f32v, C.bfv
        pos1 = pos[:, HALO0:HALO0 + NT]
        wk = {"i": 0,
              "qb": [bfv(4736, 512), bfv(5760, 512)], "tq": [Tok(), Tok()],
              "t1": [f32v(6784, 512), f32v(8832, 512)], "tt1": [Tok(), Tok()],
              "t2": [f32v(10880, 512), f32v(12928, 512)], "tt2": [Tok(), Tok()]}
        TcosA = f32v(16384, NT)
        TsinA = f32v(16384 + 4096, NT)
        t_trigA = Tok()
        lnc_done = (k.sem["pool"], k.cnt["pool"])
        preA = [C.t_big[c__][th__] for c__ in range(6) for th__ in range(2)] + C.t_tmp + C.t_sa + C.t_stat + C.t_stage + C.t_h32 + [tt for l_ in C.t_cmb for tt in l_]
        rope_tables(C, pos1, NT, TcosA, TsinA, t_trigA, C.big[:, 0:2, :].rearrange("p a t -> p (a t)").bitcast(I32),
                    C.big[:, 2:4, :].rearrange("p a t -> p (a t)").bitcast(F32), C.big[:, 4:6, :].rearrange("p a t -> p (a t)").bitcast(F32), pre=preA)
        for c__ in range(6):
            for th__ in range(2):
                C.t_big[c__][th__].w = t_trigA.w
                C.t_big[c__][th__].r = {}
        stg = C.big[:, 0:16, :].rearrange("p (a g) t -> p a g t", g=4)
        t_stg = [[C.t_big[a * 4 + g][0] for g in range(4)] for a in range(4)]
        col0 = [2048, 2304, 2560, 3072]
        kblocks = []
        order = []
        for a in range(4):
            for b in range(2):
                srcs = []
                for m in range(2):
                    g = b * 2 + m
                    col = col0[a] + g * 64
                    srcs.append((w_in[:, col:col + 64], m * 128))
                    srcs.append((w_in[:, col:col + 64], m * 128 + 64))
                kblocks.append((srcs, 2))
                order.append((a, b))

        def evacA(bi, m, th, pb, tp):
            a, b = order[bi]
            g = b * 2 + m
            dst = stg[:, a, g, tsl(th)]
            toks = [C.t_big[a * 4 + g][th]]
            if a >= 2:
                cs = tsl(th)
                rope_evac(C, pb, tp, 512, C.zero1[:, 0:1], TcosA[:, cs], TsinA[:, cs], t_trigA, dst, toks[0], wk)
            else:
                k.op("act", lambda e: e.copy(out=dst, in_=pb[:]), reads=[tp], writes=toks)
        linear_fm(C, kblocks, lambda kc, th: C.hT[:, kc, HALO0 + th * 512:HALO0 + (th + 1) * 512],
                  lambda th: (lambda kc: [C.t_h[kc][1 + th]]), 2, evacA)
        t_kvsrc = [Tok() for _ in range(12)]
        t_kvdst = [Tok() for _ in range(12)]
        bigflat = C.big[:].rearrange("p c t -> p (c t)")
        for ci in range(8):
            k.dma("sp", kv_src[ci], bigflat[:, ci * 2048:(ci + 1) * 2048], reads=[C.t_big[c_][th_] for c_ in (2 * ci, 2 * ci + 1) for th_ in range(2)], writes=[t_kvsrc[ci]])
            emit_collective(k, nc, "AllGather", kv_src[ci], kv_dst[ci], [t_kvsrc[ci]], [t_kvdst[ci]], "cc_kv%d" % ci)
        vaug = bfv(0, 2 * 2080).rearrange("p (a g t d) -> p a g t d", a=2, g=4, t=8)
        t_vaug = Tok()
        t_vaug.w = (k.sem["dve"], k.cnt["dve"])
        k.op("pool", lambda e: e.memset(bfv(0, 2 * 2080), 1.0), writes=[t_vaug])
        for vi, col in enumerate((2816, 3328)):
            vbuf, tvb = load_wblock(C, [(w_in[:, col:col + 256], 0)])
            for t in range(8):
                pb, tp = next_pb(C)
                for kc in range(NCH):
                    k.op("pe", lambda e: e.matmul(pb[:, 0:256], lhsT=C.hT[:, kc, HALO0 + t * 128:HALO0 + (t + 1) * 128], rhs=vbuf[:, kc, :], start=(kc == 0), stop=(kc == NCH - 1)),
                         reads=[tvb, C.t_h[kc][1], C.t_h[kc][2]], writes=[tp], inc=(kc == NCH - 1))
                k.op("act", lambda e: e.copy(out=vaug[:, vi, :, t, 0:64], in_=pb[:, 0:256].rearrange("p (g d) -> p g d", g=4)), reads=[tp], writes=[t_vaug])
        for ci in range(8, 12):
            k.dma("sp", kv_src[ci], bfv(0, 2 * 2080)[:, (ci - 8) * 1040:(ci - 7) * 1040], reads=[t_vaug], writes=[t_kvsrc[ci]])
            emit_collective(k, nc, "AllGather", kv_src[ci], kv_dst[ci], [t_kvsrc[ci]], [t_kvdst[ci]], "cc_kv%d" % ci)
        gch = [d_.rearrange("(r p) n -> p r n", p=128) for d_ in kv_dst]

        def gblk(a, g):
            bl = a * 4 + g
            return gch[bl // 2][:, :, (bl % 2) * 1024:(bl % 2 + 1) * 1024], t_kvdst[bl // 2]

        def gv(vi, g):
            ci = 8 + vi * 2 + g // 2
            return gch[ci][:, :, (g % 2) * 520:(g % 2 + 1) * 520], t_kvdst[ci]
        A = Ctx()
        A.pos, A.w_in, A.w_o, A.w1, A.w3, A.w2 = pos1, w_in, w_o1, w1[1], w3[1], w2[1]
        A.phi_k1, A.phi_v1, A.causal, A.cvalid, A.sbias_d, A.xspill, A.xout = phi_k1, phi_v1, causal, cvalid, sbias_d, xspill, xout
        A.phi2_sb, A.peT_sb, A.ovl_sb, A.t_nc, A.kcT_all, A.vc_all, A.t_cmpkv = phi2_sb, peT_sb, ovl_sb, t_nc, kcT_all, vc_all, t_cmpkv
        A.cmp_prepped = False
        A.pre_toks = [t_trigA, t_vaug] + wk["tq"] + wk["tt1"] + wk["tt2"]

        def load_cmp_src(g, kv, kvA, t_kvA, selc, t_selc):
            a = kv
            src_, tsrc_ = gblk(a, g)
            k.dma("sp", selc.rearrange("p (r t) -> p r t", r=4), src_, reads=[tsrc_], writes=[t_selc])
            pecol = 16 * kv
            for jj in range(16):
                for half in range(2):
                    ps_ = slice(half * 64, (half + 1) * 64)
                    o0 = 2 * jj + half
                    k.op("dve",
                         lambda e: e.tensor_scalar(out=kvA[ps_, jj * 255:(jj + 1) * 255], in0=selc[ps_, o0:o0 + 16 * 254 + 1:16],
                                                   scalar1=peT_sb[ps_, pecol + jj:pecol + jj + 1], scalar2=None, op0=ALU.add),
                         reads=[t_selc, t_nc], writes=[t_kvA])

        def load_kv(g, kvA, t_kvA, Vs, t_Vs, Kw, t_Kw, Vw, t_Vw):
            Vsf = Vs.rearrange("p t d -> p (t d)")
            src_, tsrc_ = gv(1, g)
            k.dma("sp", Vsf.rearrange("p (r n) -> p r n", r=4), src_, reads=[tsrc_], writes=[t_Vs])
            for i in range(4):
                for (dst, src, col) in ((Vw[:, 0:4, :], Vs[:, 8 * i + 4:8 * i + 8, :], i), (Vw[:, 4:12, :], Vs[:, 8 * i:8 * i + 8, :], 4 + i)):
                    if i == 0:
                        k.op("dve", lambda e: e.tensor_scalar(out=dst, in0=src, scalar1=oneh[:, col:col + 1], scalar2=None, op0=ALU.mult), reads=[t_Vs, t_nc], writes=[t_Vw])
                    else:
                        k.op("dve", lambda e: e.scalar_tensor_tensor(out=dst, in0=src, scalar=oneh[:, col:col + 1], in1=dst, op0=ALU.mult, op1=ALU.add),
                             reads=[t_Vs, t_nc, t_Vw], writes=[t_Vw])
            src_, tsrc_ = gblk(3, g)
            k.dma("sp", kvA.rearrange("p (r t) -> p r t", r=4), src_, reads=[tsrc_], writes=[t_kvA])
            for i in range(4):
                for (dst, src, col) in ((Kw[:, 0:512], kvA[:, i * 1024 + 512:(i + 1) * 1024], i), (Kw[:, 512:1536], kvA[:, i * 1024:(i + 1) * 1024], 4 + i)):
                    if i == 0:
                        k.op("dve", lambda e: e.tensor_scalar(out=dst, in0=src, scalar1=oneh[:, col:col + 1], scalar2=None, op0=ALU.mult), reads=[t_kvA, t_nc], writes=[t_Kw])
                    else:
                        k.op("dve", lambda e: e.scalar_tensor_tensor(out=dst, in0=src, scalar=oneh[:, col:col + 1], in1=dst, op0=ALU.mult, op1=ALU.add),
                             reads=[t_kvA, t_nc, t_Kw], writes=[t_Kw])
            src_, tsrc_ = gblk(2, g)
            k.dma("sp", kvA.rearrange("p (r t) -> p r t", r=4), src_, reads=[tsrc_], writes=[t_kvA])
            src_, tsrc_ = gv(0, g)
            k.dma("sp", Vsf.rearrange("p (r n) -> p r n", r=4), src_, reads=[tsrc_], writes=[t_Vs])
        A.load_cmp_src, A.load_kv = load_cmp_src, load_kv
        nsa_core(nc, k, C, A)
        k.finish([C.t_out])
        print("fused program: %d instructions" % k.ninst, k.cnt)
    return nc


def fused_inputs(inp, core):
    b, r = core // 4, core % 4
    m0 = layer0_inputs(inp, np.zeros((128, 96), np.float32), core)
    vec0 = np.zeros((128, NV), np.float32)
    vec0[:, 0:280] = m0["vec"][:, 0:280]
    dummy = {"kc_full": None, "vc_full": None, "ks_full": None, "kw_full": np.zeros((4, 128, 4096), BF),
             "vs_aug_full": np.zeros((4, 128, 32, 65), BF), "vw_aug_full": np.zeros((4, 128, 32, 65), BF)}
    m1 = layer1b_inputs(inp, np.zeros((128, 96), np.float32), np.zeros((1, 1), np.float32), core, dummy)
    vec1 = np.zeros((128, NV), np.float32)
    vec1[:, 0:280] = m1["vec"][:, 0:280]
    wcat = np.concatenate([inp["w_ada"][0], inp["w_ada"][1]], axis=1)
    bcat = np.concatenate([inp["b_ada"][0], inp["b_ada"][1]], axis=0)
    c = inp["c"][b].astype(np.float32)
    cT = np.ascontiguousarray(np.stack([c, c], axis=1).reshape(16, 128, 2).transpose(1, 0, 2))
    oneh = np.zeros((128, 8), np.float32)
    if r > 0:
        oneh[:, r - 1] = 1.0
    oneh[:, 4 + r] = 1.0
    return {"xin": m0["xin"], "pos": m0["pos"], "vec0": vec0, "vec1": vec1, "ident_f": m0["ident_f"], "masks": m0["masks"],
            "rows0": m0["rows"], "rows1": m1["rows"], "cT": cT,
            "wada": np.ascontiguousarray(wcat[:, r * 6144:(r + 1) * 6144]), "bada": np.ascontiguousarray(bcat[None, r * 6144:(r + 1) * 6144]),
            "w_qkv": inp["swa_w_qkv"][0], "w_o0": inp["swa_w_o"][0], "wr0": m0["wr"], "wr1": m1["wr"],
            "w1": inp["moe_w1"], "w3": inp["moe_w3"], "w2": inp["moe_w2"],
            "w_in": inp["nsa_w_in"][0], "w_o1": inp["nsa_w_o"][0], "phi_k1": inp["nsa_phi_k1"][0], "phi_v1": inp["nsa_phi_v1"][0],
            "phi2": m1["phi2"], "peT": m1["peT"], "causal": m1["causal"], "cvalid": m1["cvalid"], "sbias": m1["sbias"], "ovl": m1["ovl"],
            "oneh": oneh}


def kernel(**inp):
    inp = {kk: np.asarray(vv) for kk, vv in inp.items()}
    cores = list(range(8))
    in_maps = [fused_inputs(inp, cc) for cc in cores]
    res = run_bass_kernel_spmd(build_fused(), in_maps, core_ids=cores)
    x2 = np.stack([np.asarray(r["xout"]) for r in res.results])
    return np.ascontiguousarray(x2.reshape(2, 4096, 2048)).astype(np.float32)
```

```python
import math
import numpy as np
import ml_dtypes
from contextlib import ExitStack
import concourse.bass as bass
import concourse.mybir as mybir
from concourse.bass_utils import run_bass_kernel_spmd

F32 = mybir.dt.float32
BF16 = mybir.dt.bfloat16
I32 = mybir.dt.int32
AF = mybir.ActivationFunctionType
ALU = mybir.AluOpType
AX = mybir.AxisListType

D = 2048
NCH = 16
NT = 1024
HALO0 = 128
ALPHA = 4 ** 0.25
LN_EPS = 1e-5
EPS_EFF = LN_EPS / (ALPHA * ALPHA)
ROPE_THETA = 500000.0
SCALE = 64 ** -0.5
WB = 256


class Tok:
    __slots__ = ("name", "w", "r")

    def __init__(self, name=""):
        self.name = name
        self.w = None
        self.r = {}


class K:
    ND = 6

    def __init__(self, nc, stack):
        self.nc = nc
        self.stack = stack
        self.eng = {"pe": nc.tensor, "act": nc.scalar, "dve": nc.vector, "pool": nc.gpsimd, "sp": nc.sync}
        self.sem = {}
        self.cnt = {}
        self.pend = {}
        for e in ("pe", "act", "dve", "pool"):
            self.sem[e] = stack.enter_context(nc.semaphore("s_" + e))
            self.cnt[e] = 0
            self.pend[e] = False
        self.dsem = {}
        self.dcnt = {}
        self.drr = {}
        self.dwaited = {}
        for q in ("sp", "act", "pool"):
            self.dsem[q] = [stack.enter_context(nc.semaphore("d_%s%d" % (q, i))) for i in range(self.ND)]
            self.dcnt[q] = [0] * self.ND
            self.dwaited[q] = [0] * self.ND
            self.drr[q] = 0
        self.waited = {e: {} for e in self.eng}
        self.ninst = 0
        self._n = 0

    def sb(self, name, shape, dt):
        return self.stack.enter_context(self.nc.sbuf_tensor("sb_" + name, list(shape), dt))

    def ps(self, name, shape, dt):
        return self.stack.enter_context(self.nc.psum_tensor("ps_" + name, list(shape), dt))

    def _wait(self, e, sem, val):
        w = self.waited[e]
        kk = id(sem)
        if w.get(kk, 0) >= val:
            return
        self.eng[e].wait_ge(sem, val)
        w[kk] = val

    def _deps(self, e, reads, writes):
        own = self.sem.get(e)
        pe = (e == "pe")
        for t in reads:
            if t.w is not None and not (pe and t.w[0] is own):
                self._wait(e, *t.w)
        for t in writes:
            if t.w is not None and not (pe and t.w[0] is own):
                self._wait(e, *t.w)
            for (s, v) in t.r.values():
                if pe and s is own:
                    continue
                self._wait(e, s, v)

    def _mark(self, sem, val, reads, writes):
        for t in writes:
            t.w = (sem, val)
            t.r = {}
        for t in reads:
            t.r[id(sem)] = (sem, val)

    def op(self, e, ins_fn, reads=(), writes=(), inc=True):
        self._deps(e, reads, writes)
        ins = ins_fn(self.eng[e])
        if inc:
            self.cnt[e] += 1
            ins.then_inc(self.sem[e], 1)
            self.pend[e] = False
            self._mark(self.sem[e], self.cnt[e], reads, writes)
        else:
            self.pend[e] = True
            self._mark(self.sem[e], self.cnt[e] + 1, reads, writes)
        self.ninst += 1
        return ins

    def dma(self, q, out, in_, reads=(), writes=(), **kw):
        i = self.drr[q]
        self.drr[q] = (i + 1) % self.ND
        sem = self.dsem[q][i]
        if self.dwaited[q][i] < self.dcnt[q][i]:
            self._wait(q, sem, self.dcnt[q][i])
            self.dwaited[q][i] = self.dcnt[q][i]
        self._deps(q, reads, writes)
        ins = self.eng[q].dma_start(out=out, in_=in_, **kw)
        self.dcnt[q][i] += 16
        ins.then_inc(sem, 16)
        self._mark(sem, self.dcnt[q][i], reads, writes)
        self.ninst += 1
        return ins

    def finish(self, toks, e="sp"):
        for t in toks:
            if t.w is not None:
                self._wait(e, *t.w)


class Ctx:
    pass


def setup_ctx(k, consts_ap, hoff=HALO0):
    C = Ctx()
    C.k = k
    C.pb = [k.ps("pb%d" % i, [128, 512], F32) for i in range(8)]
    C.t_pb = [Tok("pb%d" % i) for i in range(8)]
    C.pbi = 0
    C.xT = k.sb("xT", [128, NCH, NT], F32)
    C.t_x = [[Tok() for _ in range(2)] for _ in range(NCH)]
    C.HOFF = hoff
    C.hT = k.sb("hT", [128, NCH, NT + hoff], BF16)
    C.t_h = [[Tok() for _ in range(3)] for _ in range(NCH)]
    C.big = k.sb("big", [128, NCH, NT], BF16)
    C.t_big = [[Tok() for _ in range(2)] for _ in range(NCH)]
    C.NWB = 3
    C.wb = [k.sb("wb%d" % i, [128, NCH, WB], BF16) for i in range(C.NWB)]
    C.t_wb = [Tok() for _ in range(C.NWB)]
    C.wbi = 0
    C.ident_bf = k.sb("ident_bf", [128, 128], BF16)
    C.ident_f = k.sb("ident_f", [128, 128], F32)
    C.ones_f = k.sb("ones_f", [128, 128], F32)
    C.ones_bf = k.sb("ones_bf", [128, 128], BF16)
    C.t_const = Tok("const")
    k.dma("sp", C.ident_f[:], consts_ap["ident_f"], writes=[C.t_const])
    k.dma("pool", C.ident_bf[:], consts_ap["ident_f"], writes=[C.t_const])
    k.op("dve", lambda e: e.memset(C.ones_f[:], 1.0), writes=[C.t_const])
    k.op("dve", lambda e: e.memset(C.ones_bf[:], 1.0), writes=[C.t_const])
    return C


def next_pb(C):
    i = C.pbi
    C.pbi = (i + 1) % 8
    return C.pb[i], C.t_pb[i]


def next_wb(C):
    wl = getattr(C, 'wb_cur', None) or C.wb
    tl = getattr(C, 't_wb_cur', None) or C.t_wb
    i = C.wbi % len(wl)
    C.wbi = (i + 1) % len(wl)
    return wl[i], tl[i]


def tsl(th):
    return slice(th * 512, (th + 1) * 512)


def load_wblock(C, srcs, q="pool"):
    k = C.k
    buf, tok = next_wb(C)
    for (ap, off) in srcs:
        w = ap.shape[1]
        k.dma(q, buf[:, :, off:off + w], ap.rearrange("(kc p) n -> p kc n", p=128), writes=[tok])
    return buf, tok


def mm_group(C, out_ap, t_out, pairs, reads, first_in_bank=True):
    k = C.k
    n = len(pairs)
    for i, (l, r) in enumerate(pairs):
        k.op("pe", lambda e: e.matmul(out_ap, lhsT=l, rhs=r, start=(i == 0 and first_in_bank), stop=(i == n - 1),
                                      skip_group_check=True),
             reads=reads, writes=[t_out], inc=(i == n - 1))


def load_x_transposed(C, x_dram, n_tiles, dst_fn, stage, t_stage):
    k = C.k
    for t in range(n_tiles):
        s = t % 2
        k.dma("sp", stage[s][:], x_dram[t * 128:(t + 1) * 128, :], writes=[t_stage[s]])
        for c0 in range(0, NCH, 4):
            pb, tp = next_pb(C)
            for j in range(4):
                c = c0 + j
                k.op("pe", lambda e: e.transpose(pb[:, j * 128:(j + 1) * 128], stage[s][:, c * 128:(c + 1) * 128], C.ident_f[:]),
                     reads=[t_stage[s], C.t_const], writes=[tp], inc=(j == 3))
            dst_fn(t, c0, pb[:].rearrange("p (j n) -> p j n", j=4), tp)


def layer_norm_mod(C, vec, g_col, b_col, A_col, B_col, tmp, t_tmp, stat, t_stat, h_halo=False):
    k = C.k
    if hasattr(C, "t_wb4"):
        for e_ in ("act", "dve", "pool"):
            k._deps(e_, (), [C.t_wb4])
    for th in range(2):
        ts = tsl(th)
        pb_s, tp_s = next_pb(C)
        pb_q, tp_q = next_pb(C)
        for c in range(NCH):
            k.op("pe", lambda e: e.matmul(pb_s[:], lhsT=C.ones_f[:], rhs=C.xT[:, c, ts], start=(c == 0), stop=(c == NCH - 1)),
                 reads=[C.t_x[c][th], C.t_const], writes=[tp_s], inc=(c == NCH - 1))
        for c in range(NCH):
            s = c % 2
            k.op("act", lambda e: e.activation(out=tmp[s][:], in_=C.xT[:, c, ts], func=AF.Square),
                 reads=[C.t_x[c][th]], writes=[t_tmp[s]])
            k.op("pe", lambda e: e.matmul(pb_q[:], lhsT=C.ones_f[:], rhs=tmp[s][:], start=(c == 0), stop=(c == NCH - 1)),
                 reads=[t_tmp[s], C.t_const], writes=[tp_q])
        mean, rstd, msq = stat[0], stat[1], stat[2]
        k.op("act", lambda e: e.mul(out=mean[:], in_=pb_s[:], mul=1.0 / D), reads=[tp_s], writes=[t_stat[0]])
        k.op("dve", lambda e: e.tensor_tensor(out=msq[:], in0=mean[:], in1=mean[:], op=ALU.mult), reads=[t_stat[0]], writes=[t_stat[2]])
        k.op("dve", lambda e: e.scalar_tensor_tensor(out=msq[:], in0=pb_q[:], scalar=1.0 / D, in1=msq[:], op0=ALU.mult, op1=ALU.subtract),
             reads=[tp_q, t_stat[2]], writes=[t_stat[2]])
        k.op("dve", lambda e: e.tensor_scalar(out=msq[:], in0=msq[:], scalar1=EPS_EFF, scalar2=None, op0=ALU.add),
             reads=[t_stat[2]], writes=[t_stat[2]])
        k.op("act", lambda e: e.sqrt(out=msq[:], in_=msq[:]), reads=[t_stat[2]], writes=[t_stat[2]])
        k.op("dve", lambda e: e.reciprocal(out=rstd[:], in_=msq[:]), reads=[t_stat[2]], writes=[t_stat[1]])
        for c in range(NCH):
            s = c % 2
            k.op("dve", lambda e: e.tensor_tensor(out=tmp[s][:], in0=C.xT[:, c, ts], in1=mean[:], op=ALU.subtract),
                 reads=[C.t_x[c][th], t_stat[0]], writes=[t_tmp[s]])
            k.op("dve", lambda e: e.tensor_tensor(out=tmp[s][:], in0=tmp[s][:], in1=rstd[:], op=ALU.mult),
                 reads=[t_tmp[s], t_stat[1]], writes=[t_tmp[s]])
            k.op("act", lambda e: e.activation(out=C.xT[:, c, ts], in_=tmp[s][:], func=AF.Identity,
                                               scale=vec[:, g_col + c:g_col + c + 1], bias=vec[:, b_col + c:b_col + c + 1]),
                 reads=[t_tmp[s], C.t_vec], writes=[C.t_x[c][th]])
            if A_col is not None:
                k.op("act", lambda e: e.activation(out=C.hT[:, c, C.HOFF + th * 512:C.HOFF + (th + 1) * 512], in_=tmp[s][:], func=AF.Identity,
                                                   scale=vec[:, A_col + c:A_col + c + 1], bias=vec[:, B_col + c:B_col + c + 1]),
                     reads=[t_tmp[s], C.t_vec], writes=[C.t_h[c][1 + th]])


def linear_fm(C, w_blocks, rhs_fn, rhs_toks_fn, n_tok_pieces, evac_fn):
    k = C.k
    for bi, (srcs, nm) in enumerate(w_blocks):
        buf, tw = load_wblock(C, srcs)
        for m in range(nm):
            for pc in range(n_tok_pieces):
                pb, tp = next_pb(C)
                rts = rhs_toks_fn(pc)
                for kc in range(NCH):
                    r = rhs_fn(kc, pc)
                    k.op("pe", lambda e: e.matmul(pb[:, 0:r.shape[-1]] if len(r.shape) == 2 else pb[:], lhsT=buf[:, kc, m * 128:(m + 1) * 128], rhs=r,
                                                  start=(kc == 0), stop=(kc == NCH - 1)),
                         reads=[tw] + rts(kc), writes=[tp], inc=(kc == NCH - 1))
                evac_fn(bi, m, pc, pb, tp)


def moe_layer(C, L, vec, W):
    k = C.k
    if not hasattr(C, "t_wb4"):
        C.t_wb4 = Tok()
    wb4 = C.bfv(20480, 4096).rearrange("p (kc n) -> p kc n", kc=16)
    k._deps("pool", (), C.t_tmp + C.t_stat + [C.t_wb4])
    C.wb_cur = C.wb + [wb4]
    C.t_wb_cur = C.t_wb + [C.t_wb4]
    pbr, tpr = next_pb(C)
    for c in range(NCH):
        s = c % 2
        k.op("dve", lambda e: e.tensor_scalar(out=C.h32[s][:], in0=C.xT[:, c, :], scalar1=vec[:, W["sc1"] + c:W["sc1"] + c + 1],
                                              scalar2=vec[:, W["sh"] + c:W["sh"] + c + 1], op0=ALU.mult, op1=ALU.add),
             reads=[C.t_x[c][0], C.t_x[c][1], C.t_vec], writes=[C.t_h32[s]])
        for t in range(8):
            k.op("pe", lambda e: e.matmul(pbr[:, t * 20:(t + 1) * 20], lhsT=C.h32[s][:, t * 128:(t + 1) * 128], rhs=C.wr[:, c, :],
                                          start=(c == 0 and t == 0), stop=(c == NCH - 1), skip_group_check=True),
                 reads=[C.t_h32[s], C.t_wr], writes=[tpr], inc=(t == 7))
    lg = C.lg
    tl = C.t_lg
    k.op("dve", lambda e: e.tensor_tensor(out=lg[:], in0=pbr[:, 0:160].rearrange("p (t n) -> p t n", t=8),
                                          in1=C.brow[:].unsqueeze(1).to_broadcast([128, 8, 20]), op=ALU.add),
         reads=[tpr, C.t_wr], writes=[tl])
    R = C.rt
    tr = C.t_rt
    for t in range(8):
        gl = lg[:, t, 0:4]
        rl = lg[:, t, 4:20].rearrange("p (g j) -> p g j", g=4)
        k.op("dve", lambda e: e.tensor_reduce(out=R["gmax"][:], in_=gl, axis=AX.X, op=ALU.max), reads=[tl], writes=[tr])
        k.op("dve", lambda e: e.tensor_scalar(out=R["ngmax"][:], in0=R["gmax"][:], scalar1=-1.0, scalar2=None, op0=ALU.mult), reads=[tr], writes=[tr])
        k.op("act", lambda e: e.activation(out=R["ge"][:], in_=gl, func=AF.Exp, bias=R["ngmax"][:], scale=1.0, accum_out=R["gsum"][:]),
             reads=[tl, tr], writes=[tr])
        k.op("dve", lambda e: e.reciprocal(out=R["gprob"][:], in_=R["gsum"][:]), reads=[tr], writes=[tr])
        k.op("dve", lambda e: e.tensor_scalar(out=R["gw"][:], in0=gl, scalar1=R["gmax"][:], scalar2=R["gprob"][:], op0=ALU.is_equal, op1=ALU.mult),
             reads=[tl, tr], writes=[tr])
        k.op("dve", lambda e: e.tensor_reduce(out=R["m1"][:], in_=rl, axis=AX.X, op=ALU.max), reads=[tl], writes=[tr])
        m1b = R["m1"][:].unsqueeze(2).to_broadcast([128, 4, 4])
        k.op("dve", lambda e: e.tensor_tensor(out=R["eq1"][:], in0=rl, in1=m1b, op=ALU.is_equal), reads=[tl, tr], writes=[tr])
        k.op("dve", lambda e: e.scalar_tensor_tensor(out=R["rl2"][:], in0=R["eq1"][:], scalar=-1e30, in1=rl, op0=ALU.mult, op1=ALU.add),
             reads=[tl, tr], writes=[tr])
        k.op("dve", lambda e: e.tensor_reduce(out=R["m2"][:], in_=R["rl2"][:], axis=AX.X, op=ALU.max), reads=[tr], writes=[tr])
        m2b = R["m2"][:].unsqueeze(2).to_broadcast([128, 4, 4])
        k.op("dve", lambda e: e.tensor_tensor(out=R["top2"][:], in0=rl, in1=m2b, op=ALU.is_ge), reads=[tl, tr], writes=[tr])
        k.op("dve", lambda e: e.tensor_tensor(out=R["dd"][:], in0=rl, in1=m1b, op=ALU.subtract), reads=[tl, tr], writes=[tr])
        k.op("act", lambda e: e.activation(out=R["ee"][:], in_=R["dd"][:], func=AF.Exp), reads=[tr], writes=[tr])
        k.op("dve", lambda e: e.tensor_tensor(out=R["ee"][:], in0=R["ee"][:], in1=R["top2"][:], op=ALU.mult), reads=[tr], writes=[tr])
        k.op("dve", lambda e: e.tensor_reduce(out=R["den"][:], in_=R["ee"][:], axis=AX.X, op=ALU.add), reads=[tr], writes=[tr])
        k.op("dve", lambda e: e.reciprocal(out=R["den"][:], in_=R["den"][:]), reads=[tr], writes=[tr])
        k.op("dve", lambda e: e.tensor_tensor(out=R["den"][:], in0=R["den"][:], in1=R["gw"][:], op=ALU.mult), reads=[tr], writes=[tr])
        k.op("dve", lambda e: e.tensor_tensor(out=C.comb[:, t, :].rearrange("p (g j) -> p g j", g=4), in0=R["ee"][:],
                                              in1=R["den"][:].unsqueeze(2).to_broadcast([128, 4, 4]), op=ALU.mult),
             reads=[tr], writes=[C.t_comb])
    w1, w3, w2 = W["w1"], W["w3"], W["w2"]
    for gi in range(4):
        for j in range(4):
            ecol = gi * 4 + j
            for half in range(2):
                pb, tp = next_pb(C)
                for tt in range(4):
                    t = half * 4 + tt
                    s = (tt % 2)
                    k.op("dve", lambda e: e.tensor_scalar(out=C.diag[s][:], in0=C.ident_bf[:], scalar1=C.comb[:, t, ecol:ecol + 1], scalar2=None, op0=ALU.mult),
                         reads=[C.t_comb, C.t_const], writes=[C.t_diag[s]])
                    k.op("pe", lambda e: e.matmul(pb[:, tt * 128:(tt + 1) * 128], lhsT=C.ones_bf[:], rhs=C.diag[s][:], start=(tt == 0), stop=(tt == 3), skip_group_check=True),
                         reads=[C.t_diag[s], C.t_const], writes=[tp])
                k.op("act", lambda e: e.copy(out=C.cmb[:, j, half * 512:(half + 1) * 512], in_=pb[:]), reads=[tp], writes=[C.t_cmb[j][half]])
        for hb in range(8):
            j = hb // 2
            e_id = gi * 4 + j
            cols = slice((hb % 2) * 256, (hb % 2) * 256 + 256)
            b1, tw1 = load_wblock(C, [(w1[e_id][:, cols], 0)])
            b3, tw3 = load_wblock(C, [(w3[e_id][:, cols], 0)])
            for m in range(2):
                hc = hb * 2 + m
                for th in range(2):
                    hs = slice(C.HOFF + th * 512, C.HOFF + (th + 1) * 512)
                    pa, tpa = next_pb(C)
                    pbb, tpb = next_pb(C)
                    for kc in range(NCH):
                        k.op("pe", lambda e: e.matmul(pa[:], lhsT=b1[:, kc, m * 128:(m + 1) * 128], rhs=C.hT[:, kc, hs], start=(kc == 0), stop=(kc == NCH - 1)),
                             reads=[tw1, C.t_h[kc][1 + th]], writes=[tpa], inc=(kc == NCH - 1))
                    for kc in range(NCH):
                        k.op("pe", lambda e: e.matmul(pbb[:], lhsT=b3[:, kc, m * 128:(m + 1) * 128], rhs=C.hT[:, kc, hs], start=(kc == 0), stop=(kc == NCH - 1)),
                             reads=[tw3, C.t_h[kc][1 + th]], writes=[tpb], inc=(kc == NCH - 1))
                    s = (hc * 2 + th) % 2
                    k.op("act", lambda e: e.activation(out=C.sa[s][:], in_=pa[:], func=AF.Silu), reads=[tpa], writes=[C.t_sa[s]])
                    k.op("dve", lambda e: e.tensor_tensor(out=C.sa[s][:], in0=C.sa[s][:], in1=pbb[:], op=ALU.mult), reads=[C.t_sa[s], tpb], writes=[C.t_sa[s]])
                    k.op("dve", lambda e: e.tensor_tensor(out=C.big[:, hc, tsl(th)], in0=C.sa[s][:], in1=C.cmb[:, j, tsl(th)], op=ALU.mult),
                         reads=[C.t_sa[s], C.t_cmb[j][th]], writes=[C.t_big[hc][th]])
        w2g = w2[gi * 4:(gi + 1) * 4].rearrange("e h n -> (e h) n")
        for ob in range(8):
            bw, tw = load_wblock(C, [(w2g[:, ob * 256:(ob + 1) * 256], 0)])
            for m in range(2):
                c = ob * 2 + m
                for th in range(2):
                    pb, tp = next_pb(C)
                    for kc in range(NCH):
                        k.op("pe", lambda e: e.matmul(pb[:], lhsT=bw[:, kc, m * 128:(m + 1) * 128], rhs=C.big[:, kc, tsl(th)], start=(kc == 0), stop=(kc == NCH - 1)),
                             reads=[tw, C.t_big[kc][th]], writes=[tp], inc=(kc == NCH - 1))
                    k.op("dve", lambda e: e.scalar_tensor_tensor(out=C.xT[:, c, tsl(th)], in0=pb[:], scalar=vec[:, W["gp"] + c:W["gp"] + c + 1],
                                                                 in1=C.xT[:, c, tsl(th)], op0=ALU.mult, op1=ALU.add),
                         reads=[tp, C.t_vec, C.t_x[c][th]], writes=[C.t_x[c][th]])
    C.wb_cur = None
    C.t_wb_cur = None
    C.wbi = 0


V_MODT = 0
V_SH_T, V_SC_T, V_G_T, V_SH_C, V_SC_C, V_G_C = 0, 16, 32, 48, 64, 80
V_LN_T_G, V_LN_T_B, V_LN_C_G, V_LN_C_B = 96, 112, 128, 144
V_BQ, V_BK = 160, 176
V_INVF, V_M16, V_OM16, V_SGN = 180, 181, 182, 183
V_SC1_T, V_GP_T, V_SC1_C, V_GP_C, V_A_C, V_B_C = 184, 200, 216, 232, 248, 264
V_A_N, V_B_N = 280, 296
NV = 312
RBYTES = 34816


def setup_region(C):
    k = C.k
    C.R = k.sb("R", [128, RBYTES // 2], BF16)

    def f32v(b0, n):
        return C.R[:, b0 // 2:b0 // 2 + 2 * n].bitcast(F32)

    def bfv(b0, n):
        return C.R[:, b0 // 2:b0 // 2 + n]
    C.f32v, C.bfv = f32v, bfv
    C.stage = [f32v(0, 2048), f32v(8192, 2048)]
    C.t_stage = [Tok(), Tok()]
    C.h32 = [f32v(0, 1024), f32v(4096, 1024)]
    C.t_h32 = [Tok(), Tok()]
    C.cmb = bfv(8192, 4096).rearrange("p (j t) -> p j t", j=4)
    C.t_cmb = [[Tok(), Tok()] for _ in range(4)]
    C.sa = [f32v(16384, 512), f32v(18432, 512)]
    C.t_sa = [Tok(), Tok()]
    C.tmp = [f32v(20480, 512), f32v(22528, 512)]
    C.t_tmp = [Tok(), Tok()]
    C.stat = [f32v(24576, 512), f32v(26624, 512), f32v(28672, 512)]
    C.t_stat = [Tok(), Tok(), Tok()]
    C.vec = k.sb("vec", [128, NV], F32)
    C.t_vec = Tok("vec")
    C.wr = k.sb("wr", [128, NCH, 20], F32)
    C.brow = k.sb("brow", [128, 20], F32)
    C.t_wr = Tok()
    C.lg = k.sb("lg", [128, 8, 20], F32)
    C.t_lg = Tok()
    C.comb = k.sb("comb", [128, 8, 16], F32)
    C.t_comb = Tok()
    C.rt = {}
    for nm, shp in (("gmax", [128, 1]), ("ngmax", [128, 1]), ("gsum", [128, 1]), ("gprob", [128, 1]), ("ge", [128, 4]), ("gw", [128, 4]),
                    ("m1", [128, 4]), ("m2", [128, 4]), ("den", [128, 4]), ("eq1", [128, 4, 4]), ("rl2", [128, 4, 4]),
                    ("top2", [128, 4, 4]), ("dd", [128, 4, 4]), ("ee", [128, 4, 4])):
        C.rt[nm] = k.sb("rt_" + nm, shp, F32)
    C.t_rt = Tok()
    C.diag = [k.sb("diag%d" % i, [128, 128], BF16) for i in range(2)]
    C.t_diag = [Tok(), Tok()]
    C.E = [k.sb("E%d" % i, [128, 512], BF16) for i in range(4)]
    C.t_E = [Tok() for _ in range(4)]
    C.Ei = 0


def derive_vec(C):
    k = C.k
    v = C.vec
    tv = C.t_vec

    def ts(out_c, in_c, s1, s2, o0, o1=None):
        if o1 is None:
            k.op("dve", lambda e: e.tensor_scalar(out=v[:, out_c:out_c + 16], in0=v[:, in_c:in_c + 16], scalar1=s1, scalar2=None, op0=o0), reads=[tv], writes=[tv])
        else:
            k.op("dve", lambda e: e.tensor_scalar(out=v[:, out_c:out_c + 16], in0=v[:, in_c:in_c + 16], scalar1=s1, scalar2=s2, op0=o0, op1=o1), reads=[tv], writes=[tv])
    ts(V_SC1_T, V_SC_T, 1.0, None, ALU.add)
    ts(V_SC1_C, V_SC_C, 1.0, None, ALU.add)
    ts(V_GP_T, V_G_T, 1.0 / ALPHA, None, ALU.mult)
    ts(V_GP_C, V_G_C, 1.0 / ALPHA, None, ALU.mult)
    k.op("dve", lambda e: e.tensor_tensor(out=v[:, V_A_C:V_A_C + 16], in0=v[:, V_LN_T_G:V_LN_T_G + 16], in1=v[:, V_SC1_C:V_SC1_C + 16], op=ALU.mult), reads=[tv], writes=[tv])
    k.op("dve", lambda e: e.tensor_tensor(out=v[:, V_B_C:V_B_C + 16], in0=v[:, V_LN_T_B:V_LN_T_B + 16], in1=v[:, V_SC1_C:V_SC1_C + 16], op=ALU.mult), reads=[tv], writes=[tv])
    k.op("dve", lambda e: e.tensor_tensor(out=v[:, V_B_C:V_B_C + 16], in0=v[:, V_B_C:V_B_C + 16], in1=v[:, V_SH_C:V_SH_C + 16], op=ALU.add), reads=[tv], writes=[tv])


def rope_tables(C, pos_dram, ntok, Tcos, Tsin, t_trig, wk_i, wk_f, wk_a, pre=()):
    k = C.k
    v = C.vec
    tw = Tok()
    TWO_PI = 2.0 * math.pi
    k.dma("sp", wk_i, pos_dram[0:1, :].to_broadcast([128, ntok]), writes=[tw, t_trig] + list(pre))
    k.op("dve", lambda e: e.tensor_copy(out=wk_f, in_=wk_i), reads=[tw], writes=[tw])
    k.op("dve", lambda e: e.tensor_scalar(out=wk_f, in0=wk_f, scalar1=v[:, V_INVF:V_INVF + 1], scalar2=None, op0=ALU.mult), reads=[tw, C.t_vec], writes=[tw])
    for (off, dst, s1, s2) in ((0.0, Tsin, V_SGN, None), (0.5 * math.pi, Tcos, V_M16, V_OM16)):
        ta = Tok()
        k.op("dve", lambda e: e.tensor_scalar(out=wk_a, in0=wk_f, scalar1=off, scalar2=None, op0=ALU.add), reads=[tw], writes=[ta])
        k.op("dve", lambda e: e.tensor_scalar(out=wk_i, in0=wk_a, scalar1=1.0 / TWO_PI, scalar2=None, op0=ALU.mult), reads=[ta, tw], writes=[tw])
        k.op("dve", lambda e: e.tensor_copy(out=dst, in_=wk_i), reads=[tw], writes=[t_trig])
        k.op("dve", lambda e: e.scalar_tensor_tensor(out=wk_a, in0=dst, scalar=-TWO_PI, in1=wk_a, op0=ALU.mult, op1=ALU.add), reads=[t_trig, ta], writes=[ta])
        k.op("dve", lambda e: e.tensor_scalar(out=dst, in0=wk_a, scalar1=math.pi, scalar2=-TWO_PI, op0=ALU.is_gt, op1=ALU.mult), reads=[ta], writes=[t_trig])
        k.op("dve", lambda e: e.tensor_tensor(out=wk_a, in0=wk_a, in1=dst, op=ALU.add), reads=[ta, t_trig], writes=[ta])
        k.op("dve", lambda e: e.tensor_scalar(out=dst, in0=wk_a, scalar1=-math.pi, scalar2=TWO_PI, op0=ALU.is_lt, op1=ALU.mult), reads=[ta], writes=[t_trig])
        k.op("dve", lambda e: e.tensor_tensor(out=wk_a, in0=wk_a, in1=dst, op=ALU.add), reads=[ta, t_trig], writes=[ta])
        k.op("dve", lambda e: e.tensor_scalar(out=wk_a, in0=wk_a, scalar1=-math.pi, scalar2=math.pi, op0=ALU.max, op1=ALU.min), reads=[ta], writes=[ta])
        k.op("act", lambda e: e.activation(out=dst, in_=wk_a, func=AF.Sin), reads=[ta], writes=[t_trig])
        if s2 is None:
            k.op("dve", lambda e: e.tensor_scalar(out=dst, in0=dst, scalar1=v[:, s1:s1 + 1], scalar2=None, op0=ALU.mult), reads=[t_trig, C.t_vec], writes=[t_trig])
        else:
            k.op("dve", lambda e: e.tensor_scalar(out=dst, in0=dst, scalar1=v[:, s1:s1 + 1], scalar2=v[:, s2:s2 + 1], op0=ALU.mult, op1=ALU.add),
                 reads=[t_trig, C.t_vec], writes=[t_trig])


def rope_evac(C, pb, tp, n, bias_ap, Tcos_s, Tsin_s, t_trig, dst, t_dst, wk):
    k = C.k
    i = wk["i"]
    wk["i"] = (i + 1) % 2
    qb, t1, t2 = wk["qb"][i][:, 0:n], wk["t1"][i][:, 0:n], wk["t2"][i][:, 0:n]
    tq, tt1, tt2 = wk["tq"][i], wk["tt1"][i], wk["tt2"][i]
    k.op("act", lambda e: e.activation(out=qb, in_=pb[:, 0:n], func=AF.Identity, bias=bias_ap, scale=1.0), reads=[tp, C.t_vec], writes=[tq])
    pr, tpr = next_pb(C)
    k.op("pe", lambda e: e.matmul(pr[:, 0:n], lhsT=C.rperm[:], rhs=qb, start=True, stop=True), reads=[tq, C.t_const], writes=[tpr])
    k.op("dve", lambda e: e.tensor_tensor(out=t2, in0=pr[:, 0:n], in1=Tsin_s, op=ALU.mult), reads=[tpr, t_trig], writes=[tt2])
    k.op("dve", lambda e: e.tensor_tensor(out=t1, in0=qb, in1=Tcos_s, op=ALU.mult), reads=[tq, t_trig], writes=[tt1])
    k.op("dve", lambda e: e.tensor_tensor(out=dst, in0=t1, in1=t2, op=ALU.add), reads=[tt1, tt2], writes=[t_dst])


def swa_layer(C, W):
    k = C.k
    v = C.vec
    NTOK = NT + HALO0
    f32v, bfv = C.f32v, C.bfv
    Vaug = bfv(0, 9 * 4 * 65).rearrange("p (t g d) -> p t g d", t=9, g=4)
    t_V = [Tok() for _ in range(9)]
    wk = {"i": 0,
          "qb": [bfv(4736, 512), bfv(5760, 512)], "tq": [Tok(), Tok()],
          "t1": [f32v(6784, 512), f32v(8832, 512)], "tt1": [Tok(), Tok()],
          "t2": [f32v(10880, 512), f32v(12928, 512)], "tt2": [Tok(), Tok()]}
    otok = [bfv(6784, 2048), bfv(10880, 2048)]
    t_otok = [Tok(), Tok()]
    Tcos = f32v(16384, NTOK)
    Tsin = f32v(16384 + 4608, NTOK)
    t_trig = Tok()
    KT = bfv(25600, 4 * NTOK).rearrange("p (g t) -> p g t", g=4)
    t_K = [[Tok() for _ in range(3)] for _ in range(4)]
    wk_i = C.big[:, 0:3, :].rearrange("p a t -> p (a t)")[:, 0:2 * NTOK].bitcast(I32)
    wk_f = C.big[:, 3:6, :].rearrange("p a t -> p (a t)")[:, 0:2 * NTOK].bitcast(F32)
    wk_a = C.big[:, 6:9, :].rearrange("p a t -> p (a t)")[:, 0:2 * NTOK].bitcast(F32)
    rope_tables(C, W["pos"], NTOK, Tcos, Tsin, t_trig, wk_i, wk_f, wk_a, pre=[C.t_big[c__][th__] for c__ in range(9) for th__ in range(2)])
    for c in range(9):
        for th in range(2):
            C.t_big[c][th].w = t_trig.w
    wq = W["w_qkv"]
    blocks = [([(wq[:, b * WB:(b + 1) * WB], 0)], 2) for b in range(8)]

    def q_evac(bi, m, th, pb, tp):
        c = bi * 2 + m
        cs = slice(HALO0 + th * 512, HALO0 + (th + 1) * 512)
        rope_evac(C, pb, tp, 512, v[:, V_BQ + c:V_BQ + c + 1], Tcos[:, cs], Tsin[:, cs], t_trig, C.big[:, c, tsl(th)], C.t_big[c][th], wk)
    linear_fm(C, blocks, lambda kc, th: C.hT[:, kc, HALO0 + th * 512:HALO0 + (th + 1) * 512],
              lambda th: (lambda kc: [C.t_h[kc][1 + th]]), 2, q_evac)
    kblocks = []
    for b in range(2):
        srcs = []
        for m in range(2):
            g = b * 2 + m
            col = 2048 + g * 64
            srcs.append((wq[:, col:col + 64], m * 128))
            srcs.append((wq[:, col:col + 64], m * 128 + 64))
        kblocks.append((srcs, 2))

    def k_evac(bi, m, pc, pb, tp):
        g = bi * 2 + m
        cs = slice(pc * 384, (pc + 1) * 384)
        rope_evac(C, pb, tp, 384, v[:, V_BK + g:V_BK + g + 1], Tcos[:, cs], Tsin[:, cs], t_trig, KT[:, g, cs], t_K[g][pc], wk)

    def k_rt(pc):
        def f(kc):
            return [C.t_h[kc][0], C.t_h[kc][1], C.t_h[kc][2]]
        return f
    linear_fm(C, kblocks, lambda kc, pc: C.hT[:, kc, pc * 384:(pc + 1) * 384], k_rt, 3, k_evac)
    tvones = Tok()
    k.op("pool", lambda e: e.memset(Vaug[:, :, :, 64:65], 1.0), writes=t_V)
    vbuf, tvb = load_wblock(C, [(wq[:, 2304:2560], 0)])
    for t in range(9):
        pb, tp = next_pb(C)
        for kc in range(NCH):
            k.op("pe", lambda e: e.matmul(pb[:, 0:256], lhsT=C.hT[:, kc, t * 128:(t + 1) * 128], rhs=vbuf[:, kc, :], start=(kc == 0), stop=(kc == NCH - 1)),
                 reads=[tvb, C.t_h[kc][0], C.t_h[kc][1], C.t_h[kc][2]], writes=[tp], inc=(kc == NCH - 1))
        k.op("dve", lambda e: e.tensor_tensor(out=Vaug[:, t, :, 0:64], in0=pb[:, 0:256].rearrange("p (g d) -> p g d", g=4),
                                              in1=C.bvrow[:].rearrange("p (g d) -> p g d", g=4), op=ALU.add),
             reads=[tp, C.t_wr], writes=[t_V[t]])
    esink = C.esink[:].rearrange("p (i two) -> p i two", two=2)
    for j in range(8):
        so = j % 2
        ot = otok[so]
        otv = ot.rearrange("p (i two d) -> p i two d", two=2, d=64)
        for g in range(4):
            for par in range(2):
                ps_ = slice(par * 64, (par + 1) * 64)
                Es = []
                for (kt, mask) in ((j, C.mprev0 if j == 0 else C.mprev), (j + 1, C.mdiag)):
                    pb, tp = next_pb(C)
                    kcs = slice(kt * 128, (kt + 1) * 128)
                    k.op("pe", lambda e: e.matmul(pb[:].rearrange("p (h q) -> p h q", h=4), lhsT=KT[ps_, g, kcs],
                                                  rhs=C.big[ps_, 4 * g:4 * g + 4, j * 128:(j + 1) * 128], start=True, stop=True),
                         reads=[t_K[g][kt // 3], C.t_big[4 * g][j // 4], C.t_big[4 * g + 1][j // 4], C.t_big[4 * g + 2][j // 4], C.t_big[4 * g + 3][j // 4]],
                         writes=[tp])
                    ei = C.Ei
                    C.Ei = (ei + 1) % 4
                    E, tE = C.E[ei], C.t_E[ei]
                    k.op("act", lambda e: e.activation(out=E[:], in_=pb[:], func=AF.Exp, scale=SCALE), reads=[tp], writes=[tE])
                    k.op("dve", lambda e: e.tensor_tensor(out=E[:].rearrange("p (h q) -> p h q", h=4), in0=E[:].rearrange("p (h q) -> p h q", h=4),
                                                          in1=mask[:].unsqueeze(1).to_broadcast([128, 4, 128]), op=ALU.mult),
                         reads=[tE, C.t_const], writes=[tE])
                    Es.append((E, tE, kt))
                po, tpo = next_pb(C)
                first = True
                for hh in range(4):
                    for ii, (E, tE, kt) in enumerate(Es):
                        k.op("pe", lambda e: e.matmul(po[:, hh * 65:(hh + 1) * 65], lhsT=E[:, hh * 128:(hh + 1) * 128], rhs=Vaug[:, kt, g, :],
                                                      start=first, stop=(ii == 1), skip_group_check=True),
                             reads=[tE, t_V[kt]], writes=[tpo], inc=(hh == 3 and ii == 1))
                        first = False
                pov = po[:, 0:260].rearrange("p (h d) -> p h d", d=65)
                den, tden = C.rt["den"], C.t_rt
                k.op("dve", lambda e: e.tensor_tensor(out=den[:], in0=pov[:, :, 64], in1=esink[:, 4 * g:4 * g + 4, par], op=ALU.add),
                     reads=[tpo, C.t_wr], writes=[tden])
                k.op("dve", lambda e: e.reciprocal(out=den[:], in_=den[:]), reads=[tden], writes=[tden])
                k.op("dve", lambda e: e.tensor_tensor(out=otv[:, 4 * g:4 * g + 4, par, :], in0=pov[:, :, 0:64],
                                                      in1=den[:].unsqueeze(2).to_broadcast([128, 4, 64]), op=ALU.mult),
                     reads=[tpo, tden], writes=[t_otok[so]])
        for c0 in range(0, NCH, 4):
            pb, tp = next_pb(C)
            pbv = pb[:].bitcast(BF16)
            for jj in range(4):
                c = c0 + jj
                k.op("pe", lambda e: e.transpose(pbv[:, jj * 128:(jj + 1) * 128], ot[:, c * 128:(c + 1) * 128], C.ident_bf[:]),
                     reads=[t_otok[so], C.t_const], writes=[tp], inc=(jj == 3))
            th = j // 4
            k.op("act", lambda e: e.copy(out=C.hT[:, c0:c0 + 4, HALO0 + j * 128:HALO0 + (j + 1) * 128], in_=pbv[:, 0:512].rearrange("p (c q) -> p c q", c=4)),
                 reads=[tp], writes=[C.t_h[c0 + i][1 + th] for i in range(4)])
    wo = W["w_o"]
    blocks = [([(wo[:, b * WB:(b + 1) * WB], 0)], 2) for b in range(8)]

    def o_evac(bi, m, th, pb, tp):
        c = bi * 2 + m
        k.op("dve", lambda e: e.scalar_tensor_tensor(out=C.xT[:, c, tsl(th)], in0=pb[:], scalar=v[:, V_GP_T + c:V_GP_T + c + 1],
                                                     in1=C.xT[:, c, tsl(th)], op0=ALU.mult, op1=ALU.add),
             reads=[tp, C.t_vec, C.t_x[c][th]], writes=[C.t_x[c][th]])
    linear_fm(C, blocks, lambda kc, th: C.hT[:, kc, HALO0 + th * 512:HALO0 + (th + 1) * 512],
              lambda th: (lambda kc: [C.t_h[kc][1 + th]]), 2, o_evac)


def store_x(C, out_dram):
    k = C.k
    for t in range(8):
        s = t % 2
        for c0 in range(0, NCH, 4):
            pb, tp = next_pb(C)
            for jj in range(4):
                c = c0 + jj
                k.op("pe", lambda e: e.transpose(pb[:, jj * 128:(jj + 1) * 128], C.xT[:, c, t * 128:(t + 1) * 128], C.ident_f[:]),
                     reads=[C.t_x[c][t // 4], C.t_const], writes=[tp], inc=(jj == 3))
            k.op("act" if (c0 // 4) % 2 == 0 else "dve",
                 (lambda e: e.copy(out=C.stage[s][:, c0 * 128:(c0 + 4) * 128], in_=pb[:])) if (c0 // 4) % 2 == 0 else
                 (lambda e: e.tensor_copy(out=C.stage[s][:, c0 * 128:(c0 + 4) * 128], in_=pb[:])),
                 reads=[tp], writes=[C.t_stage[s]])
        k.dma("sp", out_dram[t * 128:(t + 1) * 128, :], C.stage[s], reads=[C.t_stage[s]], writes=[C.t_out])


def load_x(C, x_dram, has_halo):
    k = C.k
    v = C.vec
    n_tiles = 9 if has_halo else 8

    def dst(t, c0, pv, tp):
        if has_halo and t == 0:
            for jj in range(4):
                c = c0 + jj
                k.op("act", lambda e: e.activation(out=C.hT[:, c, 0:128], in_=pv[:, jj, :], func=AF.Identity,
                                                   scale=v[:, V_SC1_T + c:V_SC1_T + c + 1], bias=v[:, V_SH_T + c:V_SH_T + c + 1]),
                     reads=[tp, C.t_vec], writes=[C.t_h[c][0]])
        else:
            to = t - 1 if has_halo else t
            k.op("act", lambda e: e.copy(out=C.xT[:, c0:c0 + 4, to * 128:(to + 1) * 128], in_=pv),
                 reads=[tp], writes=[C.t_x[c0 + i][to // 4] for i in range(4)])
    load_x_transposed(C, x_dram, n_tiles, dst, C.stage, C.t_stage)
    for c in range(NCH):
        for th in range(2):
            k.op("dve",
                 lambda e: e.tensor_scalar(out=C.hT[:, c, C.HOFF + th * 512:C.HOFF + (th + 1) * 512], in0=C.xT[:, c, tsl(th)],
                                           scalar1=v[:, V_SC1_T + c:V_SC1_T + c + 1], scalar2=v[:, V_SH_T + c:V_SH_T + c + 1], op0=ALU.mult, op1=ALU.add),
                 reads=[C.t_x[c][th], C.t_vec], writes=[C.t_h[c][1 + th]])


def build_layer0():
    nc = bass.Bass("TRN2", target_bir_lowering=False)

    def din(name, shape, dt=F32):
        return nc.dram_tensor(name, list(shape), dt, kind="ExternalInput").ap()
    xin = din("xin", [NT + HALO0, D])
    pos = din("pos", [1, NT + HALO0], I32)
    vec_d = din("vec", [128, NV])
    ident = din("ident_f", [128, 128])
    masks = din("masks", [128, 4, 128])
    rows = din("rows", [128, 256 + 32 + 20])
    w_qkv = din("w_qkv", [D, 2560])
    w_o = din("w_o", [D, D])
    wr_d = din("wr", [D, 20])
    w1 = din("w1", [16, D, 512])
    w3 = din("w3", [16, D, 512])
    w2 = din("w2", [16, 512, D])
    xout = nc.dram_tensor("xout", [NT, D], F32, kind="ExternalOutput").ap()
    with ExitStack() as st:
        k = K(nc, st)
        C = setup_ctx(k, {"ident_f": ident})
        setup_region(C)
        C.t_out = Tok()
        C.mk = k.sb("mk", [128, 4, 128], BF16)
        k.dma("pool", C.mk[:], masks, writes=[C.t_const])
        C.mdiag, C.mprev, C.mprev0, C.rperm = C.mk[:, 0, :], C.mk[:, 1, :], C.mk[:, 2, :], C.mk[:, 3, :]
        C.rowsb = k.sb("rowsb", [128, 308], F32)
        C.bvrow = C.rowsb[:, 0:256]
        C.esink = k.sb("esink", [128, 32], F32)
        C.brow = C.rowsb[:, 288:308]
        k.dma("sp", C.rowsb[:], rows, writes=[C.t_wr])
        k.op("act", lambda e: e.activation(out=C.esink[:], in_=C.rowsb[:, 256:288], func=AF.Exp), reads=[C.t_wr], writes=[C.t_wr])
        k.dma("sp", C.wr[:], wr_d.rearrange("(kc p) n -> p kc n", p=128), writes=[C.t_wr])
        k.dma("sp", C.vec[:], vec_d, writes=[C.t_vec])
        derive_vec(C)
        load_x(C, xin, True)
        swa_layer(C, {"pos": pos, "w_qkv": w_qkv, "w_o": w_o})
        layer_norm_mod(C, C.vec, V_LN_T_G, V_LN_T_B, V_A_C, V_B_C, C.tmp, C.t_tmp, C.stat, C.t_stat)
        moe_layer(C, 0, C.vec, {"sc1": V_SC1_C, "sh": V_SH_C, "gp": V_GP_C, "w1": w1, "w3": w3, "w2": w2})
        layer_norm_mod(C, C.vec, V_LN_C_G, V_LN_C_B, None, None, C.tmp, C.t_tmp, C.stat, C.t_stat)
        store_x(C, xout)
        k.finish([C.t_out])
        print("layer0 program: %d instructions" % k.ninst, k.cnt)
    return nc


def fm(vv):
    return np.ascontiguousarray(np.asarray(vv, np.float32).reshape(16, 128).T)


def rope_consts():
    p = np.arange(128)
    d = p % 64
    invf = np.where(d < 16, ROPE_THETA ** (-(2.0 * (d % 8)) / 16.0), 0.0).astype(np.float32)
    m16 = (d < 16).astype(np.float32)
    om16 = 1.0 - m16
    sgn = np.where(d < 8, -1.0, np.where(d < 16, 1.0, 0.0)).astype(np.float32)
    return invf, m16, om16, sgn


def const_masks(first_quarter):
    kk = np.arange(128)[:, None]
    qq = np.arange(128)[None, :]
    mdiag = (kk <= qq).astype(np.float32)
    mprev = (kk > qq).astype(np.float32)
    mprev0 = np.zeros_like(mprev) if first_quarter else mprev
    rperm = np.zeros((128, 128), np.float32)
    for m in range(128):
        d = m % 64
        if d < 8:
            rperm[m + 8, m] = 1.0
        elif d < 16:
            rperm[m - 8, m] = 1.0
    return np.ascontiguousarray(np.stack([mdiag, mprev, mprev0, rperm], axis=1))


def layer0_inputs(inp, modT, core):
    b, r = core // 4, core % 4
    i = 0
    t0 = r * NT
    x = inp["x"][b]
    halo = x[t0 - HALO0:t0] if r > 0 else np.zeros((HALO0, D), np.float32)
    xin = np.ascontiguousarray(np.concatenate([halo, x[t0:t0 + NT]], axis=0))
    p = inp["positions"][b]
    ph = p[t0 - HALO0:t0] if r > 0 else np.zeros((HALO0,), np.int32)
    pos = np.ascontiguousarray(np.concatenate([ph, p[t0:t0 + NT]])[None, :].astype(np.int32))
    vec = np.zeros((128, NV), np.float32)
    vec[:, 0:96] = modT
    vec[:, V_LN_T_G:V_LN_T_G + 16] = fm(inp["ln_t_g"][i])
    vec[:, V_LN_T_B:V_LN_T_B + 16] = fm(inp["ln_t_b"][i])
    vec[:, V_LN_C_G:V_LN_C_G + 16] = fm(inp["ln_c_g"][i])
    vec[:, V_LN_C_B:V_LN_C_B + 16] = fm(inp["ln_c_b"][i])
    bq = inp["swa_b_qkv"][0]
    vec[:, V_BQ:V_BQ + 16] = fm(bq[0:2048])
    for g in range(4):
        bk = bq[2048 + g * 64:2048 + (g + 1) * 64]
        vec[:, V_BK + g] = np.concatenate([bk, bk])
    invf, m16, om16, sgn = rope_consts()
    vec[:, V_INVF], vec[:, V_M16], vec[:, V_OM16], vec[:, V_SGN] = invf, m16, om16, sgn
    rows = np.zeros((128, 308), np.float32)
    rows[:, 0:256] = bq[2304:2560][None, :]
    rows[:, 256:288] = inp["swa_sinks"][0][None, :]
    rows[:, 288:292] = inp["moe_b_group"][i][None, :]
    rows[:, 292:308] = inp["moe_b_router"][i][None, :]
    wr = np.ascontiguousarray(np.concatenate([inp["moe_w_group"][i], inp["moe_w_router"][i]], axis=1))
    return {"xin": xin, "pos": pos, "vec": vec, "ident_f": np.eye(128, dtype=np.float32), "masks": const_masks(r == 0),
            "rows": rows, "w_qkv": inp["swa_w_qkv"][0], "w_o": inp["swa_w_o"][0], "wr": wr,
            "w1": inp["moe_w1"][i], "w3": inp["moe_w3"][i], "w2": inp["moe_w2"][i]}


NSA_Q = 2048
KVW = 256
HALO1 = 512


def build_layer1a():
    nc = bass.Bass("TRN2", target_bir_lowering=False)

    def din(name, shape, dt=F32):
        return nc.dram_tensor(name, list(shape), dt, kind="ExternalInput").ap()
    xin = din("xin", [NT, D])
    pos = din("pos", [1, NT], I32)
    vec_d = din("vec", [128, NV])
    ident = din("ident_f", [128, 128])
    masks = din("masks", [128, 4, 128])
    w_in = din("w_in", [D, 3680])
    outs = {}
    for nm in ("kc_T", "vc_T", "ks_T", "kw_T"):
        outs[nm] = nc.dram_tensor(nm, [128, 4, NT], BF16, kind="ExternalOutput").ap()
    for nm in ("vs", "vw"):
        outs[nm] = nc.dram_tensor(nm, [128, 8, 256], BF16, kind="ExternalOutput").ap()
    with ExitStack() as st:
        k = K(nc, st)
        C = setup_ctx(k, {"ident_f": ident}, hoff=0)
        setup_region(C)
        C.t_out = Tok()
        C.mk = k.sb("mk", [128, 4, 128], BF16)
        k.dma("pool", C.mk[:], masks, writes=[C.t_const])
        C.rperm = C.mk[:, 3, :]
        k.dma("sp", C.vec[:], vec_d, writes=[C.t_vec])
        derive_vec(C)
        load_x(C, xin, False)
        v = C.vec
        f32v, bfv = C.f32v, C.bfv
        wk = {"i": 0,
              "qb": [bfv(4736, 512), bfv(5760, 512)], "tq": [Tok(), Tok()],
              "t1": [f32v(6784, 512), f32v(8832, 512)], "tt1": [Tok(), Tok()],
              "t2": [f32v(10880, 512), f32v(12928, 512)], "tt2": [Tok(), Tok()]}
        Tcos = f32v(16384, NT)
        Tsin = f32v(16384 + 4096, NT)
        t_trig = Tok()
        wk_i = C.big[:, 0:2, :].rearrange("p a t -> p (a t)").bitcast(I32)
        wk_f = C.big[:, 2:4, :].rearrange("p a t -> p (a t)").bitcast(F32)
        wk_a = C.big[:, 4:6, :].rearrange("p a t -> p (a t)").bitcast(F32)
        rope_tables(C, pos, NT, Tcos, Tsin, t_trig, wk_i, wk_f, wk_a)
        KO = {"kc_T": 0, "vc_T": 1, "ks_T": 2, "kw_T": 3}
        col0 = {"kc_T": 2048, "vc_T": 2304, "ks_T": 2560, "kw_T": 3072}
        stg = C.big[:, 0:16, :].rearrange("p (a g) t -> p a g t", g=4)
        t_stg = [[Tok() for _ in range(4)] for _ in range(4)]
        for a in range(4):
            for g in range(4):
                t_stg[a][g].w = t_trig.w
        kblocks = []
        order = []
        for nm in ("kc_T", "vc_T", "ks_T", "kw_T"):
            for b in range(2):
                srcs = []
                for m in range(2):
                    g = b * 2 + m
                    col = col0[nm] + g * 64
                    srcs.append((w_in[:, col:col + 64], m * 128))
                    srcs.append((w_in[:, col:col + 64], m * 128 + 64))
                kblocks.append((srcs, 2))
                order.append((nm, b))
        C.zero1 = k.sb("zero1", [128, 1], F32)
        k.op("dve", lambda e: e.memset(C.zero1[:], 0.0), writes=[C.t_vec])

        def evac(bi, m, th, pb, tp):
            nm, b = order[bi]
            g = b * 2 + m
            a = KO[nm]
            dst = stg[:, a, g, tsl(th)]
            if nm in ("ks_T", "kw_T"):
                cs = tsl(th)
                rope_evac(C, pb, tp, 512, C.zero1[:, 0:1], Tcos[:, cs], Tsin[:, cs], t_trig, dst, t_stg[a][g], wk)
            else:
                k.op("act", lambda e: e.copy(out=dst, in_=pb[:]), reads=[tp], writes=[t_stg[a][g]])
        linear_fm(C, kblocks, lambda kc, th: C.hT[:, kc, C.HOFF + th * 512:C.HOFF + (th + 1) * 512],
                  lambda th: (lambda kc: [C.t_h[kc][1 + th]]), 2, evac)
        for nm in ("kc_T", "vc_T", "ks_T", "kw_T"):
            a = KO[nm]
            k.dma("sp", outs[nm], stg[:, a, :, :], reads=t_stg[a], writes=[C.t_out])
        vst = [C.stage[0].bitcast(BF16)[:, 0:2048].rearrange("p (t n) -> p t n", t=8), C.stage[1].bitcast(BF16)[:, 0:2048].rearrange("p (t n) -> p t n", t=8)]
        for vi, (nm, col) in enumerate((("vs", 2816), ("vw", 3328))):
            vbuf, tvb = load_wblock(C, [(w_in[:, col:col + 256], 0)])
            for t in range(8):
                pb, tp = next_pb(C)
                for kc in range(NCH):
                    k.op("pe", lambda e: e.matmul(pb[:, 0:256], lhsT=C.hT[:, kc, C.HOFF + t * 128:C.HOFF + (t + 1) * 128], rhs=vbuf[:, kc, :], start=(kc == 0), stop=(kc == NCH - 1)),
                         reads=[tvb, C.t_h[kc][1], C.t_h[kc][2]], writes=[tp], inc=(kc == NCH - 1))
                k.op("act", lambda e: e.copy(out=vst[vi][:, t, :], in_=pb[:, 0:256]), reads=[tp], writes=[C.t_stage[vi]])
            k.dma("sp", outs[nm], vst[vi], reads=[C.t_stage[vi]], writes=[C.t_out])
        k.finish([C.t_out])
        print("layer1a program: %d instructions" % k.ninst, k.cnt)
    return nc


def layer1a_inputs(inp, modT, x1_core, core):
    b, r = core // 4, core % 4
    t0 = r * NT
    pos = np.ascontiguousarray(inp["positions"][b][t0:t0 + NT][None, :].astype(np.int32))
    vec = np.zeros((128, NV), np.float32)
    vec[:, 0:96] = modT
    invf, m16, om16, sgn = rope_consts()
    vec[:, V_INVF], vec[:, V_M16], vec[:, V_OM16], vec[:, V_SGN] = invf, m16, om16, sgn
    return {"xin": np.ascontiguousarray(x1_core), "pos": pos, "vec": vec, "ident_f": np.eye(128, dtype=np.float32),
            "masks": const_masks(False), "w_in": inp["nsa_w_in"][0]}


DBG = {'compress': True, 'cmp': True, 'sel': True, 'win': True, 'moe': True, 'glim': 4, 'jlim': 8}


def nsa_core(nc, k, C, A):
    v = C.vec
    pos, w_in, w_o, w1, w3, w2 = A.pos, A.w_in, A.w_o, A.w1, A.w3, A.w2
    phi_k1, phi_v1, causal, cvalid, sbias_d, xspill, xout = A.phi_k1, A.phi_v1, A.causal, A.cvalid, A.sbias_d, A.xspill, A.xout
    phi2_sb, peT_sb, ovl_sb, t_nc, kcT_all, vc_all, t_cmpkv = A.phi2_sb, A.peT_sb, A.ovl_sb, A.t_nc, A.kcT_all, A.vc_all, A.t_cmpkv
    kc_full = vc_full = None
    t_spill = Tok()
    allx = [C.t_x[c][th] for c in range(NCH) for th in range(2)]
    k.dma("sp", xspill, C.xT[:].rearrange("p c t -> p (c t)"), reads=allx, writes=[t_spill] + list(getattr(A, 'pre_toks', [])))
    X = C.xT[:].rearrange("p c t -> p (c t)").bitcast(BF16)

    def xbf(b0, n):
        return X[:, b0 // 2:b0 // 2 + n]

    def xf32(b0, n):
        return X[:, b0 // 2:b0 // 2 + 2 * n].bitcast(F32)
    xs_toks = []

    def xtok():
        t = Tok()
        t.w = t_spill.w
        xs_toks.append(t)
        return t
    q_raw = xbf(0, 4096).rearrange("p (c t) -> p c t", c=4)
    q_rot = xbf(8192, 4096).rearrange("p (c t) -> p c t", c=4)
    t_qraw = [[xtok(), xtok()] for _ in range(4)]
    t_qrot = [[xtok(), xtok()] for _ in range(4)]
    kvA = xbf(16384, 4096)
    t_kvA = xtok()
    Vs = xbf(24576, 32 * 65).rearrange("p (t d) -> p t d", d=65)
    t_Vs = xtok()
    Kw = xbf(28736, NT + HALO1)
    t_Kw = xtok()
    Vw = xbf(31808, 12 * 65).rearrange("p (t d) -> p t d", d=65)
    t_Vw = xtok()
    Tcos = xf32(34144, NT)
    Tsin = xf32(38240, NT)
    t_trig = xtok()
    wk = {"i": 0,
          "qb": [xbf(42336, 512), xbf(43360, 512)], "tq": [xtok(), xtok()],
          "t1": [xf32(44384, 512), xf32(46432, 512)], "tt1": [xtok(), xtok()],
          "t2": [xf32(48480, 512), xf32(50528, 512)], "tt2": [xtok(), xtok()]}
    selc = xbf(52576, 4096)
    t_selc = xtok()
    hid = xbf(60768, 512).rearrange("p (c n) -> p c n", c=2)
    t_hid = xtok()
    if kcT_all is None:
        kcT_all = xbf(61792, 1024).rearrange("p (g n) -> p g n", g=4)
        vc_all = xbf(63840, 520).rearrange("p (g nt d) -> p g nt d", g=4, nt=2)
        t_cmpkv = xtok()
    f32v, bfv = C.f32v, C.bfv
    wj = bfv(0, 4096)
    t_wj = Tok()
    otok = [bfv(8192, 512), bfv(9216, 512)]
    t_otok = [Tok(), Tok()]
    gates = f32v(16384, 768).rearrange("p (t n) -> p t n", t=8)
    t_gates = Tok()
    sbias = f32v(19456, 512).rearrange("p (j s) -> p j s", j=8)
    t_sb = Tok()
    cval = bfv(21504, 256).rearrange("p (nt t) -> p nt t", nt=2)
    t_cval = Tok()
    score = f32v(22016, 64)
    score2 = f32v(22272, 64)
    mx8 = f32v(22528, 8)
    selm = bfv(22592, 64)
    t_sc = Tok()
    acc = f32v(23552, 256).rearrange("p (h d) -> p h d", h=4)
    tacc = f32v(24576, 256).rearrange("p (h d) -> p h d", h=4)
    t_acc = Tok()
    fac = f32v(25600, 4)
    rcs = f32v(25632, 8)
    t_fac = Tok()
    impt = f32v(25664, 64)
    C.accP = [f32v(26624, 256).rearrange('p (h d) -> p h d', h=4), f32v(27648, 256).rearrange('p (h d) -> p h d', h=4)]
    C.t_accP = [Tok(), Tok()]
    oTt = [f32v(28672, 512), f32v(28672, 512)]
    t_o1 = Tok()
    t_oTt = [t_o1, t_o1]
    E6 = list(C.E) + [bfv(30720, 512), bfv(31744, 512)]
    tE6 = list(C.t_E) + [Tok(), Tok()]
    for t_ in C.t_accP + [t_o1] + tE6[4:]:
        t_.w = t_spill.w
    st6 = {"i": 0}

    def nextE():
        i_ = st6["i"]
        st6["i"] = (i_ + 1) % 6
        return E6[i_], tE6[i_]

    def back_to_token_major(pacc, tpacc, par):
        k.op("act", lambda e: e.copy(out=oTt[par][0:65, :], in_=pacc[0:65, :]), reads=[tpacc], writes=[t_oTt[par]])
        pt, tpt = npb()
        for hh in range(4):
            k.op("pe", lambda e: e.transpose(pt[:, hh * 65:(hh + 1) * 65], oTt[par][0:65, hh * 128:(hh + 1) * 128], C.ident_f[0:65, 0:65]),
                 reads=[t_oTt[par], C.t_const], writes=[tpt], inc=(hh == 3))
        return pt, tpt
    for t_ in (t_wj, t_otok[0], t_otok[1], t_gates, t_sb, t_cval, t_sc, t_acc, t_fac):
        t_.w = t_spill.w
    k.dma("sp", sbias, sbias_d.rearrange("p (j s) -> p j s", j=8), writes=[t_sb])
    rope_tables(C, pos, NT, Tcos, Tsin, t_trig, C.big[:, 0:2, :].rearrange("p a t -> p (a t)").bitcast(I32), C.big[:, 2:4, :].rearrange("p a t -> p (a t)").bitcast(F32), C.big[:, 4:6, :].rearrange("p a t -> p (a t)").bitcast(F32), pre=[C.t_big[c__][th__] for c__ in range(6) for th__ in range(2)])
    for c_ in range(6):
        for th_ in range(2):
            C.t_big[c_][th_].w = t_trig.w
    for tl_ in t_qraw + t_qrot:
        for t_ in tl_:
            t_.w = t_trig.w
    t_kvA.w = t_trig.w
    C.NPB = 6

    def npb():
        i = C.pbi % 4
        C.pbi = (i + 1) % 4
        return C.pb[i], C.t_pb[i]
    C.pbi = 0
    gbuf, tgb = load_wblock(C, [(w_in[:, 3584:3680], 0)])
    for t in range(8):
        pb, tp = npb()
        for kc in range(NCH):
            k.op("pe", lambda e: e.matmul(pb[:, 0:96], lhsT=C.hT[:, kc, C.HOFF + t * 128:C.HOFF + (t + 1) * 128], rhs=gbuf[:, kc, 0:96], start=(kc == 0), stop=(kc == NCH - 1)),
                 reads=[tgb, C.t_h[kc][1], C.t_h[kc][2]], writes=[tp], inc=(kc == NCH - 1))
        k.op("act", lambda e: e.activation(out=gates[:, t, :], in_=pb[:, 0:96], func=AF.Sigmoid), reads=[tp], writes=[t_gates])
    k.op("pool", lambda e: e.memset(kcT_all[:], 0.0), writes=[t_cmpkv])
    k.op("pool", lambda e: e.memset(vc_all[:], 1.0), writes=[t_cmpkv])
    k.op("pool", lambda e: e.memset(hid[:], 0.0), writes=[t_hid])
    kvP = kvA[:, 0:16 * 255].rearrange("p (jj n) -> p jj n", jj=16)
    for kv, (phi1_d, src_full, pecol) in enumerate(((phi_k1, None, 0), (phi_v1, None, 16)) if DBG['compress'] else ()):
        pbuf, tpw = load_wblock(C, [(phi1_d, 0)])
        for g in range(4):
            A.load_cmp_src(g, kv, kvA, t_kvA, selc, t_selc)
            if A.cmp_prepped:
                k.op("dve", lambda e: e.tensor_tensor(out=kvP, in0=kvP, in1=peT_sb[:, pecol:pecol + 16].unsqueeze(2).to_broadcast([128, 16, 255]), op=ALU.add),
                     reads=[t_kvA, t_nc], writes=[t_kvA])
            for hc in range(2):
                pb, tp = npb()
                for jj in range(16):
                    k.op("pe", lambda e: e.matmul(pb[:, 0:255], lhsT=pbuf[:, jj, hc * 128:(hc + 1) * 128], rhs=kvP[:, jj, :], start=(jj == 0), stop=(jj == 15)),
                         reads=[tpw, t_kvA], writes=[tp], inc=(jj == 15))
                k.op("act", lambda e: e.activation(out=hid[:, hc, 0:255], in_=pb[:, 0:255], func=AF.Silu), reads=[tp], writes=[t_hid])
            if kv == 0:
                pb, tp = npb()
                for hc in range(2):
                    k.op("pe", lambda e: e.matmul(pb[:, 0:255], lhsT=phi2_sb[:, hc, 0:128], rhs=hid[:, hc, 0:255], start=(hc == 0), stop=(hc == 1)),
                         reads=[t_nc, t_hid], writes=[tp], inc=(hc == 1))
                k.op("act", lambda e: e.copy(out=kcT_all[:, g, 0:255], in_=pb[:, 0:255]), reads=[tp], writes=[t_cmpkv])
            else:
                for nt in range(2):
                    pb, tp = npb()
                    for hc in range(2):
                        k.op("pe", lambda e: e.matmul(pb[:, 0:64], lhsT=hid[:, hc, nt * 128:(nt + 1) * 128], rhs=phi2_sb[:, hc, 128:192], start=(hc == 0), stop=(hc == 1)),
                             reads=[t_nc, t_hid], writes=[tp], inc=(hc == 1))
                    k.op("act", lambda e: e.copy(out=vc_all[:, g, nt, 0:64], in_=pb[:, 0:64]), reads=[tp], writes=[t_cmpkv])
    NEG = -1e30
    for g in range(DBG['glim']):
        blocks = [([(w_in[:, (4 * g + 2 * b) * 128:(4 * g + 2 * b + 2) * 128], 0)], 2) for b in range(2)]

        def q_evac(bi, m, th, pb, tp):
            cc = bi * 2 + m
            cs = tsl(th)
            qb = q_raw[:, cc, cs]
            tq = t_qraw[cc][th]
            k.op("act", lambda e: e.copy(out=qb, in_=pb[:]), reads=[tp], writes=[tq])
            pr, tpr = npb()
            k.op("pe", lambda e: e.matmul(pr[:], lhsT=C.rperm[:], rhs=qb, start=True, stop=True), reads=[tq, C.t_const], writes=[tpr])
            i = wk["i"]
            wk["i"] = (i + 1) % 2
            t1, t2 = wk["t1"][i], wk["t2"][i]
            k.op("dve", lambda e: e.tensor_tensor(out=t2, in0=pr[:], in1=Tsin[:, cs], op=ALU.mult), reads=[tpr, t_trig], writes=[wk["tt2"][i]])
            k.op("dve", lambda e: e.tensor_tensor(out=t1, in0=qb, in1=Tcos[:, cs], op=ALU.mult), reads=[tq, t_trig], writes=[wk["tt1"][i]])
            k.op("dve", lambda e: e.tensor_tensor(out=q_rot[:, cc, cs], in0=t1, in1=t2, op=ALU.add), reads=[wk["tt1"][i], wk["tt2"][i]], writes=[t_qrot[cc][th]])
        for bi, (srcs, nm_) in enumerate(blocks):
            buf, tw = load_wblock(C, srcs)
            for m in range(nm_):
                for th in range(2):
                    pb, tp = npb()
                    for kc in range(NCH):
                        k.op("pe", lambda e: e.matmul(pb[:], lhsT=buf[:, kc, m * 128:(m + 1) * 128], rhs=C.hT[:, kc, C.HOFF + th * 512:C.HOFF + (th + 1) * 512],
                                                      start=(kc == 0), stop=(kc == NCH - 1)),
                             reads=[tw, C.t_h[kc][1 + th]], writes=[tp], inc=(kc == NCH - 1))
                    q_evac(bi, m, th, pb, tp)
        A.load_kv(g, kvA, t_kvA, Vs, t_Vs, Kw, t_Kw, Vw, t_Vw)
        for j in range(DBG['jlim']):
            th = j // 4
            qs = slice(j * 128, (j + 1) * 128)
            if g == 0 or True:
                pass
            k.dma("sp", wj, causal[j], writes=[t_wj])
            k.dma("sp", cval, cvalid[j].rearrange("p (nt t) -> p nt t", nt=2), writes=[t_cval])
            so = (g * 8 + j) % 2
            ot = otok[so]
            otv = ot.rearrange("p (i two d) -> p i two d", two=2, d=64)
            gv = gates[:, j, :].rearrange("p (hh two i) -> p hh two i", two=2, i=3)
            pU, tpU = C.pb[5], C.t_pb[5]
            Ecs = {}
            for par in (range(2) if DBG['cmp'] else ()):
                ps_ = slice(par * 64, (par + 1) * 64)
                for nt in range(2):
                    pb, tp = npb()
                    k.op("pe", lambda e: e.matmul(pb[:].rearrange("p (h q) -> p h q", h=4), lhsT=kcT_all[ps_, g, nt * 128:(nt + 1) * 128],
                                                  rhs=q_raw[ps_, :, qs], start=True, stop=True),
                         reads=[t_cmpkv] + [t_qraw[c_][th] for c_ in range(4)], writes=[tp])
                    ei = C.Ei
                    C.Ei = (ei + 1) % 4
                    E, tE = C.E[ei], C.t_E[ei]
                    k.op("act", lambda e: e.activation(out=E[:], in_=pb[:], func=AF.Exp, scale=SCALE), reads=[tp], writes=[tE])
                    k.op("dve", lambda e: e.tensor_tensor(out=E[:].rearrange("p (h q) -> p h q", h=4), in0=E[:].rearrange("p (h q) -> p h q", h=4),
                                                          in1=cval[:, nt, :].unsqueeze(1).to_broadcast([128, 4, 128]), op=ALU.mult),
                         reads=[tE, t_cval], writes=[tE])
                    Ecs[(par, nt)] = (E, tE)
                po, tpo = npb()
                first = True
                for hh in range(4):
                    for nt in range(2):
                        E, tE = Ecs[(par, nt)]
                        k.op("pe", lambda e: e.matmul(po[:, hh * 65:(hh + 1) * 65], lhsT=E[:, hh * 128:(hh + 1) * 128], rhs=vc_all[:, g, nt, :],
                                                      start=first, stop=(nt == 1), skip_group_check=True),
                             reads=[tE, t_cmpkv], writes=[tpo], inc=(hh == 3 and nt == 1))
                        first = False
                for hh in range(4):
                    for nt in range(2):
                        E, tE = Ecs[(par, nt)]
                        col = (par * 4 + hh) * 64
                        k.op("pe", lambda e: e.matmul(pU[:, col:col + 64], lhsT=E[:, hh * 128:(hh + 1) * 128], rhs=ovl_sb[:, nt, :],
                                                      start=(par == 0 and hh == 0 and nt == 0), stop=(nt == 1), skip_group_check=True),
                             reads=[tE, t_nc], writes=[tpU], inc=(hh == 3 and nt == 1))
                pov = po[:, 0:260].rearrange("p (h d) -> p h d", d=65)
                rc = rcs[:, par * 4:par * 4 + 4]
                k.op("dve", lambda e: e.tensor_scalar(out=rc, in0=pov[:, :, 64], scalar1=1e-30, scalar2=None, op0=ALU.max), reads=[tpo], writes=[t_fac])
                k.op("dve", lambda e: e.reciprocal(out=rc, in_=rc), reads=[t_fac], writes=[t_fac])
                k.op("dve", lambda e: e.tensor_tensor(out=fac, in0=rc, in1=gv[:, 4 * g:4 * g + 4, par, 0], op=ALU.mult), reads=[t_fac, t_gates], writes=[t_fac])
                k.op("dve", lambda e: e.tensor_tensor(out=C.accP[par], in0=pov[:, :, 0:64], in1=fac.unsqueeze(2).to_broadcast([128, 4, 64]), op=ALU.mult),
                     reads=[tpo, t_fac], writes=[C.t_accP[par]])
            for h8 in (range(8) if DBG['cmp'] else ()):
                if h8 == 0:
                    k.op("dve", lambda e: e.tensor_scalar(out=impt, in0=pU[:, 0:64], scalar1=rcs[:, 0:1], scalar2=None, op0=ALU.mult), reads=[tpU, t_fac], writes=[t_sc])
                else:
                    k.op("dve", lambda e: e.scalar_tensor_tensor(out=impt, in0=pU[:, h8 * 64:(h8 + 1) * 64], scalar=rcs[:, h8:h8 + 1], in1=impt, op0=ALU.mult, op1=ALU.add),
                         reads=[tpU, t_fac, t_sc], writes=[t_sc])
            if DBG['cmp']:
              k.op("dve", lambda e: e.tensor_tensor(out=score, in0=impt, in1=sbias[:, j, :], op=ALU.add), reads=[t_sc, t_sb], writes=[t_sc])
            k.op("dve", lambda e: e.max(out=mx8, in_=score), reads=[t_sc], writes=[t_sc])
            k.op("dve", lambda e: e.match_replace(out=score2, in_to_replace=mx8, in_values=score, imm_value=-3e38), reads=[t_sc], writes=[t_sc])
            k.op("dve", lambda e: e.max(out=mx8, in_=score2), reads=[t_sc], writes=[t_sc])
            k.op("dve", lambda e: e.tensor_scalar(out=selm, in0=score, scalar1=mx8[:, 7:8], scalar2=None, op0=ALU.is_ge), reads=[t_sc], writes=[t_sc])
            k.op("dve", lambda e: e.tensor_tensor(out=selc.rearrange("p (s q) -> p s q", s=64), in0=wj.rearrange("p (s q) -> p s q", s=64),
                                                  in1=selm.unsqueeze(2).to_broadcast([128, 64, 64]), op=ALU.mult),
                 reads=[t_sc, t_wj], writes=[t_selc])
            pow_ = [(C.pb[6], C.t_pb[6]), (C.pb[7], C.t_pb[7])]
            pendw = []

            def wstage1(i5):
                kt = j + i5
                banks = [npb(), npb()]
                outs = []
                for par in range(2):
                    ps_ = slice(par * 64, (par + 1) * 64)
                    pb, tp = banks[par]
                    k.op("pe", lambda e: e.matmul(pb[:].rearrange("p (h q) -> p h q", h=4), lhsT=Kw[ps_, kt * 128:(kt + 1) * 128],
                                                  rhs=q_rot[ps_, :, qs], start=True, stop=True),
                         reads=[t_Kw] + [t_qrot[c_][th] for c_ in range(4)], writes=[tp])
                for par in range(2):
                    pb, tp = banks[par]
                    E, tE = nextE()
                    k.op("act", lambda e: e.activation(out=E[:], in_=pb[:], func=AF.Exp, scale=SCALE), reads=[tp], writes=[tE])
                    mask = C.mprev if i5 == 0 else (C.mdiag if i5 == 4 else None)
                    if mask is not None:
                        k.op("dve", lambda e: e.tensor_tensor(out=E[:].rearrange("p (h q) -> p h q", h=4), in0=E[:].rearrange("p (h q) -> p h q", h=4),
                                                              in1=mask.unsqueeze(1).to_broadcast([128, 4, 128]), op=ALU.mult),
                             reads=[tE, C.t_const], writes=[tE])
                    outs.append((E, tE, par, kt, i5))
                return outs

            def wstage2(outs):
                for (E, tE, par, kt, i5) in outs:
                    po, tpo = pow_[par]
                    k.op("pe", lambda e: e.matmul(po[0:65, :], lhsT=Vw[:, kt, :], rhs=E[:], start=(i5 == 0), stop=(i5 == 4)),
                         reads=[tE, t_Vw], writes=[tpo])
            for n_ in range(5 + 1):
                if n_ < 5:
                    pendw.append(wstage1(n_))
                if n_ >= 1:
                    wstage2(pendw[n_ - 1])
            for par in range(2):
                po, tpo = back_to_token_major(pow_[par][0], pow_[par][1], par)
                pov = po[:, 0:260].rearrange("p (h d) -> p h d", d=65)
                k.op("dve", lambda e: e.reciprocal(out=fac, in_=pov[:, :, 64]), reads=[tpo], writes=[t_fac])
                k.op("dve", lambda e: e.tensor_tensor(out=fac, in0=fac, in1=gv[:, 4 * g:4 * g + 4, par, 2], op=ALU.mult), reads=[t_fac, t_gates], writes=[t_fac])
                k.op("dve", lambda e: e.tensor_tensor(out=tacc, in0=pov[:, :, 0:64], in1=fac.unsqueeze(2).to_broadcast([128, 4, 64]), op=ALU.mult),
                     reads=[tpo, t_fac], writes=[t_acc])
                k.op("dve", lambda e: e.tensor_tensor(out=C.accP[par], in0=C.accP[par], in1=tacc, op=ALU.add), reads=[t_acc, C.t_accP[par]], writes=[C.t_accP[par]])
            posel = [C.pb[6], C.pb[7]]
            tposel = [C.t_pb[6], C.t_pb[7]]
            firsts = [True, True]
            LOOK = 2
            kg_max = (24 + j) // 4
            last_kt = kg_max * 4 + 3
            blocks = [(kg, kk) for kg in range(kg_max + 1) for kk in range(4)]
            pend = []
            pm_state = {}

            def stage1(kg, kk):
                if kk == 0:
                    bi_ = 4 + (kg % 2)
                    pm, tpm = C.pb[bi_], C.t_pb[bi_]
                    pmv_ = pm[:].bitcast(BF16)
                    for k4 in range(4):
                        kt_ = kg * 4 + k4
                        k.op("pe", lambda e: e.transpose(pmv_[:, k4 * 128:(k4 + 1) * 128], selc[:, kt_ * 128:(kt_ + 1) * 128], C.ident_bf[:]),
                             reads=[t_selc, C.t_const], writes=[tpm], inc=(k4 == 3))
                    pm_state[kg] = (pmv_, tpm)
                pmv, tpm = pm_state[kg]
                kt = kg * 4 + kk
                outs = []
                banks = [npb(), npb()]
                for par in range(2):
                    ps_ = slice(par * 64, (par + 1) * 64)
                    pb, tp = banks[par]
                    k.op("pe", lambda e: e.matmul(pb[:].rearrange("p (h q) -> p h q", h=4), lhsT=kvA[ps_, kt * 128:(kt + 1) * 128],
                                                  rhs=q_rot[ps_, :, qs], start=True, stop=True),
                         reads=[t_kvA] + [t_qrot[c_][th] for c_ in range(4)], writes=[tp])
                for par in range(2):
                    pb, tp = banks[par]
                    E, tE = nextE()
                    k.op("act", lambda e: e.activation(out=E[:], in_=pb[:], func=AF.Exp, scale=SCALE), reads=[tp], writes=[tE])
                    k.op("dve", lambda e: e.tensor_tensor(out=E[:].rearrange("p (h q) -> p h q", h=4), in0=E[:].rearrange("p (h q) -> p h q", h=4),
                                                          in1=pmv[:, kk * 128:(kk + 1) * 128].unsqueeze(1).to_broadcast([128, 4, 128]), op=ALU.mult),
                         reads=[tE, tpm], writes=[tE])
                    outs.append((E, tE, par, kt))
                return outs

            def stage2(outs):
                for (E, tE, par, kt) in outs:
                    k.op("pe", lambda e: e.matmul(posel[par][0:65, :], lhsT=Vs[:, kt, :], rhs=E[:], start=firsts[par], stop=(kt == last_kt)),
                         reads=[tE, t_Vs], writes=[tposel[par]])
                    firsts[par] = False
            for n_ in range(len(blocks) + LOOK):
                if n_ < len(blocks):
                    pend.append(stage1(*blocks[n_]))
                if n_ >= LOOK:
                    stage2(pend[n_ - LOOK])
            for par in range(2):
                pt, tpt = back_to_token_major(posel[par], tposel[par], par)
                pov = pt[:, 0:260].rearrange("p (h d) -> p h d", d=65)
                k.op("dve", lambda e: e.reciprocal(out=fac, in_=pov[:, :, 64]), reads=[tpt], writes=[t_fac])
                k.op("dve", lambda e: e.tensor_tensor(out=fac, in0=fac, in1=gv[:, 4 * g:4 * g + 4, par, 1], op=ALU.mult), reads=[t_fac, t_gates], writes=[t_fac])
                k.op("dve", lambda e: e.tensor_tensor(out=tacc, in0=pov[:, :, 0:64], in1=fac.unsqueeze(2).to_broadcast([128, 4, 64]), op=ALU.mult),
                     reads=[tpt, t_fac], writes=[t_acc])
                k.op("dve", lambda e: e.tensor_tensor(out=otv[:, :, par, :], in0=C.accP[par], in1=tacc, op=ALU.add),
                     reads=[t_acc, C.t_accP[par]], writes=[t_otok[so]])
            pb, tp = npb()
            pbv = pb[:].bitcast(BF16)
            for jj in range(4):
                k.op("pe", lambda e: e.transpose(pbv[:, jj * 128:(jj + 1) * 128], ot[:, jj * 128:(jj + 1) * 128], C.ident_bf[:]),
                     reads=[t_otok[so], C.t_const], writes=[tp], inc=(jj == 3))
            k.op("act", lambda e: e.copy(out=C.big[:, 4 * g:4 * g + 4, qs], in_=pbv[:, 0:512].rearrange("p (c q) -> p c q", c=4)),
                 reads=[tp], writes=[C.t_big[4 * g + i][th] for i in range(4)])
    if DBG.get('dump'):
        dbg_o = nc.dram_tensor('dbg_oT', [128, NCH * NT], BF16, kind='ExternalOutput').ap()
        k.dma('sp', dbg_o, C.big[:].rearrange('p c t -> p (c t)'), reads=[C.t_big[c_][th_] for c_ in range(NCH) for th_ in range(2)], writes=[C.t_out])
    k.dma("sp", C.xT[:].rearrange("p c t -> p (c t)"), xspill, reads=[t_spill], writes=xs_toks + allx + [t_spill])
    C.pbi = 0
    blocks = [([(w_o[:, b * WB:(b + 1) * WB], 0)], 2) for b in range(8)]

    def o_evac(bi, m, th, pb, tp):
        c = bi * 2 + m
        k.op("dve", lambda e: e.scalar_tensor_tensor(out=C.xT[:, c, tsl(th)], in0=pb[:], scalar=v[:, V_GP_T + c:V_GP_T + c + 1],
                                                     in1=C.xT[:, c, tsl(th)], op0=ALU.mult, op1=ALU.add),
             reads=[tp, C.t_vec, C.t_x[c][th]], writes=[C.t_x[c][th]])
    linear_fm(C, blocks, lambda kc, th: C.big[:, kc, tsl(th)], lambda th: (lambda kc: [C.t_big[kc][th]]), 2, o_evac)
    layer_norm_mod(C, C.vec, V_LN_T_G, V_LN_T_B, V_A_C, V_B_C, C.tmp, C.t_tmp, C.stat, C.t_stat)
    if DBG['moe']:
        moe_layer(C, 1, C.vec, {"sc1": V_SC1_C, "sh": V_SH_C, "gp": V_GP_C, "w1": w1, "w3": w3, "w2": w2})
    layer_norm_mod(C, C.vec, V_LN_C_G, V_LN_C_B, None, None, C.tmp, C.t_tmp, C.stat, C.t_stat)
    store_x(C, xout)


def build_layer1b():
    nc = bass.Bass("TRN2", target_bir_lowering=False)

    def din(name, shape, dt=F32):
        return nc.dram_tensor(name, list(shape), dt, kind="ExternalInput").ap()
    xin = din("xin", [NT, D])
    pos = din("pos", [1, NT], I32)
    vec_d = din("vec", [128, NV])
    ident = din("ident_f", [128, 128])
    masks = din("masks", [128, 4, 128])
    rows = din("rows", [128, 308])
    w_in = din("w_in", [D, 3680])
    w_o = din("w_o", [D, D])
    wr_d = din("wr", [D, 20])
    w1 = din("w1", [16, D, 512])
    w3 = din("w3", [16, D, 512])
    w2 = din("w2", [16, 512, D])
    kc_full = din("kc_full", [4, 128, 16 * 255], BF16)
    vc_full = din("vc_full", [4, 128, 16 * 255], BF16)
    ks_full = din("ks_full", [4, 128, 4096], BF16)
    vs_aug = din("vs_aug", [4, 128, 32 * 65], BF16)
    kw_core = din("kw_core", [4, 128, NT + HALO1], BF16)
    vw_aug = din("vw_aug", [4, 128, 12 * 65], BF16)
    phi_k1 = din("phi_k1", [D, 256])
    phi_v1 = din("phi_v1", [D, 256])
    phi2 = din("phi2", [128, 2, 192])
    peT = din("peT", [128, 32])
    causal = din("causal", [8, 128, 4096], BF16)
    cvalid = din("cvalid", [8, 128, 256], BF16)
    sbias_d = din("sbias", [128, 8 * 64])
    ovl = din("ovl", [128, 2, 64])
    xspill = nc.dram_tensor("xspill", [128, NCH * NT], F32, kind="Internal").ap()
    xout = nc.dram_tensor("xout", [NT, D], F32, kind="ExternalOutput").ap()
    with ExitStack() as st:
        k = K(nc, st)
        C = setup_ctx(k, {"ident_f": ident}, hoff=0)
        setup_region(C)
        C.t_out = Tok()
        C.mk = k.sb("mk", [128, 4, 128], BF16)
        k.dma("pool", C.mk[:], masks, writes=[C.t_const])
        C.mdiag, C.mprev, C.rperm = C.mk[:, 0, :], C.mk[:, 1, :], C.mk[:, 3, :]
        C.rowsb = k.sb("rowsb", [128, 308], F32)
        C.brow = C.rowsb[:, 288:308]
        k.dma("sp", C.rowsb[:], rows, writes=[C.t_wr])
        k.dma("sp", C.wr[:], wr_d.rearrange("(kc p) n -> p kc n", p=128), writes=[C.t_wr])
        k.dma("sp", C.vec[:], vec_d, writes=[C.t_vec])
        derive_vec(C)
        C.zero1 = k.sb("zero1", [128, 1], F32)
        k.op("dve", lambda e: e.memset(C.zero1[:], 0.0), writes=[C.t_vec])
        phi2_sb = k.sb("phi2_sb", [128, 2, 192], BF16)
        peT_sb = k.sb("peT_sb", [128, 32], BF16)
        ovl_sb = k.sb("ovl_sb", [128, 2, 64], BF16)
        t_nc = Tok()
        k.dma("pool", phi2_sb[:], phi2, writes=[t_nc])
        k.dma("pool", peT_sb[:], peT, writes=[t_nc])
        k.dma("pool", ovl_sb[:], ovl, writes=[t_nc])
        kcT_all = k.sb("kcT_all", [128, 4, 256], BF16)
        vc_all = k.sb("vc_all", [128, 4, 2, 65], BF16)
        t_cmpkv = Tok()
        v = C.vec
        load_x(C, xin, False)
        A = Ctx()
        A.pos, A.w_in, A.w_o, A.w1, A.w3, A.w2 = pos, w_in, w_o, w1, w3, w2
        A.phi_k1, A.phi_v1, A.causal, A.cvalid, A.sbias_d, A.xspill, A.xout = phi_k1, phi_v1, causal, cvalid, sbias_d, xspill, xout
        A.phi2_sb, A.peT_sb, A.ovl_sb, A.t_nc, A.kcT_all, A.vc_all, A.t_cmpkv = phi2_sb, peT_sb, ovl_sb, t_nc, kcT_all, vc_all, t_cmpkv

        def load_cmp_src(g, kv, kvA, t_kvA, selc, t_selc):
            k.dma("sp", kvA[:, 0:16 * 255], (kc_full, vc_full)[kv][g], writes=[t_kvA])

        def load_kv(g, kvA, t_kvA, Vs, t_Vs, Kw, t_Kw, Vw, t_Vw):
            k.dma("sp", kvA, ks_full[g], writes=[t_kvA])
            k.dma("sp", Vs.rearrange("p t d -> p (t d)"), vs_aug[g], writes=[t_Vs])
            k.dma("sp", Kw, kw_core[g], writes=[t_Kw])
            k.dma("sp", Vw.rearrange("p t d -> p (t d)"), vw_aug[g], writes=[t_Vw])
        A.load_cmp_src, A.load_kv = load_cmp_src, load_kv
        A.cmp_prepped = True
        nsa_core(nc, k, C, A)
        k.finish([C.t_out])
        print("layer1b program: %d instructions" % k.ninst, k.cnt)
    return nc


BF = ml_dtypes.bfloat16


def layer1b_consts(r):
    tg = (1024 * r + np.arange(1024)).reshape(8, 128)
    keys = np.arange(4096)
    causal = (keys[None, None, :] <= tg[:, :, None]).astype(BF)
    n = np.arange(256)
    cend = 16 * n + 31
    valid = (cend[None, :, None] <= tg[:, None, :])
    valid[:, 255, :] = False
    cvalid = np.ascontiguousarray(valid.reshape(8, 2, 128, 128).transpose(0, 2, 1, 3).reshape(8, 128, 256)).astype(BF)
    s = np.arange(64)
    cur = tg // 64
    caus = (s[None, None, :] * 64 <= tg[:, :, None])
    forced = (s[None, None, :] == 0) | (s[None, None, :] == cur[:, :, None]) | (s[None, None, :] == cur[:, :, None] - 1)
    sb = np.where(caus, np.where(forced, 1e4, 0.0), -1e30).astype(np.float32)
    sbias = np.ascontiguousarray(sb.transpose(1, 0, 2).reshape(128, 512))
    c0 = n[:, None] * 16
    s0 = s[None, :] * 64
    ov = np.clip(np.minimum(c0 + 32, s0 + 64) - np.maximum(c0, s0), 0, None).astype(np.float32) / 32.0
    ov[255] = 0.0
    ovl = np.ascontiguousarray(ov.reshape(2, 128, 64).transpose(1, 0, 2))
    return causal, cvalid, sbias, ovl


def layer1b_inputs(inp, modT, x1_core, core, kvb):
    b, r = core // 4, core % 4
    i = 1
    t0 = r * NT
    pos = np.ascontiguousarray(inp["positions"][b][t0:t0 + NT][None, :].astype(np.int32))
    vec = np.zeros((128, NV), np.float32)
    vec[:, 0:96] = modT
    vec[:, V_LN_T_G:V_LN_T_G + 16] = fm(inp["ln_t_g"][i])
    vec[:, V_LN_T_B:V_LN_T_B + 16] = fm(inp["ln_t_b"][i])
    vec[:, V_LN_C_G:V_LN_C_G + 16] = fm(inp["ln_c_g"][i])
    vec[:, V_LN_C_B:V_LN_C_B + 16] = fm(inp["ln_c_b"][i])
    invf, m16, om16, sgn = rope_consts()
    vec[:, V_INVF], vec[:, V_M16], vec[:, V_OM16], vec[:, V_SGN] = invf, m16, om16, sgn
    rows = np.zeros((128, 308), np.float32)
    rows[:, 288:292] = inp["moe_b_group"][i][None, :]
    rows[:, 292:308] = inp["moe_b_router"][i][None, :]
    wr = np.ascontiguousarray(np.concatenate([inp["moe_w_group"][i], inp["moe_w_router"][i]], axis=1))
    causal, cvalid, sbias, ovl = layer1b_consts(r)
    kw_full = kvb["kw_full"]
    kw_core = np.zeros((4, 128, NT + HALO1), BF)
    lo = t0 - HALO1
    if lo >= 0:
        kw_core[:] = kw_full[:, :, lo:t0 + NT]
    else:
        kw_core[:, :, -lo:] = kw_full[:, :, 0:t0 + NT]
    vw_full = kvb["vw_aug_full"]
    vw = np.zeros((4, 128, 12, 65), BF)
    tlo = 8 * r - 4
    if tlo >= 0:
        vw[:] = vw_full[:, :, tlo:tlo + 12, :]
    else:
        vw[:, :, -tlo:, :] = vw_full[:, :, 0:tlo + 12, :]
    k2, v2 = inp["nsa_phi_k2"][0], inp["nsa_phi_v2"][0]
    phi2 = np.zeros((128, 2, 192), np.float32)
    for hc in range(2):
        phi2[:, hc, 0:64] = k2[hc * 128:(hc + 1) * 128]
        phi2[:, hc, 64:128] = k2[hc * 128:(hc + 1) * 128]
        phi2[:, hc, 128:192] = v2[hc * 128:(hc + 1) * 128]
    peT = np.zeros((128, 32), np.float32)
    pk, pv = inp["nsa_pe_k"][0], inp["nsa_pe_v"][0]
    peT[0:64, 0:16] = pk[0::2].T
    peT[64:128, 0:16] = pk[1::2].T
    peT[0:64, 16:32] = pv[0::2].T
    peT[64:128, 16:32] = pv[1::2].T
    return {"xin": np.ascontiguousarray(x1_core), "pos": pos, "vec": vec, "ident_f": np.eye(128, dtype=np.float32),
            "masks": const_masks(False), "rows": rows, "w_in": inp["nsa_w_in"][0], "w_o": inp["nsa_w_o"][0], "wr": wr,
            "w1": inp["moe_w1"][i], "w3": inp["moe_w3"][i], "w2": inp["moe_w2"][i],
            "kc_full": kvb["kc_full"], "vc_full": kvb["vc_full"], "ks_full": kvb["ks_full"],
            "vs_aug": np.ascontiguousarray(kvb["vs_aug_full"].reshape(4, 128, 32 * 65)),
            "kw_core": kw_core, "vw_aug": np.ascontiguousarray(vw.reshape(4, 128, 12 * 65)),
            "phi_k1": inp["nsa_phi_k1"][0], "phi_v1": inp["nsa_phi_v1"][0], "phi2": phi2, "peT": peT,
            "causal": causal, "cvalid": cvalid, "sbias": sbias, "ovl": ovl}


def assemble_kv(resA, b):
    cores = [resA[b * 4 + r] for r in range(4)]

    def catT(nm):
        full = np.concatenate([np.asarray(c[nm]) for c in cores], axis=2)
        return np.ascontiguousarray(full.transpose(1, 0, 2))

    def vaug(nm):
        full = np.concatenate([np.asarray(c[nm]) for c in cores], axis=1)
        full = full.reshape(128, 32, 4, 64)
        aug = np.ones((128, 32, 4, 65), BF)
        aug[:, :, :, 0:64] = full
        return np.ascontiguousarray(aug.transpose(2, 0, 1, 3))
    def perm(a):
        out = np.zeros((4, 128, 16, 255), BF)
        n16 = 16 * np.arange(255)
        for jj in range(16):
            out[:, 0:64, jj, :] = a[:, 0:64, :][:, :, n16 + 2 * jj]
            out[:, 64:128, jj, :] = a[:, 64:128, :][:, :, n16 + 2 * jj + 1]
        return np.ascontiguousarray(out.reshape(4, 128, 16 * 255))
    return {"kc_full": perm(catT("kc_T")), "vc_full": perm(catT("vc_T")), "ks_full": catT("ks_T"), "kw_full": catT("kw_T"),
            "vs_aug_full": vaug("vs"), "vw_aug_full": vaug("vw")}


def build_mod():
    nc = bass.Bass("TRN2", target_bir_lowering=False)
    cT = nc.dram_tensor("cT", [128, 16, 2], F32, kind="ExternalInput").ap()
    w = nc.dram_tensor("w", [2048, 3072], F32, kind="ExternalInput").ap()
    b = nc.dram_tensor("b", [1, 3072], F32, kind="ExternalInput").ap()
    out = nc.dram_tensor("out", [2, 3072], F32, kind="ExternalOutput").ap()
    with ExitStack() as st:
        k = K(nc, st)
        c_sb = k.sb("c_sb", [128, 16, 2], F32)
        t_c = Tok()
        ca = k.sb("ca", [128, 16, 2], F32)
        t_ca = Tok()
        wb = [k.sb("wb%d" % i, [128, 16, 512], F32) for i in range(2)]
        t_wb = [Tok(), Tok()]
        bb = k.sb("bb", [1, 3072], F32)
        t_bb = Tok()
        ones = k.sb("ones", [1, 2], F32)
        t_ones = Tok()
        res = k.sb("res", [2, 3072], F32)
        t_res = Tok()
        pb = [k.ps("pb%d" % i, [128, 512], F32) for i in range(2)]
        t_pb = [Tok(), Tok()]
        k.dma("sp", c_sb[:], cT, writes=[t_c])
        k.dma("sp", bb[:], b, writes=[t_bb])
        k.op("dve", lambda e: e.memset(ones[:], 1.0), writes=[t_ones])
        k.op("act", lambda e: e.activation(out=ca[:], in_=c_sb[:], func=AF.Silu), reads=[t_c], writes=[t_ca])
        wv = w.rearrange("(kc p) n -> p kc n", p=128)
        for j in range(6):
            i = j % 2
            k.dma("sp", wb[i][:], wv[:, :, j * 512:(j + 1) * 512], writes=[t_wb[i]])
            for kc in range(16):
                k.op("pe", lambda e: e.matmul(pb[i][0:2, :], lhsT=ca[:, kc, :], rhs=wb[i][:, kc, :], start=(kc == 0), stop=False),
                     reads=[t_ca, t_wb[i]], writes=[t_pb[i]])
            k.op("pe", lambda e: e.matmul(pb[i][0:2, :], lhsT=ones[:], rhs=bb[:, j * 512:(j + 1) * 512], start=False, stop=True),
                 reads=[t_ones, t_bb], writes=[t_pb[i]])
            k.op("dve", lambda e: e.tensor_copy(out=res[:, j * 512:(j + 1) * 512], in_=pb[i][0:2, :]), reads=[t_pb[i]], writes=[t_res])
        k.dma("sp", out, res[:], reads=[t_res], writes=[t_res])
        k.finish([t_res])
    return nc


def kernel_unfused(**inp):
    inp = {kk: np.asarray(vv) for kk, vv in inp.items()}
    cores = list(range(8))
    c = inp["c"].astype(np.float32)
    wcat = np.concatenate([inp["w_ada"][0], inp["w_ada"][1]], axis=1)
    bcat = np.concatenate([inp["b_ada"][0], inp["b_ada"][1]], axis=0)
    cT = np.ascontiguousarray(c.T.reshape(16, 128, 2).transpose(1, 0, 2))
    in_maps = [{"cT": cT, "w": np.ascontiguousarray(wcat[:, i * 3072:(i + 1) * 3072]),
                "b": np.ascontiguousarray(bcat[None, i * 3072:(i + 1) * 3072])} for i in cores]
    res = run_bass_kernel_spmd(build_mod(), in_maps, core_ids=cores)
    mod = np.concatenate([np.asarray(r["out"]) for r in res.results], axis=1)

    def modT(b, layer):
        return np.ascontiguousarray(mod[b, layer * 12288:(layer + 1) * 12288].reshape(96, 128).T)
    in_maps = [layer0_inputs(inp, modT(cc // 4, 0), cc) for cc in cores]
    res = run_bass_kernel_spmd(build_layer0(), in_maps, core_ids=cores)
    x1 = [np.asarray(r["xout"]) for r in res.results]
    in_maps = [layer1a_inputs(inp, modT(cc // 4, 1), x1[cc], cc) for cc in cores]
    res = run_bass_kernel_spmd(build_layer1a(), in_maps, core_ids=cores)
    resA = [{kk: np.asarray(vv) for kk, vv in r.items()} for r in res.results]
    kvbs = [assemble_kv(resA, b) for b in range(2)]
    in_maps = [layer1b_inputs(inp, modT(cc // 4, 1), x1[cc], cc, kvbs[cc // 4]) for cc in cores]
    res = run_bass_kernel_spmd(build_layer1b(), in_maps, core_ids=cores)
    x2 = np.stack([np.asarray(r["xout"]) for r in res.results])
    return np.ascontiguousarray(x2.reshape(2, 4096, 2048)).astype(np.float32)


GW = 16384 + 2 * 2080
GROUPS = [[0, 1, 2, 3], [4, 5, 6, 7]]


def emit_collective(k, nc, kind, src, dst, reads, writes, name):
    k._deps("pool", reads, writes)
    ins = nc.gpsimd.collective_compute(kind, ALU.bypass, replica_groups=GROUPS, ins=[src], outs=[dst])
    sem = k.stack.enter_context(nc.semaphore(name))
    ins.then_inc(sem)
    k._mark(sem, 1, reads, writes)
    k.ninst += 1


def build_fused():
    nc = bass.Bass("TRN2", target_bir_lowering=False)

    def din(name, shape, dt=F32):
        return nc.dram_tensor(name, list(shape), dt, kind="ExternalInput").ap()
    xin = din("xin", [NT + HALO0, D])
    pos = din("pos", [1, NT + HALO0], I32)
    vec0_d = din("vec0", [128, NV])
    vec1_d = din("vec1", [128, NV])
    ident = din("ident_f", [128, 128])
    masks = din("masks", [128, 4, 128])
    rows0 = din("rows0", [128, 308])
    rows1 = din("rows1", [128, 308])
    cT = din("cT", [128, 16, 2])
    wada = din("wada", [D, 6144])
    bada = din("bada", [1, 6144])
    w_qkv = din("w_qkv", [D, 2560])
    w_o0 = din("w_o0", [D, D])
    wr0 = din("wr0", [D, 20])
    wr1 = din("wr1", [D, 20])
    w1 = din("w1", [2, 16, D, 512])
    w3 = din("w3", [2, 16, D, 512])
    w2 = din("w2", [2, 16, 512, D])
    w_in = din("w_in", [D, 3680])
    w_o1 = din("w_o1", [D, D])
    phi_k1 = din("phi_k1", [D, 256])
    phi_v1 = din("phi_v1", [D, 256])
    phi2 = din("phi2", [128, 2, 192])
    peT = din("peT", [128, 32])
    causal = din("causal", [8, 128, 4096], BF16)
    cvalid = din("cvalid", [8, 128, 256], BF16)
    sbias_d = din("sbias", [128, 8 * 64])
    ovl = din("ovl", [128, 2, 64])
    oneh_d = din("oneh", [128, 8])
    xspill = nc.dram_tensor("xspill", [128, NCH * NT], F32).ap()
    mod_src = nc.dram_tensor("mod_src", [2, 6144], F32).ap()
    mod_dst = nc.dram_tensor("mod_dst", [8, 6144], F32).ap()
    kv_src = [nc.dram_tensor("kv_src%d" % i, [128, 2048 if i < 8 else 1040], BF16).ap() for i in range(12)]
    kv_dst = [nc.dram_tensor("kv_dst%d" % i, [512, 2048 if i < 8 else 1040], BF16).ap() for i in range(12)]
    xout = nc.dram_tensor("xout", [NT, D], F32, kind="ExternalOutput").ap()
    with ExitStack() as st:
        k = K(nc, st)
        C = setup_ctx(k, {"ident_f": ident})
        setup_region(C)
        C.t_out = Tok()
        vec0 = C.vec
        vec1 = k.sb("vec1", [128, NV], F32)
        C.mk = k.sb("mk", [128, 4, 128], BF16)
        k.dma("pool", C.mk[:], masks, writes=[C.t_const])
        C.mdiag, C.mprev, C.mprev0, C.rperm = C.mk[:, 0, :], C.mk[:, 1, :], C.mk[:, 2, :], C.mk[:, 3, :]
        C.rowsb = k.sb("rowsb", [128, 308], F32)
        C.bvrow = C.rowsb[:, 0:256]
        C.esink = k.sb("esink", [128, 32], F32)
        C.brow = C.rowsb[:, 288:308]
        k.dma("sp", C.rowsb[:], rows0, writes=[C.t_wr])
        k.op("act", lambda e: e.activation(out=C.esink[:], in_=C.rowsb[:, 256:288], func=AF.Exp), reads=[C.t_wr], writes=[C.t_wr])
        k.dma("sp", C.wr[:], wr0.rearrange("(kc p) n -> p kc n", p=128), writes=[C.t_wr])
        k.dma("sp", vec0[:], vec0_d, writes=[C.t_vec])
        k.dma("sp", vec1[:], vec1_d, writes=[C.t_vec])
        C.zero1 = k.sb("zero1", [128, 1], F32)
        k.op("dve", lambda e: e.memset(C.zero1[:], 0.0), writes=[C.t_vec])
        oneh = k.sb("oneh", [128, 8], F32)
        phi2_sb = k.sb("phi2_sb", [128, 2, 192], BF16)
        peT_sb = k.sb("peT_sb", [128, 32], F32)
        ovl_sb = k.sb("ovl_sb", [128, 2, 64], BF16)
        t_nc = Tok()
        k.dma("sp", oneh[:], oneh_d, writes=[t_nc])
        k.dma("pool", phi2_sb[:], phi2, writes=[t_nc])
        k.dma("sp", peT_sb[:], peT, writes=[t_nc])
        k.dma("pool", ovl_sb[:], ovl, writes=[t_nc])
        kcT_all = vc_all = t_cmpkv = None
        c_sb = k.sb("c_sb", [128, 16, 2], F32)
        t_c = Tok()
        k.dma("sp", c_sb[:], cT, writes=[t_c])
        k.op("act", lambda e: e.activation(out=c_sb[:], in_=c_sb[:], func=AF.Silu), reads=[t_c], writes=[t_c])
        wst = [C.big[:].rearrange("p c t -> p (c t)").bitcast(F32).rearrange("p (kc n) -> p kc n", kc=16),
               C.hT[:].rearrange("p c t -> p (c t)").bitcast(F32)[:, 0:8192].rearrange("p (kc n) -> p kc n", kc=16)]
        t_wst = [Tok(), Tok()]
        brow_m = C.stage[0][0:1, :]
        bsb = C.xT[0:1, 8:14, :].rearrange("p c t -> p (c t)")
        t_bsb = Tok()
        k.dma("sp", bsb, bada, writes=[t_bsb])
        ones2 = k.sb("ones2", [1, 2], F32)
        k.op("dve", lambda e: e.memset(ones2[:], 1.0), writes=[t_c])
        mres = C.xT[0:2, 0:6, :].rearrange("p c t -> p (c t)")
        t_mres = Tok()
        wv = wada.rearrange("(kc p) n -> p kc n", p=128)
        for jb in range(12):
            i = jb % 2
            k.dma("sp", wst[i], wv[:, :, jb * 512:(jb + 1) * 512], writes=[t_wst[i]])
            pb, tp = next_pb(C)
            for kc in range(16):
                k.op("pe", lambda e: e.matmul(pb[0:2, :], lhsT=c_sb[:, kc, :], rhs=wst[i][:, kc, :], start=(kc == 0), stop=False),
                     reads=[t_c, t_wst[i]], writes=[tp], inc=False)
            k.op("pe", lambda e: e.matmul(pb[0:2, :], lhsT=ones2[:], rhs=bsb[:, jb * 512:(jb + 1) * 512], start=False, stop=True),
                 reads=[t_c, t_bsb], writes=[tp])
            k.op("dve", lambda e: e.tensor_copy(out=mres[:, jb * 512:(jb + 1) * 512], in_=pb[0:2, :]), reads=[tp], writes=[t_mres])
        t_msrc, t_mdst = Tok(), Tok()
        k.dma("sp", mod_src, mres, reads=[t_mres], writes=[t_msrc])
        emit_collective(k, nc, "AllGather", mod_src, mod_dst, [t_msrc], [t_mdst], "cc_mod")
        mt = [C.stage[0][:, 0:128], C.stage[0][0:64, 128:256]]
        for r_ in range(4):
            rowsrc = mod_dst[2 * r_:2 * r_ + 1, :].rearrange("o (j p) -> (o j) p", p=128)
            lo = 48 * r_
            for (a0, a1) in ((lo, min(lo + 48, 128)), (max(lo, 128), lo + 48)):
                if a1 <= a0:
                    continue
                if a0 < 128:
                    k.dma("sp", mt[0][a0:a1, :], rowsrc[a0 - lo:a1 - lo, :], reads=[t_mdst], writes=[C.t_stage[0]])
                else:
                    k.dma("sp", mt[1][a0 - 128:a1 - 128, :], rowsrc[a0 - lo:a1 - lo, :], reads=[t_mdst], writes=[C.t_stage[0]])
        pb, tp = next_pb(C)
        k.op("pe", lambda e: e.transpose(pb[:, 0:128], mt[0], C.ident_f[:]), reads=[C.t_stage[0], C.t_const], writes=[tp])
        k.op("pe", lambda e: e.transpose(pb[:, 128:192], mt[1], C.ident_f[0:64, 0:64]), reads=[C.t_stage[0], C.t_const], writes=[tp])
        k.op("dve", lambda e: e.tensor_copy(out=vec0[:, 0:96], in_=pb[:, 0:96]), reads=[tp, C.t_vec], writes=[C.t_vec])
        k.op("dve", lambda e: e.tensor_copy(out=vec1[:, 0:96], in_=pb[:, 96:192]), reads=[tp, C.t_vec], writes=[C.t_vec])
        for c_ in range(NCH):
            for t_ in C.t_x[c_]:
                t_.w = t_msrc.w
        last_pe = (k.sem["pe"], k.cnt["pe"])
        for c_ in range(NCH):
            for t_ in C.t_big[c_] + C.t_h[c_]:
                t_.w = last_pe
        for t_ in C.t_stage + C.t_h32 + C.t_sa + C.t_tmp + C.t_stat + [tt for l_ in C.t_cmb for tt in l_]:
            t_.w = (k.sem["dve"], k.cnt["dve"])
        derive_vec(C)
        C.vec = vec1
        derive_vec(C)
        C.vec = vec0
        tv = C.t_vec
        k.op("dve", lambda e: e.tensor_tensor(out=vec0[:, V_A_N:V_A_N + 16], in0=vec0[:, V_LN_C_G:V_LN_C_G + 16], in1=vec1[:, V_SC1_T:V_SC1_T + 16], op=ALU.mult), reads=[tv], writes=[tv])
        k.op("dve", lambda e: e.tensor_tensor(out=vec0[:, V_B_N:V_B_N + 16], in0=vec0[:, V_LN_C_B:V_LN_C_B + 16], in1=vec1[:, V_SC1_T:V_SC1_T + 16], op=ALU.mult), reads=[tv], writes=[tv])
        k.op("dve", lambda e: e.tensor_tensor(out=vec0[:, V_B_N:V_B_N + 16], in0=vec0[:, V_B_N:V_B_N + 16], in1=vec1[:, V_SH_T:V_SH_T + 16], op=ALU.add), reads=[tv], writes=[tv])
        load_x(C, xin, True)
        swa_layer(C, {"pos": pos, "w_qkv": w_qkv, "w_o": w_o0})
        layer_norm_mod(C, vec0, V_LN_T_G, V_LN_T_B, V_A_C, V_B_C, C.tmp, C.t_tmp, C.stat, C.t_stat)
        moe_layer(C, 0, vec0, {"sc1": V_SC1_C, "sh": V_SH_C, "gp": V_GP_C, "w1": w1[0], "w3": w3[0], "w2": w2[0]})
        layer_norm_mod(C, vec0, V_LN_C_G, V_LN_C_B, V_A_N, V_B_N, C.tmp, C.t_tmp, C.stat, C.t_stat)
        if DBG.get('fdump'):
            d1 = nc.dram_tensor('dbg_vec', [128, 2 * NV], F32, kind='ExternalOutput').ap()
            k.dma('sp', d1[:, 0:NV], vec0[:], reads=[C.t_vec], writes=[C.t_out])
            k.dma('sp', d1[:, NV:2 * NV], vec1[:], reads=[C.t_vec], writes=[C.t_out])
            d2 = nc.dram_tensor('dbg_x1', [128, NCH * NT], F32, kind='ExternalOutput').ap()
            k.dma('sp', d2, C.xT[:].rearrange('p c t -> p (c t)'), reads=[C.t_x[c_][th_] for c_ in range(NCH) for th_ in range(2)], writes=[C.t_out])
            d3 = nc.dram_tensor('dbg_h1', [128, NCH * (NT + HALO0)], BF16, kind='ExternalOutput').ap()
            k.dma('sp', d3, C.hT[:].rearrange('p c t -> p (c t)'), reads=[C.t_h[c_][th_] for c_ in range(NCH) for th_ in range(3)], writes=[C.t_out])
        C.vec = vec1
        v = vec1
        k.dma("sp", C.rowsb[:], rows1, writes=[C.t_wr])
        k.dma("sp", C.wr[:], wr1.rearrange("(kc p) n -> p kc n", p=128), writes=[C.t_wr])
        f32v, bfv = C.f32v, C.bfv
        pos1 = pos[:, HALO0:HALO0 + NT]
        wk = {"i": 0,
              "qb": [bfv(4736, 512), bfv(5760, 512)], "tq": [Tok(), Tok()],
              "t1": [f32v(6784, 512), f32v(8832, 512)], "tt1": [Tok(), Tok()],
              "t2": [f32v(10880, 512), f32v(12928, 512)], "tt2": [Tok(), Tok()]}
        TcosA = f32v(16384, NT)
        TsinA = f32v(16384 + 4096, NT)
        t_trigA = Tok()
        lnc_done = (k.sem["pool"], k.cnt["pool"])
        preA = [C.t_big[c__][th__] for c__ in range(6) for th__ in range(2)] + C.t_tmp + C.t_sa + C.t_stat + C.t_stage + C.t_h32 + [tt for l_ in C.t_cmb for tt in l_]
        rope_tables(C, pos1, NT, TcosA, TsinA, t_trigA, C.big[:, 0:2, :].rearrange("p a t -> p (a t)").bitcast(I32),
                    C.big[:, 2:4, :].rearrange("p a t -> p (a t)").bitcast(F32), C.big[:, 4:6, :].rearrange("p a t -> p (a t)").bitcast(F32), pre=preA)
        for c__ in range(6):
            for th__ in range(2):
                C.t_big[c__][th__].w = t_trigA.w
                C.t_big[c__][th__].r = {}
        stg = C.big[:, 0:16, :].rearrange("p (a g) t -> p a g t", g=4)
        t_stg = [[C.t_big[a * 4 + g][0] for g in range(4)] for a in range(4)]
        col0 = [2048, 2304, 2560, 3072]
        kblocks = []
        order = []
        for a in range(4):
            for b in range(2):
                srcs = []
                for m in range(2):
                    g = b * 2 + m
                    col = col0[a] + g * 64
                    srcs.append((w_in[:, col:col + 64], m * 128))
                    srcs.append((w_in[:, col:col + 64], m * 128 + 64))
                kblocks.append((srcs, 2))
                order.append((a, b))

        def evacA(bi, m, th, pb, tp):
            a, b = order[bi]
            g = b * 2 + m
            dst = stg[:, a, g, tsl(th)]
            toks = [C.t_big[a * 4 + g][th]]
            if a >= 2:
                cs = tsl(th)
                rope_evac(C, pb, tp, 512, C.zero1[:, 0:1], TcosA[:, cs], TsinA[:, cs], t_trigA, dst, toks[0], wk)
            else:
                k.op("act", lambda e: e.copy(out=dst, in_=pb[:]), reads=[tp], writes=toks)
        linear_fm(C, kblocks, lambda kc, th: C.hT[:, kc, HALO0 + th * 512:HALO0 + (th + 1) * 512],
                  lambda th: (lambda kc: [C.t_h[kc][1 + th]]), 2, evacA)
        t_kvsrc = [Tok() for _ in range(12)]
        t_kvdst = [Tok() for _ in range(12)]
        bigflat = C.big[:].rearrange("p c t -> p (c t)")
        for ci in range(8):
            k.dma("sp", kv_src[ci], bigflat[:, ci * 2048:(ci + 1) * 2048], reads=[C.t_big[c_][th_] for c_ in (2 * ci, 2 * ci + 1) for th_ in range(2)], writes=[t_kvsrc[ci]])
            emit_collective(k, nc, "AllGather", kv_src[ci], kv_dst[ci], [t_kvsrc[ci]], [t_kvdst[ci]], "cc_kv%d" % ci)
        vaug = bfv(0, 2 * 2080).rearrange("p (a g t d) -> p a g t d", a=2, g=4, t=8)
        t_vaug = Tok()
        t_vaug.w = (k.sem["dve"], k.cnt["dve"])
        k.op("pool", lambda e: e.memset(bfv(0, 2 * 2080), 1.0), writes=[t_vaug])
        for vi, col in enumerate((2816, 3328)):
            vbuf, tvb = load_wblock(C, [(w_in[:, col:col + 256], 0)])
            for t in range(8):
                pb, tp = next_pb(C)
                for kc in range(NCH):
                    k.op("pe", lambda e: e.matmul(pb[:, 0:256], lhsT=C.hT[:, kc, HALO0 + t * 128:HALO0 + (t + 1) * 128], rhs=vbuf[:, kc, :], start=(kc == 0), stop=(kc == NCH - 1)),
                         reads=[tvb, C.t_h[kc][1], C.t_h[kc][2]], writes=[tp], inc=(kc == NCH - 1))
                k.op("act", lambda e: e.copy(out=vaug[:, vi, :, t, 0:64], in_=pb[:, 0:256].rearrange("p (g d) -> p g d", g=4)), reads=[tp], writes=[t_vaug])
        for ci in range(8, 12):
            k.dma("sp", kv_src[ci], bfv(0, 2 * 2080)[:, (ci - 8) * 1040:(ci - 7) * 1040], reads=[t_vaug], writes=[t_kvsrc[ci]])
            emit_collective(k, nc, "AllGather", kv_src[ci], kv_dst[ci], [t_kvsrc[ci]], [t_kvdst[ci]], "cc_kv%d" % ci)
        gch = [d_.rearrange("(r p) n -> p r n", p=128) for d_ in kv_dst]

        def gblk(a, g):
            bl = a * 4 + g
            return gch[bl // 2][:, :, (bl % 2) * 1024:(bl % 2 + 1) * 1024], t_kvdst[bl // 2]

        def gv(vi, g):
            ci = 8 + vi * 2 + g // 2
            return gch[ci][:, :, (g % 2) * 520:(g % 2 + 1) * 520], t_kvdst[ci]
        A = Ctx()
        A.pos, A.w_in, A.w_o, A.w1, A.w3, A.w2 = pos1, w_in, w_o1, w1[1], w3[1], w2[1]
        A.phi_k1, A.phi_v1, A.causal, A.cvalid, A.sbias_d, A.xspill, A.xout = phi_k1, phi_v1, causal, cvalid, sbias_d, xspill, xout
        A.phi2_sb, A.peT_sb, A.ovl_sb, A.t_nc, A.kcT_all, A.vc_all, A.t_cmpkv = phi2_sb, peT_sb, ovl_sb, t_nc, kcT_all, vc_all, t_cmpkv
        A.cmp_prepped = False
        A.pre_toks = [t_trigA, t_vaug] + wk["tq"] + wk["tt1"] + wk["tt2"]

        def load_cmp_src(g, kv, kvA, t_kvA, selc, t_selc):
            a = kv
            src_, tsrc_ = gblk(a, g)
            k.dma("sp", selc.rearrange("p (r t) -> p r t", r=4), src_, reads=[tsrc_], writes=[t_selc])
            pecol = 16 * kv
            for jj in range(16):
                for half in range(2):
                    ps_ = slice(half * 64, (half + 1) * 64)
                    o0 = 2 * jj + half
                    k.op("dve",
                         lambda e: e.tensor_scalar(out=kvA[ps_, jj * 255:(jj + 1) * 255], in0=selc[ps_, o0:o0 + 16 * 254 + 1:16],
                                                   scalar1=peT_sb[ps_, pecol + jj:pecol + jj + 1], scalar2=None, op0=ALU.add),
                         reads=[t_selc, t_nc], writes=[t_kvA])

        def load_kv(g, kvA, t_kvA, Vs, t_Vs, Kw, t_Kw, Vw, t_Vw):
            Vsf = Vs.rearrange("p t d -> p (t d)")
            src_, tsrc_ = gv(1, g)
            k.dma("sp", Vsf.rearrange("p (r n) -> p r n", r=4), src_, reads=[tsrc_], writes=[t_Vs])
            for i in range(4):
                for (dst, src, col) in ((Vw[:, 0:4, :], Vs[:, 8 * i + 4:8 * i + 8, :], i), (Vw[:, 4:12, :], Vs[:, 8 * i:8 * i + 8, :], 4 + i)):
                    if i == 0:
                        k.op("dve", lambda e: e.tensor_scalar(out=dst, in0=src, scalar1=oneh[:, col:col + 1], scalar2=None, op0=ALU.mult), reads=[t_Vs, t_nc], writes=[t_Vw])
                    else:
                        k.op("dve", lambda e: e.scalar_tensor_tensor(out=dst, in0=src, scalar=oneh[:, col:col + 1], in1=dst, op0=ALU.mult, op1=ALU.add),
                             reads=[t_Vs, t_nc, t_Vw], writes=[t_Vw])
            src_, tsrc_ = gblk(3, g)
            k.dma("sp", kvA.rearrange("p (r t) -> p r t", r=4), src_, reads=[tsrc_], writes=[t_kvA])
            for i in range(4):
                for (dst, src, col) in ((Kw[:, 0:512], kvA[:, i * 1024 + 512:(i + 1) * 1024], i), (Kw[:, 512:1536], kvA[:, i * 1024:(i + 1) * 1024], 4 + i)):
                    if i == 0:
                        k.op("dve", lambda e: e.tensor_scalar(out=dst, in0=src, scalar1=oneh[:, col:col + 1], scalar2=None, op0=ALU.mult), reads=[t_kvA, t_nc], writes=[t_Kw])
                    else:
                        k.op("dve", lambda e: e.scalar_tensor_tensor(out=dst, in0=src, scalar=oneh[:, col:col + 1], in1=dst, op0=ALU.mult, op1=ALU.add),
                             reads=[t_kvA, t_nc, t_Kw], writes=[t_Kw])
            src_, tsrc_ = gblk(2, g)
            k.dma("sp", kvA.rearrange("p (r t) -> p r t", r=4), src_, reads=[tsrc_], writes=[t_kvA])
            src_, tsrc_ = gv(0, g)
            k.dma("sp", Vsf.rearrange("p (r n) -> p r n", r=4), src_, reads=[tsrc_], writes=[t_Vs])
        A.load_cmp_src, A.load_kv = load_cmp_src, load_kv
        nsa_core(nc, k, C, A)
        k.finish([C.t_out])
        print("fused program: %d instructions" % k.ninst, k.cnt)
    return nc


def fused_inputs(inp, core):
    b, r = core // 4, core % 4
    m0 = layer0_inputs(inp, np.zeros((128, 96), np.float32), core)
    vec0 = np.zeros((128, NV), np.float32)
    vec0[:, 0:280] = m0["vec"][:, 0:280]
    dummy = {"kc_full": None, "vc_full": None, "ks_full": None, "kw_full": np.zeros((4, 128, 4096), BF),
             "vs_aug_full": np.zeros((4, 128, 32, 65), BF), "vw_aug_full": np.zeros((4, 128, 32, 65), BF)}
    m1 = layer1b_inputs(inp, np.zeros((128, 96), np.float32), np.zeros((1, 1), np.float32), core, dummy)
    vec1 = np.zeros((128, NV), np.float32)
    vec1[:, 0:280] = m1["vec"][:, 0:280]
    wcat = np.concatenate([inp["w_ada"][0], inp["w_ada"][1]], axis=1)
    bcat = np.concatenate([inp["b_ada"][0], inp["b_ada"][1]], axis=0)
    c = inp["c"][b].astype(np.float32)
    cT = np.ascontiguousarray(np.stack([c, c], axis=1).reshape(16, 128, 2).transpose(1, 0, 2))
    oneh = np.zeros((128, 8), np.float32)
    if r > 0:
        oneh[:, r - 1] = 1.0
    oneh[:, 4 + r] = 1.0
    return {"xin": m0["xin"], "pos": m0["pos"], "vec0": vec0, "vec1": vec1, "ident_f": m0["ident_f"], "masks": m0["masks"],
            "rows0": m0["rows"], "rows1": m1["rows"], "cT": cT,
            "wada": np.ascontiguousarray(wcat[:, r * 6144:(r + 1) * 6144]), "bada": np.ascontiguousarray(bcat[None, r * 6144:(r + 1) * 6144]),
            "w_qkv": inp["swa_w_qkv"][0], "w_o0": inp["swa_w_o"][0], "wr0": m0["wr"], "wr1": m1["wr"],
            "w1": inp["moe_w1"], "w3": inp["moe_w3"], "w2": inp["moe_w2"],
            "w_in": inp["nsa_w_in"][0], "w_o1": inp["nsa_w_o"][0], "phi_k1": inp["nsa_phi_k1"][0], "phi_v1": inp["nsa_phi_v1"][0],
            "phi2": m1["phi2"], "peT": m1["peT"], "causal": m1["causal"], "cvalid": m1["cvalid"], "sbias": m1["sbias"], "ovl": m1["ovl"],
            "oneh": oneh}


def kernel(**inp):
    inp = {kk: np.asarray(vv) for kk, vv in inp.items()}
    cores = list(range(8))
    in_maps = [fused_inputs(inp, cc) for cc in cores]
    res = run_bass_kernel_spmd(build_fused(), in_maps, core_ids=cores)
    x2 = np.stack([np.asarray(r["xout"]) for r in res.results])
    return np.ascontiguousarray(x2.reshape(2, 4096, 2048)).astype(np.float32)
```

```python
import math
import numpy as np
import ml_dtypes
from contextlib import ExitStack
import concourse.bass as bass
import concourse.mybir as mybir
from concourse.bass_utils import run_bass_kernel_spmd

F32 = mybir.dt.float32
BF16 = mybir.dt.bfloat16
I32 = mybir.dt.int32
AF = mybir.ActivationFunctionType
ALU = mybir.AluOpType
AX = mybir.AxisListType

D = 2048
NCH = 16
NT = 1024
HALO0 = 128
ALPHA = 4 ** 0.25
LN_EPS = 1e-5
EPS_EFF = LN_EPS / (ALPHA * ALPHA)
ROPE_THETA = 500000.0
SCALE = 64 ** -0.5
WB = 256


class Tok:
    __slots__ = ("name", "w", "r")

    def __init__(self, name=""):
        self.name = name
        self.w = None
        self.r = {}


class K:
    ND = 6

    def __init__(self, nc, stack):
        self.nc = nc
        self.stack = stack
        self.eng = {"pe": nc.tensor, "act": nc.scalar, "dve": nc.vector, "pool": nc.gpsimd, "sp": nc.sync}
        self.sem = {}
        self.cnt = {}
        self.pend = {}
        for e in ("pe", "act", "dve", "pool"):
            self.sem[e] = stack.enter_context(nc.semaphore("s_" + e))
            self.cnt[e] = 0
            self.pend[e] = False
        self.dsem = {}
        self.dcnt = {}
        self.drr = {}
        self.dwaited = {}
        for q in ("sp", "act", "pool"):
            self.dsem[q] = [stack.enter_context(nc.semaphore("d_%s%d" % (q, i))) for i in range(self.ND)]
            self.dcnt[q] = [0] * self.ND
            self.dwaited[q] = [0] * self.ND
            self.drr[q] = 0
        self.waited = {e: {} for e in self.eng}
        self.ninst = 0
        self._n = 0

    def sb(self, name, shape, dt):
        return self.stack.enter_context(self.nc.sbuf_tensor("sb_" + name, list(shape), dt))

    def ps(self, name, shape, dt):
        return self.stack.enter_context(self.nc.psum_tensor("ps_" + name, list(shape), dt))

    def _wait(self, e, sem, val):
        w = self.waited[e]
        kk = id(sem)
        if w.get(kk, 0) >= val:
            return
        self.eng[e].wait_ge(sem, val)
        w[kk] = val

    def _deps(self, e, reads, writes):
        own = self.sem.get(e)
        pe = (e == "pe")
        for t in reads:
            if t.w is not None and not (pe and t.w[0] is own):
                self._wait(e, *t.w)
        for t in writes:
            if t.w is not None and not (pe and t.w[0] is own):
                self._wait(e, *t.w)
            for (s, v) in t.r.values():
                if pe and s is own:
                    continue
                self._wait(e, s, v)

    def _mark(self, sem, val, reads, writes):
        for t in writes:
            t.w = (sem, val)
            t.r = {}
        for t in reads:
            t.r[id(sem)] = (sem, val)

    def op(self, e, ins_fn, reads=(), writes=(), inc=True):
        self._deps(e, reads, writes)
        ins = ins_fn(self.eng[e])
        if inc:
            self.cnt[e] += 1
            ins.then_inc(self.sem[e], 1)
            self.pend[e] = False
            self._mark(self.sem[e], self.cnt[e], reads, writes)
        else:
            self.pend[e] = True
            self._mark(self.sem[e], self.cnt[e] + 1, reads, writes)
        self.ninst += 1
        return ins

    def dma(self, q, out, in_, reads=(), writes=(), **kw):
        i = self.drr[q]
        self.drr[q] = (i + 1) % self.ND
        sem = self.dsem[q][i]
        if self.dwaited[q][i] < self.dcnt[q][i]:
            self._wait(q, sem, self.dcnt[q][i])
            self.dwaited[q][i] = self.dcnt[q][i]
        self._deps(q, reads, writes)
        ins = self.eng[q].dma_start(out=out, in_=in_, **kw)
        self.dcnt[q][i] += 16
        ins.then_inc(sem, 16)
        self._mark(sem, self.dcnt[q][i], reads, writes)
        self.ninst += 1
        return ins

    def finish(self, toks, e="sp"):
        for t in toks:
            if t.w is not None:
                self._wait(e, *t.w)


class Ctx:
    pass


def setup_ctx(k, consts_ap, hoff=HALO0):
    C = Ctx()
    C.k = k
    C.pb = [k.ps("pb%d" % i, [128, 512], F32) for i in range(8)]
    C.t_pb = [Tok("pb%d" % i) for i in range(8)]
    C.pbi = 0
    C.xT = k.sb("xT", [128, NCH, NT], F32)
    C.t_x = [[Tok() for _ in range(2)] for _ in range(NCH)]
    C.HOFF = hoff
    C.hT = k.sb("hT", [128, NCH, NT + hoff], BF16)
    C.t_h = [[Tok() for _ in range(3)] for _ in range(NCH)]
    C.big = k.sb("big", [128, NCH, NT], BF16)
    C.t_big = [[Tok() for _ in range(2)] for _ in range(NCH)]
    C.NWB = 3
    C.wb = [k.sb("wb%d" % i, [128, NCH, WB], BF16) for i in range(C.NWB)]
    C.t_wb = [Tok() for _ in range(C.NWB)]
    C.wbi = 0
    C.ident_bf = k.sb("ident_bf", [128, 128], BF16)
    C.ident_f = k.sb("ident_f", [128, 128], F32)
    C.ones_f = k.sb("ones_f", [128, 128], F32)
    C.ones_bf = k.sb("ones_bf", [128, 128], BF16)
    C.t_const = Tok("const")
    k.dma("sp", C.ident_f[:], consts_ap["ident_f"], writes=[C.t_const])
    k.dma("pool", C.ident_bf[:], consts_ap["ident_f"], writes=[C.t_const])
    k.op("dve", lambda e: e.memset(C.ones_f[:], 1.0), writes=[C.t_const])
    k.op("dve", lambda e: e.memset(C.ones_bf[:], 1.0), writes=[C.t_const])
    return C


def next_pb(C):
    i = C.pbi
    C.pbi = (i + 1) % 8
    return C.pb[i], C.t_pb[i]


def next_wb(C):
    wl = getattr(C, 'wb_cur', None) or C.wb
    tl = getattr(C, 't_wb_cur', None) or C.t_wb
    i = C.wbi % len(wl)
    C.wbi = (i + 1) % len(wl)
    return wl[i], tl[i]


def tsl(th):
    return slice(th * 512, (th + 1) * 512)


def load_wblock(C, srcs, q="pool"):
    k = C.k
    buf, tok = next_wb(C)
    for (ap, off) in srcs:
        w = ap.shape[1]
        k.dma(q, buf[:, :, off:off + w], ap.rearrange("(kc p) n -> p kc n", p=128), writes=[tok])
    return buf, tok


def mm_group(C, out_ap, t_out, pairs, reads, first_in_bank=True):
    k = C.k
    n = len(pairs)
    for i, (l, r) in enumerate(pairs):
        k.op("pe", lambda e: e.matmul(out_ap, lhsT=l, rhs=r, start=(i == 0 and first_in_bank), stop=(i == n - 1),
                                      skip_group_check=True),
             reads=reads, writes=[t_out], inc=(i == n - 1))


def load_x_transposed(C, x_dram, n_tiles, dst_fn, stage, t_stage):
    k = C.k
    for t in range(n_tiles):
        s = t % 2
        k.dma("sp", stage[s][:], x_dram[t * 128:(t + 1) * 128, :], writes=[t_stage[s]])
        for c0 in range(0, NCH, 4):
            pb, tp = next_pb(C)
            for j in range(4):
                c = c0 + j
                k.op("pe", lambda e: e.transpose(pb[:, j * 128:(j + 1) * 128], stage[s][:, c * 128:(c + 1) * 128], C.ident_f[:]),
                     reads=[t_stage[s], C.t_const], writes=[tp], inc=(j == 3))
            dst_fn(t, c0, pb[:].rearrange("p (j n) -> p j n", j=4), tp)


def layer_norm_mod(C, vec, g_col, b_col, A_col, B_col, tmp, t_tmp, stat, t_stat, h_halo=False):
    k = C.k
    if hasattr(C, "t_wb4"):
        for e_ in ("act", "dve", "pool"):
            k._deps(e_, (), [C.t_wb4])
    for th in range(2):
        ts = tsl(th)
        pb_s, tp_s = next_pb(C)
        pb_q, tp_q = next_pb(C)
        for c in range(NCH):
            k.op("pe", lambda e: e.matmul(pb_s[:], lhsT=C.ones_f[:], rhs=C.xT[:, c, ts], start=(c == 0), stop=(c == NCH - 1)),
                 reads=[C.t_x[c][th], C.t_const], writes=[tp_s], inc=(c == NCH - 1))
        for c in range(NCH):
            s = c % 2
            k.op("act", lambda e: e.activation(out=tmp[s][:], in_=C.xT[:, c, ts], func=AF.Square),
                 reads=[C.t_x[c][th]], writes=[t_tmp[s]])
            k.op("pe", lambda e: e.matmul(pb_q[:], lhsT=C.ones_f[:], rhs=tmp[s][:], start=(c == 0), stop=(c == NCH - 1)),
                 reads=[t_tmp[s], C.t_const], writes=[tp_q])
        mean, rstd, msq = stat[0], stat[1], stat[2]
        k.op("act", lambda e: e.mul(out=mean[:], in_=pb_s[:], mul=1.0 / D), reads=[tp_s], writes=[t_stat[0]])
        k.op("dve", lambda e: e.tensor_tensor(out=msq[:], in0=mean[:], in1=mean[:], op=ALU.mult), reads=[t_stat[0]], writes=[t_stat[2]])
        k.op("dve", lambda e: e.scalar_tensor_tensor(out=msq[:], in0=pb_q[:], scalar=1.0 / D, in1=msq[:], op0=ALU.mult, op1=ALU.subtract),
             reads=[tp_q, t_stat[2]], writes=[t_stat[2]])
        k.op("dve", lambda e: e.tensor_scalar(out=msq[:], in0=msq[:], scalar1=EPS_EFF, scalar2=None, op0=ALU.add),
             reads=[t_stat[2]], writes=[t_stat[2]])
        k.op("act", lambda e: e.sqrt(out=msq[:], in_=msq[:]), reads=[t_stat[2]], writes=[t_stat[2]])
        k.op("dve", lambda e: e.reciprocal(out=rstd[:], in_=msq[:]), reads=[t_stat[2]], writes=[t_stat[1]])
        for c in range(NCH):
            s = c % 2
            k.op("dve", lambda e: e.tensor_tensor(out=tmp[s][:], in0=C.xT[:, c, ts], in1=mean[:], op=ALU.subtract),
                 reads=[C.t_x[c][th], t_stat[0]], writes=[t_tmp[s]])
            k.op("dve", lambda e: e.tensor_tensor(out=tmp[s][:], in0=tmp[s][:], in1=rstd[:], op=ALU.mult),
                 reads=[t_tmp[s], t_stat[1]], writes=[t_tmp[s]])
            k.op("act", lambda e: e.activation(out=C.xT[:, c, ts], in_=tmp[s][:], func=AF.Identity,
                                               scale=vec[:, g_col + c:g_col + c + 1], bias=vec[:, b_col + c:b_col + c + 1]),
                 reads=[t_tmp[s], C.t_vec], writes=[C.t_x[c][th]])
            if A_col is not None:
                k.op("act", lambda e: e.activation(out=C.hT[:, c, C.HOFF + th * 512:C.HOFF + (th + 1) * 512], in_=tmp[s][:], func=AF.Identity,
                                                   scale=vec[:, A_col + c:A_col + c + 1], bias=vec[:, B_col + c:B_col + c + 1]),
                     reads=[t_tmp[s], C.t_vec], writes=[C.t_h[c][1 + th]])


def linear_fm(C, w_blocks, rhs_fn, rhs_toks_fn, n_tok_pieces, evac_fn):
    k = C.k
    for bi, (srcs, nm) in enumerate(w_blocks):
        buf, tw = load_wblock(C, srcs)
        for m in range(nm):
            for pc in range(n_tok_pieces):
                pb, tp = next_pb(C)
                rts = rhs_toks_fn(pc)
                for kc in range(NCH):
                    r = rhs_fn(kc, pc)
                    k.op("pe", lambda e: e.matmul(pb[:, 0:r.shape[-1]] if len(r.shape) == 2 else pb[:], lhsT=buf[:, kc, m * 128:(m + 1) * 128], rhs=r,
                                                  start=(kc == 0), stop=(kc == NCH - 1)),
                         reads=[tw] + rts(kc), writes=[tp], inc=(kc == NCH - 1))
                evac_fn(bi, m, pc, pb, tp)


def moe_layer(C, L, vec, W):
    k = C.k
    if not hasattr(C, "t_wb4"):
        C.t_wb4 = Tok()
    wb4 = C.bfv(20480, 4096).rearrange("p (kc n) -> p kc n", kc=16)
    k._deps("pool", (), C.t_tmp + C.t_stat + [C.t_wb4])
    C.wb_cur = C.wb + [wb4]
    C.t_wb_cur = C.t_wb + [C.t_wb4]
    pbr, tpr = next_pb(C)
    for c in range(NCH):
        s = c % 2
        k.op("dve", lambda e: e.tensor_scalar(out=C.h32[s][:], in0=C.xT[:, c, :], scalar1=vec[:, W["sc1"] + c:W["sc1"] + c + 1],
                                              scalar2=vec[:, W["sh"] + c:W["sh"] + c + 1], op0=ALU.mult, op1=ALU.add),
             reads=[C.t_x[c][0], C.t_x[c][1], C.t_vec], writes=[C.t_h32[s]])
        for t in range(8):
            k.op("pe", lambda e: e.matmul(pbr[:, t * 20:(t + 1) * 20], lhsT=C.h32[s][:, t * 128:(t + 1) * 128], rhs=C.wr[:, c, :],
                                          start=(c == 0 and t == 0), stop=(c == NCH - 1), skip_group_check=True),
                 reads=[C.t_h32[s], C.t_wr], writes=[tpr], inc=(t == 7))
    lg = C.lg
    tl = C.t_lg
    k.op("dve", lambda e: e.tensor_tensor(out=lg[:], in0=pbr[:, 0:160].rearrange("p (t n) -> p t n", t=8),
                                          in1=C.brow[:].unsqueeze(1).to_broadcast([128, 8, 20]), op=ALU.add),
         reads=[tpr, C.t_wr], writes=[tl])
    R = C.rt
    tr = C.t_rt
    for t in range(8):
        gl = lg[:, t, 0:4]
        rl = lg[:, t, 4:20].rearrange("p (g j) -> p g j", g=4)
        k.op("dve", lambda e: e.tensor_reduce(out=R["gmax"][:], in_=gl, axis=AX.X, op=ALU.max), reads=[tl], writes=[tr])
        k.op("dve", lambda e: e.tensor_scalar(out=R["ngmax"][:], in0=R["gmax"][:], scalar1=-1.0, scalar2=None, op0=ALU.mult), reads=[tr], writes=[tr])
        k.op("act", lambda e: e.activation(out=R["ge"][:], in_=gl, func=AF.Exp, bias=R["ngmax"][:], scale=1.0, accum_out=R["gsum"][:]),
             reads=[tl, tr], writes=[tr])
        k.op("dve", lambda e: e.reciprocal(out=R["gprob"][:], in_=R["gsum"][:]), reads=[tr], writes=[tr])
        k.op("dve", lambda e: e.tensor_scalar(out=R["gw"][:], in0=gl, scalar1=R["gmax"][:], scalar2=R["gprob"][:], op0=ALU.is_equal, op1=ALU.mult),
             reads=[tl, tr], writes=[tr])
        k.op("dve", lambda e: e.tensor_reduce(out=R["m1"][:], in_=rl, axis=AX.X, op=ALU.max), reads=[tl], writes=[tr])
        m1b = R["m1"][:].unsqueeze(2).to_broadcast([128, 4, 4])
        k.op("dve", lambda e: e.tensor_tensor(out=R["eq1"][:], in0=rl, in1=m1b, op=ALU.is_equal), reads=[tl, tr], writes=[tr])
        k.op("dve", lambda e: e.scalar_tensor_tensor(out=R["rl2"][:], in0=R["eq1"][:], scalar=-1e30, in1=rl, op0=ALU.mult, op1=ALU.add),
             reads=[tl, tr], writes=[tr])
        k.op("dve", lambda e: e.tensor_reduce(out=R["m2"][:], in_=R["rl2"][:], axis=AX.X, op=ALU.max), reads=[tr], writes=[tr])
        m2b = R["m2"][:].unsqueeze(2).to_broadcast([128, 4, 4])
        k.op("dve", lambda e: e.tensor_tensor(out=R["top2"][:], in0=rl, in1=m2b, op=ALU.is_ge), reads=[tl, tr], writes=[tr])
        k.op("dve", lambda e: e.tensor_tensor(out=R["dd"][:], in0=rl, in1=m1b, op=ALU.subtract), reads=[tl, tr], writes=[tr])
        k.op("act", lambda e: e.activation(out=R["ee"][:], in_=R["dd"][:], func=AF.Exp), reads=[tr], writes=[tr])
        k.op("dve", lambda e: e.tensor_tensor(out=R["ee"][:], in0=R["ee"][:], in1=R["top2"][:], op=ALU.mult), reads=[tr], writes=[tr])
        k.op("dve", lambda e: e.tensor_reduce(out=R["den"][:], in_=R["ee"][:], axis=AX.X, op=ALU.add), reads=[tr], writes=[tr])
        k.op("dve", lambda e: e.reciprocal(out=R["den"][:], in_=R["den"][:]), reads=[tr], writes=[tr])
        k.op("dve", lambda e: e.tensor_tensor(out=R["den"][:], in0=R["den"][:], in1=R["gw"][:], op=ALU.mult), reads=[tr], writes=[tr])
        k.op("dve", lambda e: e.tensor_tensor(out=C.comb[:, t, :].rearrange("p (g j) -> p g j", g=4), in0=R["ee"][:],
                                              in1=R["den"][:].unsqueeze(2).to_broadcast([128, 4, 4]), op=ALU.mult),
             reads=[tr], writes=[C.t_comb])
    w1, w3, w2 = W["w1"], W["w3"], W["w2"]
    for gi in range(4):
        for j in range(4):
            ecol = gi * 4 + j
            for half in range(2):
                pb, tp = next_pb(C)
                for tt in range(4):
                    t = half * 4 + tt
                    s = (tt % 2)
                    k.op("dve", lambda e: e.tensor_scalar(out=C.diag[s][:], in0=C.ident_bf[:], scalar1=C.comb[:, t, ecol:ecol + 1], scalar2=None, op0=ALU.mult),
                         reads=[C.t_comb, C.t_const], writes=[C.t_diag[s]])
                    k.op("pe", lambda e: e.matmul(pb[:, tt * 128:(tt + 1) * 128], lhsT=C.ones_bf[:], rhs=C.diag[s][:], start=(tt == 0), stop=(tt == 3), skip_group_check=True),
                         reads=[C.t_diag[s], C.t_const], writes=[tp])
                k.op("act", lambda e: e.copy(out=C.cmb[:, j, half * 512:(half + 1) * 512], in_=pb[:]), reads=[tp], writes=[C.t_cmb[j][half]])
        for hb in range(8):
            j = hb // 2
            e_id = gi * 4 + j
            cols = slice((hb % 2) * 256, (hb % 2) * 256 + 256)
            b1, tw1 = load_wblock(C, [(w1[e_id][:, cols], 0)])
            b3, tw3 = load_wblock(C, [(w3[e_id][:, cols], 0)])
            for m in range(2):
                hc = hb * 2 + m
                for th in range(2):
                    hs = slice(C.HOFF + th * 512, C.HOFF + (th + 1) * 512)
                    pa, tpa = next_pb(C)
                    pbb, tpb = next_pb(C)
                    for kc in range(NCH):
                        k.op("pe", lambda e: e.matmul(pa[:], lhsT=b1[:, kc, m * 128:(m + 1) * 128], rhs=C.hT[:, kc, hs], start=(kc == 0), stop=(kc == NCH - 1)),
                             reads=[tw1, C.t_h[kc][1 + th]], writes=[tpa], inc=(kc == NCH - 1))
                    for kc in range(NCH):
                        k.op("pe", lambda e: e.matmul(pbb[:], lhsT=b3[:, kc, m * 128:(m + 1) * 128], rhs=C.hT[:, kc, hs], start=(kc == 0), stop=(kc == NCH - 1)),
                             reads=[tw3, C.t_h[kc][1 + th]], writes=[tpb], inc=(kc == NCH - 1))
                    s = (hc * 2 + th) % 2
                    k.op("act", lambda e: e.activation(out=C.sa[s][:], in_=pa[:], func=AF.Silu), reads=[tpa], writes=[C.t_sa[s]])
                    k.op("dve", lambda e: e.tensor_tensor(out=C.sa[s][:], in0=C.sa[s][:], in1=pbb[:], op=ALU.mult), reads=[C.t_sa[s], tpb], writes=[C.t_sa[s]])
                    k.op("dve", lambda e: e.tensor_tensor(out=C.big[:, hc, tsl(th)], in0=C.sa[s][:], in1=C.cmb[:, j, tsl(th)], op=ALU.mult),
                         reads=[C.t_sa[s], C.t_cmb[j][th]], writes=[C.t_big[hc][th]])
        w2g = w2[gi * 4:(gi + 1) * 4].rearrange("e h n -> (e h) n")
        for ob in range(8):
            bw, tw = load_wblock(C, [(w2g[:, ob * 256:(ob + 1) * 256], 0)])
            for m in range(2):
                c = ob * 2 + m
                for th in range(2):
                    pb, tp = next_pb(C)
                    for kc in range(NCH):
                        k.op("pe", lambda e: e.matmul(pb[:], lhsT=bw[:, kc, m * 128:(m + 1) * 128], rhs=C.big[:, kc, tsl(th)], start=(kc == 0), stop=(kc == NCH - 1)),
                             reads=[tw, C.t_big[kc][th]], writes=[tp], inc=(kc == NCH - 1))
                    k.op("dve", lambda e: e.scalar_tensor_tensor(out=C.xT[:, c, tsl(th)], in0=pb[:], scalar=vec[:, W["gp"] + c:W["gp"] + c + 1],
                                                                 in1=C.xT[:, c, tsl(th)], op0=ALU.mult, op1=ALU.add),
                         reads=[tp, C.t_vec, C.t_x[c][th]], writes=[C.t_x[c][th]])
    C.wb_cur = None
    C.t_wb_cur = None
    C.wbi = 0


V_MODT = 0
V_SH_T, V_SC_T, V_G_T, V_SH_C, V_SC_C, V_G_C = 0, 16, 32, 48, 64, 80
V_LN_T_G, V_LN_T_B, V_LN_C_G, V_LN_C_B = 96, 112, 128, 144
V_BQ, V_BK = 160, 176
V_INVF, V_M16, V_OM16, V_SGN = 180, 181, 182, 183
V_SC1_T, V_GP_T, V_SC1_C, V_GP_C, V_A_C, V_B_C = 184, 200, 216, 232, 248, 264
V_A_N, V_B_N = 280, 296
NV = 312
RBYTES = 34816


def setup_region(C):
    k = C.k
    C.R = k.sb("R", [128, RBYTES // 2], BF16)

    def f32v(b0, n):
        return C.R[:, b0 // 2:b0 // 2 + 2 * n].bitcast(F32)

    def bfv(b0, n):
        return C.R[:, b0 // 2:b0 // 2 + n]
    C.f32v, C.bfv = f32v, bfv
    C.stage = [f32v(0, 2048), f32v(8192, 2048)]
    C.t_stage = [Tok(), Tok()]
    C.h32 = [f32v(0, 1024), f32v(4096, 1024)]
    C.t_h32 = [Tok(), Tok()]
    C.cmb = bfv(8192, 4096).rearrange("p (j t) -> p j t", j=4)
    C.t_cmb = [[Tok(), Tok()] for _ in range(4)]
    C.sa = [f32v(16384, 512), f32v(18432, 512)]
    C.t_sa = [Tok(), Tok()]
    C.tmp = [f32v(20480, 512), f32v(22528, 512)]
    C.t_tmp = [Tok(), Tok()]
    C.stat = [f32v(24576, 512), f32v(26624, 512), f32v(28672, 512)]
    C.t_stat = [Tok(), Tok(), Tok()]
    C.vec = k.sb("vec", [128, NV], F32)
    C.t_vec = Tok("vec")
    C.wr = k.sb("wr", [128, NCH, 20], F32)
    C.brow = k.sb("brow", [128, 20], F32)
    C.t_wr = Tok()
    C.lg = k.sb("lg", [128, 8, 20], F32)
    C.t_lg = Tok()
    C.comb = k.sb("comb", [128, 8, 16], F32)
    C.t_comb = Tok()
    C.rt = {}
    for nm, shp in (("gmax", [128, 1]), ("ngmax", [128, 1]), ("gsum", [128, 1]), ("gprob", [128, 1]), ("ge", [128, 4]), ("gw", [128, 4]),
                    ("m1", [128, 4]), ("m2", [128, 4]), ("den", [128, 4]), ("eq1", [128, 4, 4]), ("rl2", [128, 4, 4]),
                    ("top2", [128, 4, 4]), ("dd", [128, 4, 4]), ("ee", [128, 4, 4])):
        C.rt[nm] = k.sb("rt_" + nm, shp, F32)
    C.t_rt = Tok()
    C.diag = [k.sb("diag%d" % i, [128, 128], BF16) for i in range(2)]
    C.t_diag = [Tok(), Tok()]
    C.E = [k.sb("E%d" % i, [128, 512], BF16) for i in range(4)]
    C.t_E = [Tok() for _ in range(4)]
    C.Ei = 0


def derive_vec(C):
    k = C.k
    v = C.vec
    tv = C.t_vec

    def ts(out_c, in_c, s1, s2, o0, o1=None):
        if o1 is None:
            k.op("dve", lambda e: e.tensor_scalar(out=v[:, out_c:out_c + 16], in0=v[:, in_c:in_c + 16], scalar1=s1, scalar2=None, op0=o0), reads=[tv], writes=[tv])
        else:
            k.op("dve", lambda e: e.tensor_scalar(out=v[:, out_c:out_c + 16], in0=v[:, in_c:in_c + 16], scalar1=s1, scalar2=s2, op0=o0, op1=o1), reads=[tv], writes=[tv])
    ts(V_SC1_T, V_SC_T, 1.0, None, ALU.add)
    ts(V_SC1_C, V_SC_C, 1.0, None, ALU.add)
    ts(V_GP_T, V_G_T, 1.0 / ALPHA, None, ALU.mult)
    ts(V_GP_C, V_G_C, 1.0 / ALPHA, None, ALU.mult)
    k.op("dve", lambda e: e.tensor_tensor(out=v[:, V_A_C:V_A_C + 16], in0=v[:, V_LN_T_G:V_LN_T_G + 16], in1=v[:, V_SC1_C:V_SC1_C + 16], op=ALU.mult), reads=[tv], writes=[tv])
    k.op("dve", lambda e: e.tensor_tensor(out=v[:, V_B_C:V_B_C + 16], in0=v[:, V_LN_T_B:V_LN_T_B + 16], in1=v[:, V_SC1_C:V_SC1_C + 16], op=ALU.mult), reads=[tv], writes=[tv])
    k.op("dve", lambda e: e.tensor_tensor(out=v[:, V_B_C:V_B_C + 16], in0=v[:, V_B_C:V_B_C + 16], in1=v[:, V_SH_C:V_SH_C + 16], op=ALU.add), reads=[tv], writes=[tv])


def rope_tables(C, pos_dram, ntok, Tcos, Tsin, t_trig, wk_i, wk_f, wk_a, pre=()):
    k = C.k
    v = C.vec
    tw = Tok()
    TWO_PI = 2.0 * math.pi
    k.dma("sp", wk_i, pos_dram[0:1, :].to_broadcast([128, ntok]), writes=[tw, t_trig] + list(pre))
    k.op("dve", lambda e: e.tensor_copy(out=wk_f, in_=wk_i), reads=[tw], writes=[tw])
    k.op("dve", lambda e: e.tensor_scalar(out=wk_f, in0=wk_f, scalar1=v[:, V_INVF:V_INVF + 1], scalar2=None, op0=ALU.mult), reads=[tw, C.t_vec], writes=[tw])
    for (off, dst, s1, s2) in ((0.0, Tsin, V_SGN, None), (0.5 * math.pi, Tcos, V_M16, V_OM16)):
        ta = Tok()
        k.op("dve", lambda e: e.tensor_scalar(out=wk_a, in0=wk_f, scalar1=off, scalar2=None, op0=ALU.add), reads=[tw], writes=[ta])
        k.op("dve", lambda e: e.tensor_scalar(out=wk_i, in0=wk_a, scalar1=1.0 / TWO_PI, scalar2=None, op0=ALU.mult), reads=[ta, tw], writes=[tw])
        k.op("dve", lambda e: e.tensor_copy(out=dst, in_=wk_i), reads=[tw], writes=[t_trig])
        k.op("dve", lambda e: e.scalar_tensor_tensor(out=wk_a, in0=dst, scalar=-TWO_PI, in1=wk_a, op0=ALU.mult, op1=ALU.add), reads=[t_trig, ta], writes=[ta])
        k.op("dve", lambda e: e.tensor_scalar(out=dst, in0=wk_a, scalar1=math.pi, scalar2=-TWO_PI, op0=ALU.is_gt, op1=ALU.mult), reads=[ta], writes=[t_trig])
        k.op("dve", lambda e: e.tensor_tensor(out=wk_a, in0=wk_a, in1=dst, op=ALU.add), reads=[ta, t_trig], writes=[ta])
        k.op("dve", lambda e: e.tensor_scalar(out=dst, in0=wk_a, scalar1=-math.pi, scalar2=TWO_PI, op0=ALU.is_lt, op1=ALU.mult), reads=[ta], writes=[t_trig])
        k.op("dve", lambda e: e.tensor_tensor(out=wk_a, in0=wk_a, in1=dst, op=ALU.add), reads=[ta, t_trig], writes=[ta])
        k.op("dve", lambda e: e.tensor_scalar(out=wk_a, in0=wk_a, scalar1=-math.pi, scalar2=math.pi, op0=ALU.max, op1=ALU.min), reads=[ta], writes=[ta])
        k.op("act", lambda e: e.activation(out=dst, in_=wk_a, func=AF.Sin), reads=[ta], writes=[t_trig])
        if s2 is None:
            k.op("dve", lambda e: e.tensor_scalar(out=dst, in0=dst, scalar1=v[:, s1:s1 + 1], scalar2=None, op0=ALU.mult), reads=[t_trig, C.t_vec], writes=[t_trig])
        else:
            k.op("dve", lambda e: e.tensor_scalar(out=dst, in0=dst, scalar1=v[:, s1:s1 + 1], scalar2=v[:, s2:s2 + 1], op0=ALU.mult, op1=ALU.add),
                 reads=[t_trig, C.t_vec], writes=[t_trig])


def rope_evac(C, pb, tp, n, bias_ap, Tcos_s, Tsin_s, t_trig, dst, t_dst, wk):
    k = C.k
    i = wk["i"]
    wk["i"] = (i + 1) % 2
    qb, t1, t2 = wk["qb"][i][:, 0:n], wk["t1"][i][:, 0:n], wk["t2"][i][:, 0:n]
    tq, tt1, tt2 = wk["tq"][i], wk["tt1"][i], wk["tt2"][i]
    k.op("act", lambda e: e.activation(out=qb, in_=pb[:, 0:n], func=AF.Identity, bias=bias_ap, scale=1.0), reads=[tp, C.t_vec], writes=[tq])
    pr, tpr = next_pb(C)
    k.op("pe", lambda e: e.matmul(pr[:, 0:n], lhsT=C.rperm[:], rhs=qb, start=True, stop=True), reads=[tq, C.t_const], writes=[tpr])
    k.op("dve", lambda e: e.tensor_tensor(out=t2, in0=pr[:, 0:n], in1=Tsin_s, op=ALU.mult), reads=[tpr, t_trig], writes=[tt2])
    k.op("dve", lambda e: e.tensor_tensor(out=t1, in0=qb, in1=Tcos_s, op=ALU.mult), reads=[tq, t_trig], writes=[tt1])
    k.op("dve", lambda e: e.tensor_tensor(out=dst, in0=t1, in1=t2, op=ALU.add), reads=[tt1, tt2], writes=[t_dst])


def swa_layer(C, W):
    k = C.k
    v = C.vec
    NTOK = NT + HALO0
    f32v, bfv = C.f32v, C.bfv
    Vaug = bfv(0, 9 * 4 * 65).rearrange("p (t g d) -> p t g d", t=9, g=4)
    t_V = [Tok() for _ in range(9)]
    wk = {"i": 0,
          "qb": [bfv(4736, 512), bfv(5760, 512)], "tq": [Tok(), Tok()],
          "t1": [f32v(6784, 512), f32v(8832, 512)], "tt1": [Tok(), Tok()],
          "t2": [f32v(10880, 512), f32v(12928, 512)], "tt2": [Tok(), Tok()]}
    otok = [bfv(6784, 2048), bfv(10880, 2048)]
    t_otok = [Tok(), Tok()]
    Tcos = f32v(16384, NTOK)
    Tsin = f32v(16384 + 4608, NTOK)
    t_trig = Tok()
    KT = bfv(25600, 4 * NTOK).rearrange("p (g t) -> p g t", g=4)
    t_K = [[Tok() for _ in range(3)] for _ in range(4)]
    wk_i = C.big[:, 0:3, :].rearrange("p a t -> p (a t)")[:, 0:2 * NTOK].bitcast(I32)
    wk_f = C.big[:, 3:6, :].rearrange("p a t -> p (a t)")[:, 0:2 * NTOK].bitcast(F32)
    wk_a = C.big[:, 6:9, :].rearrange("p a t -> p (a t)")[:, 0:2 * NTOK].bitcast(F32)
    rope_tables(C, W["pos"], NTOK, Tcos, Tsin, t_trig, wk_i, wk_f, wk_a, pre=[C.t_big[c__][th__] for c__ in range(9) for th__ in range(2)])
    for c in range(9):
        for th in range(2):
            C.t_big[c][th].w = t_trig.w
    wq = W["w_qkv"]
    blocks = [([(wq[:, b * WB:(b + 1) * WB], 0)], 2) for b in range(8)]

    def q_evac(bi, m, th, pb, tp):
        c = bi * 2 + m
        cs = slice(HALO0 + th * 512, HALO0 + (th + 1) * 512)
        rope_evac(C, pb, tp, 512, v[:, V_BQ + c:V_BQ + c + 1], Tcos[:, cs], Tsin[:, cs], t_trig, C.big[:, c, tsl(th)], C.t_big[c][th], wk)
    linear_fm(C, blocks, lambda kc, th: C.hT[:, kc, HALO0 + th * 512:HALO0 + (th + 1) * 512],
              lambda th: (lambda kc: [C.t_h[kc][1 + th]]), 2, q_evac)
    kblocks = []
    for b in range(2):
        srcs = []
        for m in range(2):
            g = b * 2 + m
            col = 2048 + g * 64
            srcs.append((wq[:, col:col + 64], m * 128))
            srcs.append((wq[:, col:col + 64], m * 128 + 64))
        kblocks.append((srcs, 2))

    def k_evac(bi, m, pc, pb, tp):
        g = bi * 2 + m
        cs = slice(pc * 384, (pc + 1) * 384)
        rope_evac(C, pb, tp, 384, v[:, V_BK + g:V_BK + g + 1], Tcos[:, cs], Tsin[:, cs], t_trig, KT[:, g, cs], t_K[g][pc], wk)

    def k_rt(pc):
        def f(kc):
            return [C.t_h[kc][0], C.t_h[kc][1], C.t_h[kc][2]]
        return f
    linear_fm(C, kblocks, lambda kc, pc: C.hT[:, kc, pc * 384:(pc + 1) * 384], k_rt, 3, k_evac)
    tvones = Tok()
    k.op("pool", lambda e: e.memset(Vaug[:, :, :, 64:65], 1.0), writes=t_V)
    vbuf, tvb = load_wblock(C, [(wq[:, 2304:2560], 0)])
    for t in range(9):
        pb, tp = next_pb(C)
        for kc in range(NCH):
            k.op("pe", lambda e: e.matmul(pb[:, 0:256], lhsT=C.hT[:, kc, t * 128:(t + 1) * 128], rhs=vbuf[:, kc, :], start=(kc == 0), stop=(kc == NCH - 1)),
                 reads=[tvb, C.t_h[kc][0], C.t_h[kc][1], C.t_h[kc][2]], writes=[tp], inc=(kc == NCH - 1))
        k.op("dve", lambda e: e.tensor_tensor(out=Vaug[:, t, :, 0:64], in0=pb[:, 0:256].rearrange("p (g d) -> p g d", g=4),
                                              in1=C.bvrow[:].rearrange("p (g d) -> p g d", g=4), op=ALU.add),
             reads=[tp, C.t_wr], writes=[t_V[t]])
    esink = C.esink[:].rearrange("p (i two) -> p i two", two=2)
    for j in range(8):
        so = j % 2
        ot = otok[so]
        otv = ot.rearrange("p (i two d) -> p i two d", two=2, d=64)
        for g in range(4):
            for par in range(2):
                ps_ = slice(par * 64, (par + 1) * 64)
                Es = []
                for (kt, mask) in ((j, C.mprev0 if j == 0 else C.mprev), (j + 1, C.mdiag)):
                    pb, tp = next_pb(C)
                    kcs = slice(kt * 128, (kt + 1) * 128)
                    k.op("pe", lambda e: e.matmul(pb[:].rearrange("p (h q) -> p h q", h=4), lhsT=KT[ps_, g, kcs],
                                                  rhs=C.big[ps_, 4 * g:4 * g + 4, j * 128:(j + 1) * 128], start=True, stop=True),
                         reads=[t_K[g][kt // 3], C.t_big[4 * g][j // 4], C.t_big[4 * g + 1][j // 4], C.t_big[4 * g + 2][j // 4], C.t_big[4 * g + 3][j // 4]],
                         writes=[tp])
                    ei = C.Ei
                    C.Ei = (ei + 1) % 4
                    E, tE = C.E[ei], C.t_E[ei]
                    k.op("act", lambda e: e.activation(out=E[:], in_=pb[:], func=AF.Exp, scale=SCALE), reads=[tp], writes=[tE])
                    k.op("dve", lambda e: e.tensor_tensor(out=E[:].rearrange("p (h q) -> p h q", h=4), in0=E[:].rearrange("p (h q) -> p h q", h=4),
                                                          in1=mask[:].unsqueeze(1).to_broadcast([128, 4, 128]), op=ALU.mult),
                         reads=[tE, C.t_const], writes=[tE])
                    Es.append((E, tE, kt))
                po, tpo = next_pb(C)
                first = True
                for hh in range(4):
                    for ii, (E, tE, kt) in enumerate(Es):
                        k.op("pe", lambda e: e.matmul(po[:, hh * 65:(hh + 1) * 65], lhsT=E[:, hh * 128:(hh + 1) * 128], rhs=Vaug[:, kt, g, :],
                                                      start=first, stop=(ii == 1), skip_group_check=True),
                             reads=[tE, t_V[kt]], writes=[tpo], inc=(hh == 3 and ii == 1))
                        first = False
                pov = po[:, 0:260].rearrange("p (h d) -> p h d", d=65)
                den, tden = C.rt["den"], C.t_rt
                k.op("dve", lambda e: e.tensor_tensor(out=den[:], in0=pov[:, :, 64], in1=esink[:, 4 * g:4 * g + 4, par], op=ALU.add),
                     reads=[tpo, C.t_wr], writes=[tden])
                k.op("dve", lambda e: e.reciprocal(out=den[:], in_=den[:]), reads=[tden], writes=[tden])
                k.op("dve", lambda e: e.tensor_tensor(out=otv[:, 4 * g:4 * g + 4, par, :], in0=pov[:, :, 0:64],
                                                      in1=den[:].unsqueeze(2).to_broadcast([128, 4, 64]), op=ALU.mult),
                     reads=[tpo, tden], writes=[t_otok[so]])
        for c0 in range(0, NCH, 4):
            pb, tp = next_pb(C)
            pbv = pb[:].bitcast(BF16)
            for jj in range(4):
                c = c0 + jj
                k.op("pe", lambda e: e.transpose(pbv[:, jj * 128:(jj + 1) * 128], ot[:, c * 128:(c + 1) * 128], C.ident_bf[:]),
                     reads=[t_otok[so], C.t_const], writes=[tp], inc=(jj == 3))
            th = j // 4
            k.op("act", lambda e: e.copy(out=C.hT[:, c0:c0 + 4, HALO0 + j * 128:HALO0 + (j + 1) * 128], in_=pbv[:, 0:512].rearrange("p (c q) -> p c q", c=4)),
                 reads=[tp], writes=[C.t_h[c0 + i][1 + th] for i in range(4)])
    wo = W["w_o"]
    blocks = [([(wo[:, b * WB:(b + 1) * WB], 0)], 2) for b in range(8)]

    def o_evac(bi, m, th, pb, tp):
        c = bi * 2 + m
        k.op("dve", lambda e: e.scalar_tensor_tensor(out=C.xT[:, c, tsl(th)], in0=pb[:], scalar=v[:, V_GP_T + c:V_GP_T + c + 1],
                                                     in1=C.xT[:, c, tsl(th)], op0=ALU.mult, op1=ALU.add),
             reads=[tp, C.t_vec, C.t_x[c][th]], writes=[C.t_x[c][th]])
    linear_fm(C, blocks, lambda kc, th: C.hT[:, kc, HALO0 + th * 512:HALO0 + (th + 1) * 512],
              lambda th: (lambda kc: [C.t_h[kc][1 + th]]), 2, o_evac)


def store_x(C, out_dram):
    k = C.k
    for t in range(8):
        s = t % 2
        for c0 in range(0, NCH, 4):
            pb, tp = next_pb(C)
            for jj in range(4):
                c = c0 + jj
                k.op("pe", lambda e: e.transpose(pb[:, jj * 128:(jj + 1) * 128], C.xT[:, c, t * 128:(t + 1) * 128], C.ident_f[:]),
                     reads=[C.t_x[c][t // 4], C.t_const], writes=[tp], inc=(jj == 3))
            k.op("act" if (c0 // 4) % 2 == 0 else "dve",
                 (lambda e: e.copy(out=C.stage[s][:, c0 * 128:(c0 + 4) * 128], in_=pb[:])) if (c0 // 4) % 2 == 0 else
                 (lambda e: e.tensor_copy(out=C.stage[s][:, c0 * 128:(c0 + 4) * 128], in_=pb[:])),
                 reads=[tp], writes=[C.t_stage[s]])
        k.dma("sp", out_dram[t * 128:(t + 1) * 128, :], C.stage[s], reads=[C.t_stage[s]], writes=[C.t_out])


def load_x(C, x_dram, has_halo):
    k = C.k
    v = C.vec
    n_tiles = 9 if has_halo else 8

    def dst(t, c0, pv, tp):
        if has_halo and t == 0:
            for jj in range(4):
                c = c0 + jj
                k.op("act", lambda e: e.activation(out=C.hT[:, c, 0:128], in_=pv[:, jj, :], func=AF.Identity,
                                                   scale=v[:, V_SC1_T + c:V_SC1_T + c + 1], bias=v[:, V_SH_T + c:V_SH_T + c + 1]),
                     reads=[tp, C.t_vec], writes=[C.t_h[c][0]])
        else:
            to = t - 1 if has_halo else t
            k.op("act", lambda e: e.copy(out=C.xT[:, c0:c0 + 4, to * 128:(to + 1) * 128], in_=pv),
                 reads=[tp], writes=[C.t_x[c0 + i][to // 4] for i in range(4)])
    load_x_transposed(C, x_dram, n_tiles, dst, C.stage, C.t_stage)
    for c in range(NCH):
        for th in range(2):
            k.op("dve",
                 lambda e: e.tensor_scalar(out=C.hT[:, c, C.HOFF + th * 512:C.HOFF + (th + 1) * 512], in0=C.xT[:, c, tsl(th)],
                                           scalar1=v[:, V_SC1_T + c:V_SC1_T + c + 1], scalar2=v[:, V_SH_T + c:V_SH_T + c + 1], op0=ALU.mult, op1=ALU.add),
                 reads=[C.t_x[c][th], C.t_vec], writes=[C.t_h[c][1 + th]])


def build_layer0():
    nc = bass.Bass("TRN2", target_bir_lowering=False)

    def din(name, shape, dt=F32):
        return nc.dram_tensor(name, list(shape), dt, kind="ExternalInput").ap()
    xin = din("xin", [NT + HALO0, D])
    pos = din("pos", [1, NT + HALO0], I32)
    vec_d = din("vec", [128, NV])
    ident = din("ident_f", [128, 128])
    masks = din("masks", [128, 4, 128])
    rows = din("rows", [128, 256 + 32 + 20])
    w_qkv = din("w_qkv", [D, 2560])
    w_o = din("w_o", [D, D])
    wr_d = din("wr", [D, 20])
    w1 = din("w1", [16, D, 512])
    w3 = din("w3", [16, D, 512])
    w2 = din("w2", [16, 512, D])
    xout = nc.dram_tensor("xout", [NT, D], F32, kind="ExternalOutput").ap()
    with ExitStack() as st:
        k = K(nc, st)
        C = setup_ctx(k, {"ident_f": ident})
        setup_region(C)
        C.t_out = Tok()
        C.mk = k.sb("mk", [128, 4, 128], BF16)
        k.dma("pool", C.mk[:], masks, writes=[C.t_const])
        C.mdiag, C.mprev, C.mprev0, C.rperm = C.mk[:, 0, :], C.mk[:, 1, :], C.mk[:, 2, :], C.mk[:, 3, :]
        C.rowsb = k.sb("rowsb", [128, 308], F32)
        C.bvrow = C.rowsb[:, 0:256]
        C.esink = k.sb("esink", [128, 32], F32)
        C.brow = C.rowsb[:, 288:308]
        k.dma("sp", C.rowsb[:], rows, writes=[C.t_wr])
        k.op("act", lambda e: e.activation(out=C.esink[:], in_=C.rowsb[:, 256:288], func=AF.Exp), reads=[C.t_wr], writes=[C.t_wr])
        k.dma("sp", C.wr[:], wr_d.rearrange("(kc p) n -> p kc n", p=128), writes=[C.t_wr])
        k.dma("sp", C.vec[:], vec_d, writes=[C.t_vec])
        derive_vec(C)
        load_x(C, xin, True)
        swa_layer(C, {"pos": pos, "w_qkv": w_qkv, "w_o": w_o})
        layer_norm_mod(C, C.vec, V_LN_T_G, V_LN_T_B, V_A_C, V_B_C, C.tmp, C.t_tmp, C.stat, C.t_stat)
        moe_layer(C, 0, C.vec, {"sc1": V_SC1_C, "sh": V_SH_C, "gp": V_GP_C, "w1": w1, "w3": w3, "w2": w2})
        layer_norm_mod(C, C.vec, V_LN_C_G, V_LN_C_B, None, None, C.tmp, C.t_tmp, C.stat, C.t_stat)
        store_x(C, xout)
        k.finish([C.t_out])
        print("layer0 program: %d instructions" % k.ninst, k.cnt)
    return nc


def fm(vv):
    return np.ascontiguousarray(np.asarray(vv, np.float32).reshape(16, 128).T)


def rope_consts():
    p = np.arange(128)
    d = p % 64
    invf = np.where(d < 16, ROPE_THETA ** (-(2.0 * (d % 8)) / 16.0), 0.0).astype(np.float32)
    m16 = (d < 16).astype(np.float32)
    om16 = 1.0 - m16
    sgn = np.where(d < 8, -1.0, np.where(d < 16, 1.0, 0.0)).astype(np.float32)
    return invf, m16, om16, sgn


def const_masks(first_quarter):
    kk = np.arange(128)[:, None]
    qq = np.arange(128)[None, :]
    mdiag = (kk <= qq).astype(np.float32)
    mprev = (kk > qq).astype(np.float32)
    mprev0 = np.zeros_like(mprev) if first_quarter else mprev
    rperm = np.zeros((128, 128), np.float32)
    for m in range(128):
        d = m % 64
        if d < 8:
            rperm[m + 8, m] = 1.0
        elif d < 16:
            rperm[m - 8, m] = 1.0
    return np.ascontiguousarray(np.stack([mdiag, mprev, mprev0, rperm], axis=1))


def layer0_inputs(inp, modT, core):
    b, r = core // 4, core % 4
    i = 0
    t0 = r * NT
    x = inp["x"][b]
    halo = x[t0 - HALO0:t0] if r > 0 else np.zeros((HALO0, D), np.float32)
    xin = np.ascontiguousarray(np.concatenate([halo, x[t0:t0 + NT]], axis=0))
    p = inp["positions"][b]
    ph = p[t0 - HALO0:t0] if r > 0 else np.zeros((HALO0,), np.int32)
    pos = np.ascontiguousarray(np.concatenate([ph, p[t0:t0 + NT]])[None, :].astype(np.int32))
    vec = np.zeros((128, NV), np.float32)
    vec[:, 0:96] = modT
    vec[:, V_LN_T_G:V_LN_T_G + 16] = fm(inp["ln_t_g"][i])
    vec[:, V_LN_T_B:V_LN_T_B + 16] = fm(inp["ln_t_b"][i])
    vec[:, V_LN_C_G:V_LN_C_G + 16] = fm(inp["ln_c_g"][i])
    vec[:, V_LN_C_B:V_LN_C_B + 16] = fm(inp["ln_c_b"][i])
    bq = inp["swa_b_qkv"][0]
    vec[:, V_BQ:V_BQ + 16] = fm(bq[0:2048])
    for g in range(4):
        bk = bq[2048 + g * 64:2048 + (g + 1) * 64]
        vec[:, V_BK + g] = np.concatenate([bk, bk])
    invf, m16, om16, sgn = rope_consts()
    vec[:, V_INVF], vec[:, V_M16], vec[:, V_OM16], vec[:, V_SGN] = invf, m16, om16, sgn
    rows = np.zeros((128, 308), np.float32)
    rows[:, 0:256] = bq[2304:2560][None, :]
    rows[:, 256:288] = inp["swa_sinks"][0][None, :]
    rows[:, 288:292] = inp["moe_b_group"][i][None, :]
    rows[:, 292:308] = inp["moe_b_router"][i][None, :]
    wr = np.ascontiguousarray(np.concatenate([inp["moe_w_group"][i], inp["moe_w_router"][i]], axis=1))
    return {"xin": xin, "pos": pos, "vec": vec, "ident_f": np.eye(128, dtype=np.float32), "masks": const_masks(r == 0),
            "rows": rows, "w_qkv": inp["swa_w_qkv"][0], "w_o": inp["swa_w_o"][0], "wr": wr,
            "w1": inp["moe_w1"][i], "w3": inp["moe_w3"][i], "w2": inp["moe_w2"][i]}


NSA_Q = 2048
KVW = 256
HALO1 = 512


def build_layer1a():
    nc = bass.Bass("TRN2", target_bir_lowering=False)

    def din(name, shape, dt=F32):
        return nc.dram_tensor(name, list(shape), dt, kind="ExternalInput").ap()
    xin = din("xin", [NT, D])
    pos = din("pos", [1, NT], I32)
    vec_d = din("vec", [128, NV])
    ident = din("ident_f", [128, 128])
    masks = din("masks", [128, 4, 128])
    w_in = din("w_in", [D, 3680])
    outs = {}
    for nm in ("kc_T", "vc_T", "ks_T", "kw_T"):
        outs[nm] = nc.dram_tensor(nm, [128, 4, NT], BF16, kind="ExternalOutput").ap()
    for nm in ("vs", "vw"):
        outs[nm] = nc.dram_tensor(nm, [128, 8, 256], BF16, kind="ExternalOutput").ap()
    with ExitStack() as st:
        k = K(nc, st)
        C = setup_ctx(k, {"ident_f": ident}, hoff=0)
        setup_region(C)
        C.t_out = Tok()
        C.mk = k.sb("mk", [128, 4, 128], BF16)
        k.dma("pool", C.mk[:], masks, writes=[C.t_const])
        C.rperm = C.mk[:, 3, :]
        k.dma("sp", C.vec[:], vec_d, writes=[C.t_vec])
        derive_vec(C)
        load_x(C, xin, False)
        v = C.vec
        f32v, bfv = C.f32v, C.bfv
        wk = {"i": 0,
              "qb": [bfv(4736, 512), bfv(5760, 512)], "tq": [Tok(), Tok()],
              "t1": [f32v(6784, 512), f32v(8832, 512)], "tt1": [Tok(), Tok()],
              "t2": [f32v(10880, 512), f32v(12928, 512)], "tt2": [Tok(), Tok()]}
        Tcos = f32v(16384, NT)
        Tsin = f32v(16384 + 4096, NT)
        t_trig = Tok()
        wk_i = C.big[:, 0:2, :].rearrange("p a t -> p (a t)").bitcast(I32)
        wk_f = C.big[:, 2:4, :].rearrange("p a t -> p (a t)").bitcast(F32)
        wk_a = C.big[:, 4:6, :].rearrange("p a t -> p (a t)").bitcast(F32)
        rope_tables(C, pos, NT, Tcos, Tsin, t_trig, wk_i, wk_f, wk_a)
        KO = {"kc_T": 0, "vc_T": 1, "ks_T": 2, "kw_T": 3}
        col0 = {"kc_T": 2048, "vc_T": 2304, "ks_T": 2560, "kw_T": 3072}
        stg = C.big[:, 0:16, :].rearrange("p (a g) t -> p a g t", g=4)
        t_stg = [[Tok() for _ in range(4)] for _ in range(4)]
        for a in range(4):
            for g in range(4):
                t_stg[a][g].w = t_trig.w
        kblocks = []
        order = []
        for nm in ("kc_T", "vc_T", "ks_T", "kw_T"):
            for b in range(2):
                srcs = []
                for m in range(2):
                    g = b * 2 + m
                    col = col0[nm] + g * 64
                    srcs.append((w_in[:, col:col + 64], m * 128))
                    srcs.append((w_in[:, col:col + 64], m * 128 + 64))
                kblocks.append((srcs, 2))
                order.append((nm, b))
        C.zero1 = k.sb("zero1", [128, 1], F32)
        k.op("dve", lambda e: e.memset(C.zero1[:], 0.0), writes=[C.t_vec])

        def evac(bi, m, th, pb, tp):
            nm, b = order[bi]
            g = b * 2 + m
            a = KO[nm]
            dst = stg[:, a, g, tsl(th)]
            if nm in ("ks_T", "kw_T"):
                cs = tsl(th)
                rope_evac(C, pb, tp, 512, C.zero1[:, 0:1], Tcos[:, cs], Tsin[:, cs], t_trig, dst, t_stg[a][g], wk)
            else:
                k.op("act", lambda e: e.copy(out=dst, in_=pb[:]), reads=[tp], writes=[t_stg[a][g]])
        linear_fm(C, kblocks, lambda kc, th: C.hT[:, kc, C.HOFF + th * 512:C.HOFF + (th + 1) * 512],
                  lambda th: (lambda kc: [C.t_h[kc][1 + th]]), 2, evac)
        for nm in ("kc_T", "vc_T", "ks_T", "kw_T"):
            a = KO[nm]
            k.dma("sp", outs[nm], stg[:, a, :, :], reads=t_stg[a], writes=[C.t_out])
        vst = [C.stage[0].bitcast(BF16)[:, 0:2048].rearrange("p (t n) -> p t n", t=8), C.stage[1].bitcast(BF16)[:, 0:2048].rearrange("p (t n) -> p t n", t=8)]
        for vi, (nm, col) in enumerate((("vs", 2816), ("vw", 3328))):
            vbuf, tvb = load_wblock(C, [(w_in[:, col:col + 256], 0)])
            for t in range(8):
                pb, tp = next_pb(C)
                for kc in range(NCH):
                    k.op("pe", lambda e: e.matmul(pb[:, 0:256], lhsT=C.hT[:, kc, C.HOFF + t * 128:C.HOFF + (t + 1) * 128], rhs=vbuf[:, kc, :], start=(kc == 0), stop=(kc == NCH - 1)),
                         reads=[tvb, C.t_h[kc][1], C.t_h[kc][2]], writes=[tp], inc=(kc == NCH - 1))
                k.op("act", lambda e: e.copy(out=vst[vi][:, t, :], in_=pb[:, 0:256]), reads=[tp], writes=[C.t_stage[vi]])
            k.dma("sp", outs[nm], vst[vi], reads=[C.t_stage[vi]], writes=[C.t_out])
        k.finish([C.t_out])
        print("layer1a program: %d instructions" % k.ninst, k.cnt)
    return nc


def layer1a_inputs(inp, modT, x1_core, core):
    b, r = core // 4, core % 4
    t0 = r * NT
    pos = np.ascontiguousarray(inp["positions"][b][t0:t0 + NT][None, :].astype(np.int32))
    vec = np.zeros((128, NV), np.float32)
    vec[:, 0:96] = modT
    invf, m16, om16, sgn = rope_consts()
    vec[:, V_INVF], vec[:, V_M16], vec[:, V_OM16], vec[:, V_SGN] = invf, m16, om16, sgn
    return {"xin": np.ascontiguousarray(x1_core), "pos": pos, "vec": vec, "ident_f": np.eye(128, dtype=np.float32),
            "masks": const_masks(False), "w_in": inp["nsa_w_in"][0]}


DBG = {'compress': True, 'cmp': True, 'sel': True, 'win': True, 'moe': True, 'glim': 4, 'jlim': 8}


def nsa_core(nc, k, C, A):
    v = C.vec
    pos, w_in, w_o, w1, w3, w2 = A.pos, A.w_in, A.w_o, A.w1, A.w3, A.w2
    phi_k1, phi_v1, causal, cvalid, sbias_d, xspill, xout = A.phi_k1, A.phi_v1, A.causal, A.cvalid, A.sbias_d, A.xspill, A.xout
    phi2_sb, peT_sb, ovl_sb, t_nc, kcT_all, vc_all, t_cmpkv = A.phi2_sb, A.peT_sb, A.ovl_sb, A.t_nc, A.kcT_all, A.vc_all, A.t_cmpkv
    kc_full = vc_full = None
    t_spill = Tok()
    allx = [C.t_x[c][th] for c in range(NCH) for th in range(2)]
    k.dma("sp", xspill, C.xT[:].rearrange("p c t -> p (c t)"), reads=allx, writes=[t_spill] + list(getattr(A, 'pre_toks', [])))
    X = C.xT[:].rearrange("p c t -> p (c t)").bitcast(BF16)

    def xbf(b0, n):
        return X[:, b0 // 2:b0 // 2 + n]

    def xf32(b0, n):
        return X[:, b0 // 2:b0 // 2 + 2 * n].bitcast(F32)
    xs_toks = []

    def xtok():
        t = Tok()
        t.w = t_spill.w
        xs_toks.append(t)
        return t
    q_raw = xbf(0, 4096).rearrange("p (c t) -> p c t", c=4)
    q_rot = xbf(8192, 4096).rearrange("p (c t) -> p c t", c=4)
    t_qraw = [[xtok(), xtok()] for _ in range(4)]
    t_qrot = [[xtok(), xtok()] for _ in range(4)]
    kvA = xbf(16384, 4096)
    t_kvA = xtok()
    Vs = xbf(24576, 32 * 65).rearrange("p (t d) -> p t d", d=65)
    t_Vs = xtok()
    Kw = xbf(28736, NT + HALO1)
    t_Kw = xtok()
    Vw = xbf(31808, 12 * 65).rearrange("p (t d) -> p t d", d=65)
    t_Vw = xtok()
    Tcos = xf32(34144, NT)
    Tsin = xf32(38240, NT)
    t_trig = xtok()
    wk = {"i": 0,
          "qb": [xbf(42336, 512), xbf(43360, 512)], "tq": [xtok(), xtok()],
          "t1": [xf32(44384, 512), xf32(46432, 512)], "tt1": [xtok(), xtok()],
          "t2": [xf32(48480, 512), xf32(50528, 512)], "tt2": [xtok(), xtok()]}
    selc = xbf(52576, 4096)
    t_selc = xtok()
    hid = xbf(60768, 512).rearrange("p (c n) -> p c n", c=2)
    t_hid = xtok()
    if kcT_all is None:
        kcT_all = xbf(61792, 1024).rearrange("p (g n) -> p g n", g=4)
        vc_all = xbf(63840, 520).rearrange("p (g nt d) -> p g nt d", g=4, nt=2)
        t_cmpkv = xtok()
    f32v, bfv = C.f32v, C.bfv
    wj = bfv(0, 4096)
    t_wj = Tok()
    otok = [bfv(8192, 512), bfv(9216, 512)]
    t_otok = [Tok(), Tok()]
    gates = f32v(16384, 768).rearrange("p (t n) -> p t n", t=8)
    t_gates = Tok()
    sbias = f32v(19456, 512).rearrange("p (j s) -> p j s", j=8)
    t_sb = Tok()
    cval = bfv(21504, 256).rearrange("p (nt t) -> p nt t", nt=2)
    t_cval = Tok()
    score = f32v(22016, 64)
    score2 = f32v(22272, 64)
    mx8 = f32v(22528, 8)
    selm = bfv(22592, 64)
    t_sc = Tok()
    acc = f32v(23552, 256).rearrange("p (h d) -> p h d", h=4)
    tacc = f32v(24576, 256).rearrange("p (h d) -> p h d", h=4)
    t_acc = Tok()
    fac = f32v(25600, 4)
    rcs = f32v(25632, 8)
    t_fac = Tok()
    impt = f32v(25664, 64)
    C.accP = [f32v(26624, 256).rearrange('p (h d) -> p h d', h=4), f32v(27648, 256).rearrange('p (h d) -> p h d', h=4)]
    C.t_accP = [Tok(), Tok()]
    oTt = [f32v(28672, 512), f32v(28672, 512)]
    t_o1 = Tok()
    t_oTt = [t_o1, t_o1]
    E6 = list(C.E) + [bfv(30720, 512), bfv(31744, 512)]
    tE6 = list(C.t_E) + [Tok(), Tok()]
    for t_ in C.t_accP + [t_o1] + tE6[4:]:
        t_.w = t_spill.w
    st6 = {"i": 0}

    def nextE():
        i_ = st6["i"]
        st6["i"] = (i_ + 1) % 6
        return E6[i_], tE6[i_]

    def back_to_token_major(pacc, tpacc, par):
        k.op("act", lambda e: e.copy(out=oTt[par][0:65, :], in_=pacc[0:65, :]), reads=[tpacc], writes=[t_oTt[par]])
        pt, tpt = npb()
        for hh in range(4):
            k.op("pe", lambda e: e.transpose(pt[:, hh * 65:(hh + 1) * 65], oTt[par][0:65, hh * 128:(hh + 1) * 128], C.ident_f[0:65, 0:65]),
                 reads=[t_oTt[par], C.t_const], writes=[tpt], inc=(hh == 3))
        return pt, tpt
    for t_ in (t_wj, t_otok[0], t_otok[1], t_gates, t_sb, t_cval, t_sc, t_acc, t_fac):
        t_.w = t_spill.w
    k.dma("sp", sbias, sbias_d.rearrange("p (j s) -> p j s", j=8), writes=[t_sb])
    rope_tables(C, pos, NT, Tcos, Tsin, t_trig, C.big[:, 0:2, :].rearrange("p a t -> p (a t)").bitcast(I32), C.big[:, 2:4, :].rearrange("p a t -> p (a t)").bitcast(F32), C.big[:, 4:6, :].rearrange("p a t -> p (a t)").bitcast(F32), pre=[C.t_big[c__][th__] for c__ in range(6) for th__ in range(2)])
    for c_ in range(6):
        for th_ in range(2):
            C.t_big[c_][th_].w = t_trig.w
    for tl_ in t_qraw + t_qrot:
        for t_ in tl_:
            t_.w = t_trig.w
    t_kvA.w = t_trig.w
    C.NPB = 6

    def npb():
        i = C.pbi % 4
        C.pbi = (i + 1) % 4
        return C.pb[i], C.t_pb[i]
    C.pbi = 0
    gbuf, tgb = getattr(A, 'gates_w', None) or load_wblock(C, [(w_in[:, 3584:3680], 0)])
    for t in range(8):
        pb, tp = npb()
        for kc in range(NCH):
            k.op("pe", lambda e: e.matmul(pb[:, 0:96], lhsT=C.hT[:, kc, C.HOFF + t * 128:C.HOFF + (t + 1) * 128], rhs=gbuf[:, kc, 0:96], start=(kc == 0), stop=(kc == NCH - 1)),
                 reads=[tgb, C.t_h[kc][1], C.t_h[kc][2]], writes=[tp], inc=(kc == NCH - 1))
        k.op("act", lambda e: e.activation(out=gates[:, t, :], in_=pb[:, 0:96], func=AF.Sigmoid), reads=[tp], writes=[t_gates])
    k.op("pool", lambda e: e.memset(kcT_all[:], 0.0), writes=[t_cmpkv])
    k.op("pool", lambda e: e.memset(vc_all[:], 1.0), writes=[t_cmpkv])
    k.op("pool", lambda e: e.memset(hid[:], 0.0), writes=[t_hid])
    kvP = kvA[:, 0:16 * 255].rearrange("p (jj n) -> p jj n", jj=16)
    for kv, (phi1_d, src_full, pecol) in enumerate(((phi_k1, None, 0), (phi_v1, None, 16)) if DBG['compress'] else ()):
        pbuf, tpw = load_wblock(C, [(phi1_d, 0)])
        for g in range(4):
            A.load_cmp_src(g, kv, kvA, t_kvA, selc, t_selc)
            if A.cmp_prepped:
                k.op("dve", lambda e: e.tensor_tensor(out=kvP, in0=kvP, in1=peT_sb[:, pecol:pecol + 16].unsqueeze(2).to_broadcast([128, 16, 255]), op=ALU.add),
                     reads=[t_kvA, t_nc], writes=[t_kvA])
            for hc in range(2):
                pb, tp = npb()
                for jj in range(16):
                    k.op("pe", lambda e: e.matmul(pb[:, 0:255], lhsT=pbuf[:, jj, hc * 128:(hc + 1) * 128], rhs=kvP[:, jj, :], start=(jj == 0), stop=(jj == 15)),
                         reads=[tpw, t_kvA], writes=[tp], inc=(jj == 15))
                k.op("act", lambda e: e.activation(out=hid[:, hc, 0:255], in_=pb[:, 0:255], func=AF.Silu), reads=[tp], writes=[t_hid])
            if kv == 0:
                pb, tp = npb()
                for hc in range(2):
                    k.op("pe", lambda e: e.matmul(pb[:, 0:255], lhsT=phi2_sb[:, hc, 0:128], rhs=hid[:, hc, 0:255], start=(hc == 0), stop=(hc == 1)),
                         reads=[t_nc, t_hid], writes=[tp], inc=(hc == 1))
                k.op("act", lambda e: e.copy(out=kcT_all[:, g, 0:255], in_=pb[:, 0:255]), reads=[tp], writes=[t_cmpkv])
            else:
                for nt in range(2):
                    pb, tp = npb()
                    for hc in range(2):
                        k.op("pe", lambda e: e.matmul(pb[:, 0:64], lhsT=hid[:, hc, nt * 128:(nt + 1) * 128], rhs=phi2_sb[:, hc, 128:192], start=(hc == 0), stop=(hc == 1)),
                             reads=[t_nc, t_hid], writes=[tp], inc=(hc == 1))
                    k.op("act", lambda e: e.copy(out=vc_all[:, g, nt, 0:64], in_=pb[:, 0:64]), reads=[tp], writes=[t_cmpkv])
    NEG = -1e30
    for g in range(DBG['glim']):
        blocks = [([(w_in[:, (4 * g + 2 * b) * 128:(4 * g + 2 * b + 2) * 128], 0)], 2) for b in range(2)]

        def q_evac(bi, m, th, pb, tp):
            cc = bi * 2 + m
            cs = tsl(th)
            qb = q_raw[:, cc, cs]
            tq = t_qraw[cc][th]
            k.op("act", lambda e: e.copy(out=qb, in_=pb[:]), reads=[tp], writes=[tq])
            pr, tpr = npb()
            k.op("pe", lambda e: e.matmul(pr[:], lhsT=C.rperm[:], rhs=qb, start=True, stop=True), reads=[tq, C.t_const], writes=[tpr])
            i = wk["i"]
            wk["i"] = (i + 1) % 2
            t1, t2 = wk["t1"][i], wk["t2"][i]
            k.op("dve", lambda e: e.tensor_tensor(out=t2, in0=pr[:], in1=Tsin[:, cs], op=ALU.mult), reads=[tpr, t_trig], writes=[wk["tt2"][i]])
            k.op("dve", lambda e: e.tensor_tensor(out=t1, in0=qb, in1=Tcos[:, cs], op=ALU.mult), reads=[tq, t_trig], writes=[wk["tt1"][i]])
            k.op("dve", lambda e: e.tensor_tensor(out=q_rot[:, cc, cs], in0=t1, in1=t2, op=ALU.add), reads=[wk["tt1"][i], wk["tt2"][i]], writes=[t_qrot[cc][th]])
        for bi, (srcs, nm_) in enumerate(blocks):
            buf, tw = load_wblock(C, srcs)
            for m in range(nm_):
                for th in range(2):
                    pb, tp = npb()
                    for kc in range(NCH):
                        k.op("pe", lambda e: e.matmul(pb[:], lhsT=buf[:, kc, m * 128:(m + 1) * 128], rhs=C.hT[:, kc, C.HOFF + th * 512:C.HOFF + (th + 1) * 512],
                                                      start=(kc == 0), stop=(kc == NCH - 1)),
                             reads=[tw, C.t_h[kc][1 + th]], writes=[tp], inc=(kc == NCH - 1))
                    q_evac(bi, m, th, pb, tp)
        A.load_kv(g, kvA, t_kvA, Vs, t_Vs, Kw, t_Kw, Vw, t_Vw)
        for j in range(DBG['jlim']):
            th = j // 4
            qs = slice(j * 128, (j + 1) * 128)
            if g == 0 or True:
                pass
            k.dma("sp", wj, causal[j], writes=[t_wj])
            k.dma("sp", cval, cvalid[j].rearrange("p (nt t) -> p nt t", nt=2), writes=[t_cval])
            so = (g * 8 + j) % 2
            ot = otok[so]
            otv = ot.rearrange("p (i two d) -> p i two d", two=2, d=64)
            gv = gates[:, j, :].rearrange("p (hh two i) -> p hh two i", two=2, i=3)
            pU, tpU = C.pb[5], C.t_pb[5]
            Ecs = {}
            for par in (range(2) if DBG['cmp'] else ()):
                ps_ = slice(par * 64, (par + 1) * 64)
                for nt in range(2):
                    pb, tp = npb()
                    k.op("pe", lambda e: e.matmul(pb[:].rearrange("p (h q) -> p h q", h=4), lhsT=kcT_all[ps_, g, nt * 128:(nt + 1) * 128],
                                                  rhs=q_raw[ps_, :, qs], start=True, stop=True),
                         reads=[t_cmpkv] + [t_qraw[c_][th] for c_ in range(4)], writes=[tp])
                    ei = C.Ei
                    C.Ei = (ei + 1) % 4
                    E, tE = C.E[ei], C.t_E[ei]
                    k.op("act", lambda e: e.activation(out=E[:], in_=pb[:], func=AF.Exp, scale=SCALE), reads=[tp], writes=[tE])
                    k.op("dve", lambda e: e.tensor_tensor(out=E[:].rearrange("p (h q) -> p h q", h=4), in0=E[:].rearrange("p (h q) -> p h q", h=4),
                                                          in1=cval[:, nt, :].unsqueeze(1).to_broadcast([128, 4, 128]), op=ALU.mult),
                         reads=[tE, t_cval], writes=[tE])
                    Ecs[(par, nt)] = (E, tE)
                po, tpo = npb()
                first = True
                for hh in range(4):
                    for nt in range(2):
                        E, tE = Ecs[(par, nt)]
                        k.op("pe", lambda e: e.matmul(po[:, hh * 65:(hh + 1) * 65], lhsT=E[:, hh * 128:(hh + 1) * 128], rhs=vc_all[:, g, nt, :],
                                                      start=first, stop=(nt == 1), skip_group_check=True),
                             reads=[tE, t_cmpkv], writes=[tpo], inc=(hh == 3 and nt == 1))
                        first = False
                for hh in range(4):
                    for nt in range(2):
                        E, tE = Ecs[(par, nt)]
                        col = (par * 4 + hh) * 64
                        k.op("pe", lambda e: e.matmul(pU[:, col:col + 64], lhsT=E[:, hh * 128:(hh + 1) * 128], rhs=ovl_sb[:, nt, :],
                                                      start=(par == 0 and hh == 0 and nt == 0), stop=(nt == 1), skip_group_check=True),
                             reads=[tE, t_nc], writes=[tpU], inc=(hh == 3 and nt == 1))
                pov = po[:, 0:260].rearrange("p (h d) -> p h d", d=65)
                rc = rcs[:, par * 4:par * 4 + 4]
                k.op("dve", lambda e: e.tensor_scalar(out=rc, in0=pov[:, :, 64], scalar1=1e-30, scalar2=None, op0=ALU.max), reads=[tpo], writes=[t_fac])
                k.op("dve", lambda e: e.reciprocal(out=rc, in_=rc), reads=[t_fac], writes=[t_fac])
                k.op("dve", lambda e: e.tensor_tensor(out=fac, in0=rc, in1=gv[:, 4 * g:4 * g + 4, par, 0], op=ALU.mult), reads=[t_fac, t_gates], writes=[t_fac])
                k.op("dve", lambda e: e.tensor_tensor(out=C.accP[par], in0=pov[:, :, 0:64], in1=fac.unsqueeze(2).to_broadcast([128, 4, 64]), op=ALU.mult),
                     reads=[tpo, t_fac], writes=[C.t_accP[par]])
            for h8 in (range(8) if DBG['cmp'] else ()):
                if h8 == 0:
                    k.op("dve", lambda e: e.tensor_scalar(out=impt, in0=pU[:, 0:64], scalar1=rcs[:, 0:1], scalar2=None, op0=ALU.mult), reads=[tpU, t_fac], writes=[t_sc])
                else:
                    k.op("dve", lambda e: e.scalar_tensor_tensor(out=impt, in0=pU[:, h8 * 64:(h8 + 1) * 64], scalar=rcs[:, h8:h8 + 1], in1=impt, op0=ALU.mult, op1=ALU.add),
                         reads=[tpU, t_fac, t_sc], writes=[t_sc])
            if DBG['cmp']:
              k.op("dve", lambda e: e.tensor_tensor(out=score, in0=impt, in1=sbias[:, j, :], op=ALU.add), reads=[t_sc, t_sb], writes=[t_sc])
            k.op("dve", lambda e: e.max(out=mx8, in_=score), reads=[t_sc], writes=[t_sc])
            k.op("dve", lambda e: e.match_replace(out=score2, in_to_replace=mx8, in_values=score, imm_value=-3e38), reads=[t_sc], writes=[t_sc])
            k.op("dve", lambda e: e.max(out=mx8, in_=score2), reads=[t_sc], writes=[t_sc])
            k.op("dve", lambda e: e.tensor_scalar(out=selm, in0=score, scalar1=mx8[:, 7:8], scalar2=None, op0=ALU.is_ge), reads=[t_sc], writes=[t_sc])
            k.op("dve", lambda e: e.tensor_tensor(out=selc.rearrange("p (s q) -> p s q", s=64), in0=wj.rearrange("p (s q) -> p s q", s=64),
                                                  in1=selm.unsqueeze(2).to_broadcast([128, 64, 64]), op=ALU.mult),
                 reads=[t_sc, t_wj], writes=[t_selc])
            pow_ = [(C.pb[6], C.t_pb[6]), (C.pb[7], C.t_pb[7])]
            pendw = []

            def wstage1(i5):
                kt = j + i5
                banks = [npb(), npb()]
                outs = []
                for par in range(2):
                    ps_ = slice(par * 64, (par + 1) * 64)
                    pb, tp = banks[par]
                    k.op("pe", lambda e: e.matmul(pb[:].rearrange("p (h q) -> p h q", h=4), lhsT=Kw[ps_, kt * 128:(kt + 1) * 128],
                                                  rhs=q_rot[ps_, :, qs], start=True, stop=True),
                         reads=[t_Kw] + [t_qrot[c_][th] for c_ in range(4)], writes=[tp])
                for par in range(2):
                    pb, tp = banks[par]
                    E, tE = nextE()
                    k.op("act", lambda e: e.activation(out=E[:], in_=pb[:], func=AF.Exp, scale=SCALE), reads=[tp], writes=[tE])
                    mask = C.mprev if i5 == 0 else (C.mdiag if i5 == 4 else None)
                    if mask is not None:
                        k.op("dve", lambda e: e.tensor_tensor(out=E[:].rearrange("p (h q) -> p h q", h=4), in0=E[:].rearrange("p (h q) -> p h q", h=4),
                                                              in1=mask.unsqueeze(1).to_broadcast([128, 4, 128]), op=ALU.mult),
                             reads=[tE, C.t_const], writes=[tE])
                    outs.append((E, tE, par, kt, i5))
                return outs

            def wstage2(outs):
                for (E, tE, par, kt, i5) in outs:
                    po, tpo = pow_[par]
                    k.op("pe", lambda e: e.matmul(po[0:65, :], lhsT=Vw[:, kt, :], rhs=E[:], start=(i5 == 0), stop=(i5 == 4)),
                         reads=[tE, t_Vw], writes=[tpo])
            for n_ in range(5 + 1):
                if n_ < 5:
                    pendw.append(wstage1(n_))
                if n_ >= 1:
                    wstage2(pendw[n_ - 1])
            for par in range(2):
                po, tpo = back_to_token_major(pow_[par][0], pow_[par][1], par)
                pov = po[:, 0:260].rearrange("p (h d) -> p h d", d=65)
                k.op("dve", lambda e: e.reciprocal(out=fac, in_=pov[:, :, 64]), reads=[tpo], writes=[t_fac])
                k.op("dve", lambda e: e.tensor_tensor(out=fac, in0=fac, in1=gv[:, 4 * g:4 * g + 4, par, 2], op=ALU.mult), reads=[t_fac, t_gates], writes=[t_fac])
                k.op("dve", lambda e: e.tensor_tensor(out=tacc, in0=pov[:, :, 0:64], in1=fac.unsqueeze(2).to_broadcast([128, 4, 64]), op=ALU.mult),
                     reads=[tpo, t_fac], writes=[t_acc])
                k.op("dve", lambda e: e.tensor_tensor(out=C.accP[par], in0=C.accP[par], in1=tacc, op=ALU.add), reads=[t_acc, C.t_accP[par]], writes=[C.t_accP[par]])
            posel = [C.pb[6], C.pb[7]]
            tposel = [C.t_pb[6], C.t_pb[7]]
            firsts = [True, True]
            LOOK = 2
            kg_max = (24 + j) // 4
            last_kt = kg_max * 4 + 3
            blocks = [(kg, kk) for kg in range(kg_max + 1) for kk in range(4)]
            pend = []
            pm_state = {}

            def stage1(kg, kk):
                if kk == 0:
                    bi_ = 4 + (kg % 2)
                    pm, tpm = C.pb[bi_], C.t_pb[bi_]
                    pmv_ = pm[:].bitcast(BF16)
                    for k4 in range(4):
                        kt_ = kg * 4 + k4
                        k.op("pe", lambda e: e.transpose(pmv_[:, k4 * 128:(k4 + 1) * 128], selc[:, kt_ * 128:(kt_ + 1) * 128], C.ident_bf[:]),
                             reads=[t_selc, C.t_const], writes=[tpm], inc=(k4 == 3))
                    pm_state[kg] = (pmv_, tpm)
                pmv, tpm = pm_state[kg]
                kt = kg * 4 + kk
                outs = []
                banks = [npb(), npb()]
                for par in range(2):
                    ps_ = slice(par * 64, (par + 1) * 64)
                    pb, tp = banks[par]
                    k.op("pe", lambda e: e.matmul(pb[:].rearrange("p (h q) -> p h q", h=4), lhsT=kvA[ps_, kt * 128:(kt + 1) * 128],
                                                  rhs=q_rot[ps_, :, qs], start=True, stop=True),
                         reads=[t_kvA] + [t_qrot[c_][th] for c_ in range(4)], writes=[tp])
                for par in range(2):
                    pb, tp = banks[par]
                    E, tE = nextE()
                    k.op("act", lambda e: e.activation(out=E[:], in_=pb[:], func=AF.Exp, scale=SCALE), reads=[tp], writes=[tE])
                    k.op("dve", lambda e: e.tensor_tensor(out=E[:].rearrange("p (h q) -> p h q", h=4), in0=E[:].rearrange("p (h q) -> p h q", h=4),
                                                          in1=pmv[:, kk * 128:(kk + 1) * 128].unsqueeze(1).to_broadcast([128, 4, 128]), op=ALU.mult),
                         reads=[tE, tpm], writes=[tE])
                    outs.append((E, tE, par, kt))
                return outs

            def stage2(outs):
                for (E, tE, par, kt) in outs:
                    k.op("pe", lambda e: e.matmul(posel[par][0:65, :], lhsT=Vs[:, kt, :], rhs=E[:], start=firsts[par], stop=(kt == last_kt)),
                         reads=[tE, t_Vs], writes=[tposel[par]])
                    firsts[par] = False
            for n_ in range(len(blocks) + LOOK):
                if n_ < len(blocks):
                    pend.append(stage1(*blocks[n_]))
                if n_ >= LOOK:
                    stage2(pend[n_ - LOOK])
            for par in range(2):
                pt, tpt = back_to_token_major(posel[par], tposel[par], par)
                pov = pt[:, 0:260].rearrange("p (h d) -> p h d", d=65)
                k.op("dve", lambda e: e.reciprocal(out=fac, in_=pov[:, :, 64]), reads=[tpt], writes=[t_fac])
                k.op("dve", lambda e: e.tensor_tensor(out=fac, in0=fac, in1=gv[:, 4 * g:4 * g + 4, par, 1], op=ALU.mult), reads=[t_fac, t_gates], writes=[t_fac])
                k.op("dve", lambda e: e.tensor_tensor(out=tacc, in0=pov[:, :, 0:64], in1=fac.unsqueeze(2).to_broadcast([128, 4, 64]), op=ALU.mult),
                     reads=[tpt, t_fac], writes=[t_acc])
                k.op("dve", lambda e: e.tensor_tensor(out=otv[:, :, par, :], in0=C.accP[par], in1=tacc, op=ALU.add),
                     reads=[t_acc, C.t_accP[par]], writes=[t_otok[so]])
            pb, tp = npb()
            pbv = pb[:].bitcast(BF16)
            for jj in range(4):
                k.op("pe", lambda e: e.transpose(pbv[:, jj * 128:(jj + 1) * 128], ot[:, jj * 128:(jj + 1) * 128], C.ident_bf[:]),
                     reads=[t_otok[so], C.t_const], writes=[tp], inc=(jj == 3))
            k.op("act", lambda e: e.copy(out=C.big[:, 4 * g:4 * g + 4, qs], in_=pbv[:, 0:512].rearrange("p (c q) -> p c q", c=4)),
                 reads=[tp], writes=[C.t_big[4 * g + i][th] for i in range(4)])
    if DBG.get('dump'):
        dbg_o = nc.dram_tensor('dbg_oT', [128, NCH * NT], BF16, kind='ExternalOutput').ap()
        k.dma('sp', dbg_o, C.big[:].rearrange('p c t -> p (c t)'), reads=[C.t_big[c_][th_] for c_ in range(NCH) for th_ in range(2)], writes=[C.t_out])
    k.dma("sp", C.xT[:].rearrange("p c t -> p (c t)"), xspill, reads=[t_spill], writes=xs_toks + allx + [t_spill])
    C.pbi = 0
    blocks = [([(w_o[:, b * WB:(b + 1) * WB], 0)], 2) for b in range(8)]

    def o_evac(bi, m, th, pb, tp):
        c = bi * 2 + m
        k.op("dve", lambda e: e.scalar_tensor_tensor(out=C.xT[:, c, tsl(th)], in0=pb[:], scalar=v[:, V_GP_T + c:V_GP_T + c + 1],
                                                     in1=C.xT[:, c, tsl(th)], op0=ALU.mult, op1=ALU.add),
             reads=[tp, C.t_vec, C.t_x[c][th]], writes=[C.t_x[c][th]])
    linear_fm(C, blocks, lambda kc, th: C.big[:, kc, tsl(th)], lambda th: (lambda kc: [C.t_big[kc][th]]), 2, o_evac)
    layer_norm_mod(C, C.vec, V_LN_T_G, V_LN_T_B, V_A_C, V_B_C, C.tmp, C.t_tmp, C.stat, C.t_stat)
    if DBG['moe']:
        moe_layer(C, 1, C.vec, {"sc1": V_SC1_C, "sh": V_SH_C, "gp": V_GP_C, "w1": w1, "w3": w3, "w2": w2})
    layer_norm_mod(C, C.vec, V_LN_C_G, V_LN_C_B, None, None, C.tmp, C.t_tmp, C.stat, C.t_stat)
    store_x(C, xout)


def build_layer1b():
    nc = bass.Bass("TRN2", target_bir_lowering=False)

    def din(name, shape, dt=F32):
        return nc.dram_tensor(name, list(shape), dt, kind="ExternalInput").ap()
    xin = din("xin", [NT, D])
    pos = din("pos", [1, NT], I32)
    vec_d = din("vec", [128, NV])
    ident = din("ident_f", [128, 128])
    masks = din("masks", [128, 4, 128])
    rows = din("rows", [128, 308])
    w_in = din("w_in", [D, 3680])
    w_o = din("w_o", [D, D])
    wr_d = din("wr", [D, 20])
    w1 = din("w1", [16, D, 512])
    w3 = din("w3", [16, D, 512])
    w2 = din("w2", [16, 512, D])
    kc_full = din("kc_full", [4, 128, 16 * 255], BF16)
    vc_full = din("vc_full", [4, 128, 16 * 255], BF16)
    ks_full = din("ks_full", [4, 128, 4096], BF16)
    vs_aug = din("vs_aug", [4, 128, 32 * 65], BF16)
    kw_core = din("kw_core", [4, 128, NT + HALO1], BF16)
    vw_aug = din("vw_aug", [4, 128, 12 * 65], BF16)
    phi_k1 = din("phi_k1", [D, 256])
    phi_v1 = din("phi_v1", [D, 256])
    phi2 = din("phi2", [128, 2, 192])
    peT = din("peT", [128, 32])
    causal = din("causal", [8, 128, 4096], BF16)
    cvalid = din("cvalid", [8, 128, 256], BF16)
    sbias_d = din("sbias", [128, 8 * 64])
    ovl = din("ovl", [128, 2, 64])
    xspill = nc.dram_tensor("xspill", [128, NCH * NT], F32, kind="Internal").ap()
    xout = nc.dram_tensor("xout", [NT, D], F32, kind="ExternalOutput").ap()
    with ExitStack() as st:
        k = K(nc, st)
        C = setup_ctx(k, {"ident_f": ident}, hoff=0)
        setup_region(C)
        C.t_out = Tok()
        C.mk = k.sb("mk", [128, 4, 128], BF16)
        k.dma("pool", C.mk[:], masks, writes=[C.t_const])
        C.mdiag, C.mprev, C.rperm = C.mk[:, 0, :], C.mk[:, 1, :], C.mk[:, 3, :]
        C.rowsb = k.sb("rowsb", [128, 308], F32)
        C.brow = C.rowsb[:, 288:308]
        k.dma("sp", C.rowsb[:], rows, writes=[C.t_wr])
        k.dma("sp", C.wr[:], wr_d.rearrange("(kc p) n -> p kc n", p=128), writes=[C.t_wr])
        k.dma("sp", C.vec[:], vec_d, writes=[C.t_vec])
        derive_vec(C)
        C.zero1 = k.sb("zero1", [128, 1], F32)
        k.op("dve", lambda e: e.memset(C.zero1[:], 0.0), writes=[C.t_vec])
        phi2_sb = k.sb("phi2_sb", [128, 2, 192], BF16)
        peT_sb = k.sb("peT_sb", [128, 32], BF16)
        ovl_sb = k.sb("ovl_sb", [128, 2, 64], BF16)
        t_nc = Tok()
        k.dma("pool", phi2_sb[:], phi2, writes=[t_nc])
        k.dma("pool", peT_sb[:], peT, writes=[t_nc])
        k.dma("pool", ovl_sb[:], ovl, writes=[t_nc])
        kcT_all = k.sb("kcT_all", [128, 4, 256], BF16)
        vc_all = k.sb("vc_all", [128, 4, 2, 65], BF16)
        t_cmpkv = Tok()
        v = C.vec
        load_x(C, xin, False)
        A = Ctx()
        A.pos, A.w_in, A.w_o, A.w1, A.w3, A.w2 = pos, w_in, w_o, w1, w3, w2
        A.phi_k1, A.phi_v1, A.causal, A.cvalid, A.sbias_d, A.xspill, A.xout = phi_k1, phi_v1, causal, cvalid, sbias_d, xspill, xout
        A.phi2_sb, A.peT_sb, A.ovl_sb, A.t_nc, A.kcT_all, A.vc_all, A.t_cmpkv = phi2_sb, peT_sb, ovl_sb, t_nc, kcT_all, vc_all, t_cmpkv

        def load_cmp_src(g, kv, kvA, t_kvA, selc, t_selc):
            k.dma("sp", kvA[:, 0:16 * 255], (kc_full, vc_full)[kv][g], writes=[t_kvA])

        def load_kv(g, kvA, t_kvA, Vs, t_Vs, Kw, t_Kw, Vw, t_Vw):
            k.dma("sp", kvA, ks_full[g], writes=[t_kvA])
            k.dma("sp", Vs.rearrange("p t d -> p (t d)"), vs_aug[g], writes=[t_Vs])
            k.dma("sp", Kw, kw_core[g], writes=[t_Kw])
            k.dma("sp", Vw.rearrange("p t d -> p (t d)"), vw_aug[g], writes=[t_Vw])
        A.load_cmp_src, A.load_kv = load_cmp_src, load_kv
        A.cmp_prepped = True
        nsa_core(nc, k, C, A)
        k.finish([C.t_out])
        print("layer1b program: %d instructions" % k.ninst, k.cnt)
    return nc


BF = ml_dtypes.bfloat16


def layer1b_consts(r):
    tg = (1024 * r + np.arange(1024)).reshape(8, 128)
    keys = np.arange(4096)
    causal = (keys[None, None, :] <= tg[:, :, None]).astype(BF)
    n = np.arange(256)
    cend = 16 * n + 31
    valid = (cend[None, :, None] <= tg[:, None, :])
    valid[:, 255, :] = False
    cvalid = np.ascontiguousarray(valid.reshape(8, 2, 128, 128).transpose(0, 2, 1, 3).reshape(8, 128, 256)).astype(BF)
    s = np.arange(64)
    cur = tg // 64
    caus = (s[None, None, :] * 64 <= tg[:, :, None])
    forced = (s[None, None, :] == 0) | (s[None, None, :] == cur[:, :, None]) | (s[None, None, :] == cur[:, :, None] - 1)
    sb = np.where(caus, np.where(forced, 1e4, 0.0), -1e30).astype(np.float32)
    sbias = np.ascontiguousarray(sb.transpose(1, 0, 2).reshape(128, 512))
    c0 = n[:, None] * 16
    s0 = s[None, :] * 64
    ov = np.clip(np.minimum(c0 + 32, s0 + 64) - np.maximum(c0, s0), 0, None).astype(np.float32) / 32.0
    ov[255] = 0.0
    ovl = np.ascontiguousarray(ov.reshape(2, 128, 64).transpose(1, 0, 2))
    return causal, cvalid, sbias, ovl


def layer1b_inputs(inp, modT, x1_core, core, kvb):
    b, r = core // 4, core % 4
    i = 1
    t0 = r * NT
    pos = np.ascontiguousarray(inp["positions"][b][t0:t0 + NT][None, :].astype(np.int32))
    vec = np.zeros((128, NV), np.float32)
    vec[:, 0:96] = modT
    vec[:, V_LN_T_G:V_LN_T_G + 16] = fm(inp["ln_t_g"][i])
    vec[:, V_LN_T_B:V_LN_T_B + 16] = fm(inp["ln_t_b"][i])
    vec[:, V_LN_C_G:V_LN_C_G + 16] = fm(inp["ln_c_g"][i])
    vec[:, V_LN_C_B:V_LN_C_B + 16] = fm(inp["ln_c_b"][i])
    invf, m16, om16, sgn = rope_consts()
    vec[:, V_INVF], vec[:, V_M16], vec[:, V_OM16], vec[:, V_SGN] = invf, m16, om16, sgn
    rows = np.zeros((128, 308), np.float32)
    rows[:, 288:292] = inp["moe_b_group"][i][None, :]
    rows[:, 292:308] = inp["moe_b_router"][i][None, :]
    wr = np.ascontiguousarray(np.concatenate([inp["moe_w_group"][i], inp["moe_w_router"][i]], axis=1))
    causal, cvalid, sbias, ovl = layer1b_consts(r)
    kw_full = kvb["kw_full"]
    kw_core = np.zeros((4, 128, NT + HALO1), BF)
    lo = t0 - HALO1
    if lo >= 0:
        kw_core[:] = kw_full[:, :, lo:t0 + NT]
    else:
        kw_core[:, :, -lo:] = kw_full[:, :, 0:t0 + NT]
    vw_full = kvb["vw_aug_full"]
    vw = np.zeros((4, 128, 12, 65), BF)
    tlo = 8 * r - 4
    if tlo >= 0:
        vw[:] = vw_full[:, :, tlo:tlo + 12, :]
    else:
        vw[:, :, -tlo:, :] = vw_full[:, :, 0:tlo + 12, :]
    k2, v2 = inp["nsa_phi_k2"][0], inp["nsa_phi_v2"][0]
    phi2 = np.zeros((128, 2, 192), np.float32)
    for hc in range(2):
        phi2[:, hc, 0:64] = k2[hc * 128:(hc + 1) * 128]
        phi2[:, hc, 64:128] = k2[hc * 128:(hc + 1) * 128]
        phi2[:, hc, 128:192] = v2[hc * 128:(hc + 1) * 128]
    peT = np.zeros((128, 32), np.float32)
    pk, pv = inp["nsa_pe_k"][0], inp["nsa_pe_v"][0]
    peT[0:64, 0:16] = pk[0::2].T
    peT[64:128, 0:16] = pk[1::2].T
    peT[0:64, 16:32] = pv[0::2].T
    peT[64:128, 16:32] = pv[1::2].T
    return {"xin": np.ascontiguousarray(x1_core), "pos": pos, "vec": vec, "ident_f": np.eye(128, dtype=np.float32),
            "masks": const_masks(False), "rows": rows, "w_in": inp["nsa_w_in"][0], "w_o": inp["nsa_w_o"][0], "wr": wr,
            "w1": inp["moe_w1"][i], "w3": inp["moe_w3"][i], "w2": inp["moe_w2"][i],
            "kc_full": kvb["kc_full"], "vc_full": kvb["vc_full"], "ks_full": kvb["ks_full"],
            "vs_aug": np.ascontiguousarray(kvb["vs_aug_full"].reshape(4, 128, 32 * 65)),
            "kw_core": kw_core, "vw_aug": np.ascontiguousarray(vw.reshape(4, 128, 12 * 65)),
            "phi_k1": inp["nsa_phi_k1"][0], "phi_v1": inp["nsa_phi_v1"][0], "phi2": phi2, "peT": peT,
            "causal": causal, "cvalid": cvalid, "sbias": sbias, "ovl": ovl}


def assemble_kv(resA, b):
    cores = [resA[b * 4 + r] for r in range(4)]

    def catT(nm):
        full = np.concatenate([np.asarray(c[nm]) for c in cores], axis=2)
        return np.ascontiguousarray(full.transpose(1, 0, 2))

    def vaug(nm):
        full = np.concatenate([np.asarray(c[nm]) for c in cores], axis=1)
        full = full.reshape(128, 32, 4, 64)
        aug = np.ones((128, 32, 4, 65), BF)
        aug[:, :, :, 0:64] = full
        return np.ascontiguousarray(aug.transpose(2, 0, 1, 3))
    def perm(a):
        out = np.zeros((4, 128, 16, 255), BF)
        n16 = 16 * np.arange(255)
        for jj in range(16):
            out[:, 0:64, jj, :] = a[:, 0:64, :][:, :, n16 + 2 * jj]
            out[:, 64:128, jj, :] = a[:, 64:128, :][:, :, n16 + 2 * jj + 1]
        return np.ascontiguousarray(out.reshape(4, 128, 16 * 255))
    return {"kc_full": perm(catT("kc_T")), "vc_full": perm(catT("vc_T")), "ks_full": catT("ks_T"), "kw_full": catT("kw_T"),
            "vs_aug_full": vaug("vs"), "vw_aug_full": vaug("vw")}


def build_mod():
    nc = bass.Bass("TRN2", target_bir_lowering=False)
    cT = nc.dram_tensor("cT", [128, 16, 2], F32, kind="ExternalInput").ap()
    w = nc.dram_tensor("w", [2048, 3072], F32, kind="ExternalInput").ap()
    b = nc.dram_tensor("b", [1, 3072], F32, kind="ExternalInput").ap()
    out = nc.dram_tensor("out", [2, 3072], F32, kind="ExternalOutput").ap()
    with ExitStack() as st:
        k = K(nc, st)
        c_sb = k.sb("c_sb", [128, 16, 2], F32)
        t_c = Tok()
        ca = k.sb("ca", [128, 16, 2], F32)
        t_ca = Tok()
        wb = [k.sb("wb%d" % i, [128, 16, 512], F32) for i in range(2)]
        t_wb = [Tok(), Tok()]
        bb = k.sb("bb", [1, 3072], F32)
        t_bb = Tok()
        ones = k.sb("ones", [1, 2], F32)
        t_ones = Tok()
        res = k.sb("res", [2, 3072], F32)
        t_res = Tok()
        pb = [k.ps("pb%d" % i, [128, 512], F32) for i in range(2)]
        t_pb = [Tok(), Tok()]
        k.dma("sp", c_sb[:], cT, writes=[t_c])
        k.dma("sp", bb[:], b, writes=[t_bb])
        k.op("dve", lambda e: e.memset(ones[:], 1.0), writes=[t_ones])
        k.op("act", lambda e: e.activation(out=ca[:], in_=c_sb[:], func=AF.Silu), reads=[t_c], writes=[t_ca])
        wv = w.rearrange("(kc p) n -> p kc n", p=128)
        for j in range(6):
            i = j % 2
            k.dma("sp", wb[i][:], wv[:, :, j * 512:(j + 1) * 512], writes=[t_wb[i]])
            for kc in range(16):
                k.op("pe", lambda e: e.matmul(pb[i][0:2, :], lhsT=ca[:, kc, :], rhs=wb[i][:, kc, :], start=(kc == 0), stop=False),
                     reads=[t_ca, t_wb[i]], writes=[t_pb[i]])
            k.op("pe", lambda e: e.matmul(pb[i][0:2, :], lhsT=ones[:], rhs=bb[:, j * 512:(j + 1) * 512], start=False, stop=True),
                 reads=[t_ones, t_bb], writes=[t_pb[i]])
            k.op("dve", lambda e: e.tensor_copy(out=res[:, j * 512:(j + 1) * 512], in_=pb[i][0:2, :]), reads=[t_pb[i]], writes=[t_res])
        k.dma("sp", out, res[:], reads=[t_res], writes=[t_res])
        k.finish([t_res])
    return nc


def kernel_unfused(**inp):
    inp = {kk: np.asarray(vv) for kk, vv in inp.items()}
    cores = list(range(8))
    c = inp["c"].astype(np.float32)
    wcat = np.concatenate([inp["w_ada"][0], inp["w_ada"][1]], axis=1)
    bcat = np.concatenate([inp["b_ada"][0], inp["b_ada"][1]], axis=0)
    cT = np.ascontiguousarray(c.T.reshape(16, 128, 2).transpose(1, 0, 2))
    in_maps = [{"cT": cT, "w": np.ascontiguousarray(wcat[:, i * 3072:(i + 1) * 3072]),
                "b": np.ascontiguousarray(bcat[None, i * 3072:(i + 1) * 3072])} for i in cores]
    res = run_bass_kernel_spmd(build_mod(), in_maps, core_ids=cores)
    mod = np.concatenate([np.asarray(r["out"]) for r in res.results], axis=1)

    def modT(b, layer):
        return np.ascontiguousarray(mod[b, layer * 12288:(layer + 1) * 12288].reshape(96, 128).T)
    in_maps = [layer0_inputs(inp, modT(cc // 4, 0), cc) for cc in cores]
    res = run_bass_kernel_spmd(build_layer0(), in_maps, core_ids=cores)
    x1 = [np.asarray(r["xout"]) for r in res.results]
    in_maps = [layer1a_inputs(inp, modT(cc // 4, 1), x1[cc], cc) for cc in cores]
    res = run_bass_kernel_spmd(build_layer1a(), in_maps, core_ids=cores)
    resA = [{kk: np.asarray(vv) for kk, vv in r.items()} for r in res.results]
    kvbs = [assemble_kv(resA, b) for b in range(2)]
    in_maps = [layer1b_inputs(inp, modT(cc // 4, 1), x1[cc], cc, kvbs[cc // 4]) for cc in cores]
    res = run_bass_kernel_spmd(build_layer1b(), in_maps, core_ids=cores)
    x2 = np.stack([np.asarray(r["xout"]) for r in res.results])
    return np.ascontiguousarray(x2.reshape(2, 4096, 2048)).astype(np.float32)


GW = 16384 + 2 * 2080
GROUPS = [[0, 1, 2, 3], [4, 5, 6, 7]]


def emit_collective(k, nc, kind, src, dst, reads, writes, name):
    k._deps("pool", reads, writes)
    ins = nc.gpsimd.collective_compute(kind, ALU.bypass, replica_groups=GROUPS, ins=[src], outs=[dst])
    sem = k.stack.enter_context(nc.semaphore(name))
    ins.then_inc(sem)
    k._mark(sem, 1, reads, writes)
    k.ninst += 1


def build_fused():
    nc = bass.Bass("TRN2", target_bir_lowering=False)

    def din(name, shape, dt=F32):
        return nc.dram_tensor(name, list(shape), dt, kind="ExternalInput").ap()
    xin = din("xin", [NT + HALO0, D])
    pos = din("pos", [1, NT + HALO0], I32)
    vec0_d = din("vec0", [128, NV])
    vec1_d = din("vec1", [128, NV])
    ident = din("ident_f", [128, 128])
    masks = din("masks", [128, 4, 128])
    rows0 = din("rows0", [128, 308])
    rows1 = din("rows1", [128, 308])
    cT = din("cT", [128, 16, 2])
    wada = din("wada", [D, 6144])
    bada = din("bada", [1, 6144])
    w_qkv = din("w_qkv", [D, 2560])
    w_o0 = din("w_o0", [D, D])
    wr0 = din("wr0", [D, 20])
    wr1 = din("wr1", [D, 20])
    w1 = din("w1", [2, 16, D, 512])
    w3 = din("w3", [2, 16, D, 512])
    w2 = din("w2", [2, 16, 512, D])
    w_in = din("w_in", [D, 3680])
    w_o1 = din("w_o1", [D, D])
    phi_k1 = din("phi_k1", [D, 256])
    phi_v1 = din("phi_v1", [D, 256])
    phi2 = din("phi2", [128, 2, 192])
    peT = din("peT", [128, 32])
    causal = din("causal", [8, 128, 4096], BF16)
    cvalid = din("cvalid", [8, 128, 256], BF16)
    sbias_d = din("sbias", [128, 8 * 64])
    ovl = din("ovl", [128, 2, 64])
    oneh_d = din("oneh", [128, 8])
    xspill = nc.dram_tensor("xspill", [128, NCH * NT], F32).ap()
    mod_src = nc.dram_tensor("mod_src", [2, 6144], F32).ap()
    mod_dst = nc.dram_tensor("mod_dst", [8, 6144], F32).ap()
    kv_src = [nc.dram_tensor("kv_src%d" % i, [128, 2048 if i < 8 else 1040], BF16).ap() for i in range(12)]
    kv_dst = [nc.dram_tensor("kv_dst%d" % i, [512, 2048 if i < 8 else 1040], BF16).ap() for i in range(12)]
    xout = nc.dram_tensor("xout", [NT, D], F32, kind="ExternalOutput").ap()
    with ExitStack() as st:
        k = K(nc, st)
        C = setup_ctx(k, {"ident_f": ident})
        setup_region(C)
        C.t_out = Tok()
        vec0 = C.vec
        vec1 = k.sb("vec1", [128, NV], F32)
        C.mk = k.sb("mk", [128, 4, 128], BF16)
        k.dma("pool", C.mk[:], masks, writes=[C.t_const])
        C.mdiag, C.mprev, C.mprev0, C.rperm = C.mk[:, 0, :], C.mk[:, 1, :], C.mk[:, 2, :], C.mk[:, 3, :]
        C.rowsb = k.sb("rowsb", [128, 308], F32)
        C.bvrow = C.rowsb[:, 0:256]
        C.esink = k.sb("esink", [128, 32], F32)
        C.brow = C.rowsb[:, 288:308]
        k.dma("sp", C.rowsb[:], rows0, writes=[C.t_wr])
        k.op("act", lambda e: e.activation(out=C.esink[:], in_=C.rowsb[:, 256:288], func=AF.Exp), reads=[C.t_wr], writes=[C.t_wr])
        k.dma("sp", C.wr[:], wr0.rearrange("(kc p) n -> p kc n", p=128), writes=[C.t_wr])
        k.dma("sp", vec0[:], vec0_d, writes=[C.t_vec])
        k.dma("sp", vec1[:], vec1_d, writes=[C.t_vec])
        C.zero1 = k.sb("zero1", [128, 1], F32)
        k.op("dve", lambda e: e.memset(C.zero1[:], 0.0), writes=[C.t_vec])
        oneh = k.sb("oneh", [128, 8], F32)
        phi2_sb = k.sb("phi2_sb", [128, 2, 192], BF16)
        peT_sb = k.sb("peT_sb", [128, 32], F32)
        ovl_sb = k.sb("ovl_sb", [128, 2, 64], BF16)
        t_nc = Tok()
        k.dma("sp", oneh[:], oneh_d, writes=[t_nc])
        k.dma("pool", phi2_sb[:], phi2, writes=[t_nc])
        k.dma("sp", peT_sb[:], peT, writes=[t_nc])
        k.dma("pool", ovl_sb[:], ovl, writes=[t_nc])
        kcT_all = vc_all = t_cmpkv = None
        c_sb = k.sb("c_sb", [128, 16, 2], F32)
        t_c = Tok()
        k.dma("sp", c_sb[:], cT, writes=[t_c])
        k.op("act", lambda e: e.activation(out=c_sb[:], in_=c_sb[:], func=AF.Silu), reads=[t_c], writes=[t_c])
        wst = [C.big[:].rearrange("p c t -> p (c t)").bitcast(F32).rearrange("p (kc n) -> p kc n", kc=16),
               C.hT[:].rearrange("p c t -> p (c t)").bitcast(F32)[:, 0:8192].rearrange("p (kc n) -> p kc n", kc=16)]
        t_wst = [Tok(), Tok()]
        brow_m = C.stage[0][0:1, :]
        bsb = C.xT[0:1, 8:14, :].rearrange("p c t -> p (c t)")
        t_bsb = Tok()
        k.dma("sp", bsb, bada, writes=[t_bsb])
        ones2 = k.sb("ones2", [1, 2], F32)
        k.op("dve", lambda e: e.memset(ones2[:], 1.0), writes=[t_c])
        mres = C.xT[0:2, 0:6, :].rearrange("p c t -> p (c t)")
        t_mres = Tok()
        wv = wada.rearrange("(kc p) n -> p kc n", p=128)
        for jb in range(12):
            i = jb % 2
            k.dma("sp", wst[i], wv[:, :, jb * 512:(jb + 1) * 512], writes=[t_wst[i]])
            pb, tp = next_pb(C)
            for kc in range(16):
                k.op("pe", lambda e: e.matmul(pb[0:2, :], lhsT=c_sb[:, kc, :], rhs=wst[i][:, kc, :], start=(kc == 0), stop=False),
                     reads=[t_c, t_wst[i]], writes=[tp], inc=False)
            k.op("pe", lambda e: e.matmul(pb[0:2, :], lhsT=ones2[:], rhs=bsb[:, jb * 512:(jb + 1) * 512], start=False, stop=True),
                 reads=[t_c, t_bsb], writes=[tp])
            k.op("dve", lambda e: e.tensor_copy(out=mres[:, jb * 512:(jb + 1) * 512], in_=pb[0:2, :]), reads=[tp], writes=[t_mres])
        t_msrc, t_mdst = Tok(), Tok()
        k.dma("sp", mod_src, mres, reads=[t_mres], writes=[t_msrc])
        emit_collective(k, nc, "AllGather", mod_src, mod_dst, [t_msrc], [t_mdst], "cc_mod")
        mt = [C.stage[0][:, 0:128], C.stage[0][0:64, 128:256]]
        for r_ in range(4):
            rowsrc = mod_dst[2 * r_:2 * r_ + 1, :].rearrange("o (j p) -> (o j) p", p=128)
            lo = 48 * r_
            for (a0, a1) in ((lo, min(lo + 48, 128)), (max(lo, 128), lo + 48)):
                if a1 <= a0:
                    continue
                if a0 < 128:
                    k.dma("sp", mt[0][a0:a1, :], rowsrc[a0 - lo:a1 - lo, :], reads=[t_mdst], writes=[C.t_stage[0]])
                else:
                    k.dma("sp", mt[1][a0 - 128:a1 - 128, :], rowsrc[a0 - lo:a1 - lo, :], reads=[t_mdst], writes=[C.t_stage[0]])
        pb, tp = next_pb(C)
        k.op("pe", lambda e: e.transpose(pb[:, 0:128], mt[0], C.ident_f[:]), reads=[C.t_stage[0], C.t_const], writes=[tp])
        k.op("pe", lambda e: e.transpose(pb[:, 128:192], mt[1], C.ident_f[0:64, 0:64]), reads=[C.t_stage[0], C.t_const], writes=[tp])
        k.op("dve", lambda e: e.tensor_copy(out=vec0[:, 0:96], in_=pb[:, 0:96]), reads=[tp, C.t_vec], writes=[C.t_vec])
        k.op("dve", lambda e: e.tensor_copy(out=vec1[:, 0:96], in_=pb[:, 96:192]), reads=[tp, C.t_vec], writes=[C.t_vec])
        for c_ in range(NCH):
            for t_ in C.t_x[c_]:
                t_.w = t_msrc.w
        last_pe = (k.sem["pe"], k.cnt["pe"])
        for c_ in range(NCH):
            for t_ in C.t_big[c_] + C.t_h[c_]:
                t_.w = last_pe
        for t_ in C.t_stage + C.t_h32 + C.t_sa + C.t_tmp + C.t_stat + [tt for l_ in C.t_cmb for tt in l_]:
            t_.w = (k.sem["dve"], k.cnt["dve"])
        derive_vec(C)
        C.vec = vec1
        derive_vec(C)
        C.vec = vec0
        tv = C.t_vec
        k.op("dve", lambda e: e.tensor_tensor(out=vec0[:, V_A_N:V_A_N + 16], in0=vec0[:, V_LN_C_G:V_LN_C_G + 16], in1=vec1[:, V_SC1_T:V_SC1_T + 16], op=ALU.mult), reads=[tv], writes=[tv])
        k.op("dve", lambda e: e.tensor_tensor(out=vec0[:, V_B_N:V_B_N + 16], in0=vec0[:, V_LN_C_B:V_LN_C_B + 16], in1=vec1[:, V_SC1_T:V_SC1_T + 16], op=ALU.mult), reads=[tv], writes=[tv])
        k.op("dve", lambda e: e.tensor_tensor(out=vec0[:, V_B_N:V_B_N + 16], in0=vec0[:, V_B_N:V_B_N + 16], in1=vec1[:, V_SH_T:V_SH_T + 16], op=ALU.add), reads=[tv], writes=[tv])
        load_x(C, xin, True)
        swa_layer(C, {"pos": pos, "w_qkv": w_qkv, "w_o": w_o0})
        layer_norm_mod(C, vec0, V_LN_T_G, V_LN_T_B, V_A_C, V_B_C, C.tmp, C.t_tmp, C.stat, C.t_stat)
        moe_layer(C, 0, vec0, {"sc1": V_SC1_C, "sh": V_SH_C, "gp": V_GP_C, "w1": w1[0], "w3": w3[0], "w2": w2[0]})
        layer_norm_mod(C, vec0, V_LN_C_G, V_LN_C_B, V_A_N, V_B_N, C.tmp, C.t_tmp, C.stat, C.t_stat)
        if DBG.get('fdump'):
            d1 = nc.dram_tensor('dbg_vec', [128, 2 * NV], F32, kind='ExternalOutput').ap()
            k.dma('sp', d1[:, 0:NV], vec0[:], reads=[C.t_vec], writes=[C.t_out])
            k.dma('sp', d1[:, NV:2 * NV], vec1[:], reads=[C.t_vec], writes=[C.t_out])
            d2 = nc.dram_tensor('dbg_x1', [128, NCH * NT], F32, kind='ExternalOutput').ap()
            k.dma('sp', d2, C.xT[:].rearrange('p c t -> p (c t)'), reads=[C.t_x[c_][th_] for c_ in range(NCH) for th_ in range(2)], writes=[C.t_out])
            d3 = nc.dram_tensor('dbg_h1', [128, NCH * (NT + HALO0)], BF16, kind='ExternalOutput').ap()
            k.dma('sp', d3, C.hT[:].rearrange('p c t -> p (c t)'), reads=[C.t_h[c_][th_] for c_ in range(NCH) for th_ in range(3)], writes=[C.t_out])
        C.vec = vec1
        v = vec1
        k.dma("sp", C.rowsb[:], rows1, writes=[C.t_wr])
        k.dma("sp", C.wr[:], wr1.rearrange("(kc p) n -> p kc n", p=128), writes=[C.t_wr])
        f32v, bfv = C.f32v, C.bfv
        pos1 = pos[:, HALO0:HALO0 + NT]
        wk = {"i": 0,
              "qb": [bfv(4736, 512), bfv(5760, 512)], "tq": [Tok(), Tok()],
              "t1": [f32v(6784, 512), f32v(8832, 512)], "tt1": [Tok(), Tok()],
              "t2": [f32v(10880, 512), f32v(12928, 512)], "tt2": [Tok(), Tok()]}
        TcosA = f32v(16384, NT)
        TsinA = f32v(16384 + 4096, NT)
        t_trigA = Tok()
        lnc_done = (k.sem["pool"], k.cnt["pool"])
        preA = [C.t_big[c__][th__] for c__ in range(6) for th__ in range(2)] + C.t_tmp + C.t_sa + C.t_stat + C.t_stage + C.t_h32 + [tt for l_ in C.t_cmb for tt in l_]
        rope_tables(C, pos1, NT, TcosA, TsinA, t_trigA, C.big[:, 0:2, :].rearrange("p a t -> p (a t)").bitcast(I32),
                    C.big[:, 2:4, :].rearrange("p a t -> p (a t)").bitcast(F32), C.big[:, 4:6, :].rearrange("p a t -> p (a t)").bitcast(F32), pre=preA)
        for c__ in range(6):
            for th__ in range(2):
                C.t_big[c__][th__].w = t_trigA.w
                C.t_big[c__][th__].r = {}
        stg = C.big[:, 0:16, :].rearrange("p (a g) t -> p a g t", g=4)
        t_stg = [[C.t_big[a * 4 + g][0] for g in range(4)] for a in range(4)]
        col0 = [2048, 2304, 2560, 3072]
        kblocks = []
        order = []
        for a in range(4):
            for b in range(2):
                srcs = []
                for m in range(2):
                    g = b * 2 + m
                    col = col0[a] + g * 64
                    srcs.append((w_in[:, col:col + 64], m * 128))
                    srcs.append((w_in[:, col:col + 64], m * 128 + 64))
                kblocks.append((srcs, 2))
                order.append((a, b))

        def evacA(bi, m, th, pb, tp):
            a, b = order[bi]
            g = b * 2 + m
            dst = stg[:, a, g, tsl(th)]
            toks = [C.t_big[a * 4 + g][th]]
            if a >= 2:
                cs = tsl(th)
                rope_evac(C, pb, tp, 512, C.zero1[:, 0:1], TcosA[:, cs], TsinA[:, cs], t_trigA, dst, toks[0], wk)
            else:
                k.op("act", lambda e: e.copy(out=dst, in_=pb[:]), reads=[tp], writes=toks)
        linear_fm(C, kblocks, lambda kc, th: C.hT[:, kc, HALO0 + th * 512:HALO0 + (th + 1) * 512],
                  lambda th: (lambda kc: [C.t_h[kc][1 + th]]), 2, evacA)
        t_kvsrc = [Tok() for _ in range(12)]
        t_kvdst = [Tok() for _ in range(12)]
        vaug = bfv(0, 2 * 2080).rearrange("p (a g t d) -> p a g t d", a=2, g=4, t=8)
        t_vaug = Tok()
        t_vaug.w = (k.sem["dve"], k.cnt["dve"])
        k.op("pool", lambda e: e.memset(bfv(0, 2 * 2080), 1.0), writes=[t_vaug])
        for vi, col in enumerate((2816, 3328)):
            vbuf, tvb = load_wblock(C, [(w_in[:, col:col + 256], 0)])
            for t in range(8):
                pb, tp = next_pb(C)
                for kc in range(NCH):
                    k.op("pe", lambda e: e.matmul(pb[:, 0:256], lhsT=C.hT[:, kc, HALO0 + t * 128:HALO0 + (t + 1) * 128], rhs=vbuf[:, kc, :], start=(kc == 0), stop=(kc == NCH - 1)),
                         reads=[tvb, C.t_h[kc][1], C.t_h[kc][2]], writes=[tp], inc=(kc == NCH - 1))
                k.op("act", lambda e: e.copy(out=vaug[:, vi, :, t, 0:64], in_=pb[:, 0:256].rearrange("p (g d) -> p g d", g=4)), reads=[tp], writes=[t_vaug])
        A_gates_w = load_wblock(C, [(w_in[:, 3584:3680], 0)])
        bigflat = C.big[:].rearrange("p c t -> p (c t)")
        for ci in range(8):
            k.dma("sp", kv_src[ci], bigflat[:, ci * 2048:(ci + 1) * 2048], reads=[C.t_big[c_][th_] for c_ in (2 * ci, 2 * ci + 1) for th_ in range(2)], writes=[t_kvsrc[ci]])
            emit_collective(k, nc, "AllGather", kv_src[ci], kv_dst[ci], [t_kvsrc[ci]], [t_kvdst[ci]], "cc_kv%d" % ci)
        for ci in range(8, 12):
            k.dma("sp", kv_src[ci], bfv(0, 2 * 2080)[:, (ci - 8) * 1040:(ci - 7) * 1040], reads=[t_vaug], writes=[t_kvsrc[ci]])
            emit_collective(k, nc, "AllGather", kv_src[ci], kv_dst[ci], [t_kvsrc[ci]], [t_kvdst[ci]], "cc_kv%d" % ci)
        gch = [d_.rearrange("(r p) n -> p r n", p=128) for d_ in kv_dst]

        def gblk(a, g):
            bl = a * 4 + g
            return gch[bl // 2][:, :, (bl % 2) * 1024:(bl % 2 + 1) * 1024], t_kvdst[bl // 2]

        def gv(vi, g):
            ci = 8 + vi * 2 + g // 2
            return gch[ci][:, :, (g % 2) * 520:(g % 2 + 1) * 520], t_kvdst[ci]
        A = Ctx()
        A.pos, A.w_in, A.w_o, A.w1, A.w3, A.w2 = pos1, w_in, w_o1, w1[1], w3[1], w2[1]
        A.phi_k1, A.phi_v1, A.causal, A.cvalid, A.sbias_d, A.xspill, A.xout = phi_k1, phi_v1, causal, cvalid, sbias_d, xspill, xout
        A.phi2_sb, A.peT_sb, A.ovl_sb, A.t_nc, A.kcT_all, A.vc_all, A.t_cmpkv = phi2_sb, peT_sb, ovl_sb, t_nc, kcT_all, vc_all, t_cmpkv
        A.cmp_prepped = False
        A.gates_w = A_gates_w
        A.pre_toks = [t_trigA, t_vaug] + wk["tq"] + wk["tt1"] + wk["tt2"]

        def load_cmp_src(g, kv, kvA, t_kvA, selc, t_selc):
            a = kv
            src_, tsrc_ = gblk(a, g)
            k.dma("sp", selc.rearrange("p (r t) -> p r t", r=4), src_, reads=[tsrc_], writes=[t_selc])
            pecol = 16 * kv
            for jj in range(16):
                for half in range(2):
                    ps_ = slice(half * 64, (half + 1) * 64)
                    o0 = 2 * jj + half
                    k.op("dve",
                         lambda e: e.tensor_scalar(out=kvA[ps_, jj * 255:(jj + 1) * 255], in0=selc[ps_, o0:o0 + 16 * 254 + 1:16],
                                                   scalar1=peT_sb[ps_, pecol + jj:pecol + jj + 1], scalar2=None, op0=ALU.add),
                         reads=[t_selc, t_nc], writes=[t_kvA])

        def load_kv(g, kvA, t_kvA, Vs, t_Vs, Kw, t_Kw, Vw, t_Vw):
            Vsf = Vs.rearrange("p t d -> p (t d)")
            src_, tsrc_ = gv(1, g)
            k.dma("sp", Vsf.rearrange("p (r n) -> p r n", r=4), src_, reads=[tsrc_], writes=[t_Vs])
            for i in range(4):
                for (dst, src, col) in ((Vw[:, 0:4, :], Vs[:, 8 * i + 4:8 * i + 8, :], i), (Vw[:, 4:12, :], Vs[:, 8 * i:8 * i + 8, :], 4 + i)):
                    if i == 0:
                        k.op("dve", lambda e: e.tensor_scalar(out=dst, in0=src, scalar1=oneh[:, col:col + 1], scalar2=None, op0=ALU.mult), reads=[t_Vs, t_nc], writes=[t_Vw])
                    else:
                        k.op("dve", lambda e: e.scalar_tensor_tensor(out=dst, in0=src, scalar=oneh[:, col:col + 1], in1=dst, op0=ALU.mult, op1=ALU.add),
                             reads=[t_Vs, t_nc, t_Vw], writes=[t_Vw])
            src_, tsrc_ = gblk(3, g)
            k.dma("sp", kvA.rearrange("p (r t) -> p r t", r=4), src_, reads=[tsrc_], writes=[t_kvA])
            for i in range(4):
                for (dst, src, col) in ((Kw[:, 0:512], kvA[:, i * 1024 + 512:(i + 1) * 1024], i), (Kw[:, 512:1536], kvA[:, i * 1024:(i + 1) * 1024], 4 + i)):
                    if i == 0:
                        k.op("dve", lambda e: e.tensor_scalar(out=dst, in0=src, scalar1=oneh[:, col:col + 1], scalar2=None, op0=ALU.mult), reads=[t_kvA, t_nc], writes=[t_Kw])
                    else:
                        k.op("dve", lambda e: e.scalar_tensor_tensor(out=dst, in0=src, scalar=oneh[:, col:col + 1], in1=dst, op0=ALU.mult, op1=ALU.add),
                             reads=[t_kvA, t_nc, t_Kw], writes=[t_Kw])
            src_, tsrc_ = gblk(2, g)
            k.dma("sp", kvA.rearrange("p (r t) -> p r t", r=4), src_, reads=[tsrc_], writes=[t_kvA])
            src_, tsrc_ = gv(0, g)
            k.dma("sp", Vsf.rearrange("p (r n) -> p r n", r=4), src_, reads=[tsrc_], writes=[t_Vs])
        A.load_cmp_src, A.load_kv = load_cmp_src, load_kv
        nsa_core(nc, k, C, A)
        k.finish([C.t_out])
        print("fused program: %d instructions" % k.ninst, k.cnt)
    return nc


def fused_inputs(inp, core):
    b, r = core // 4, core % 4
    m0 = layer0_inputs(inp, np.zeros((128, 96), np.float32), core)
    vec0 = np.zeros((128, NV), np.float32)
    vec0[:, 0:280] = m0["vec"][:, 0:280]
    dummy = {"kc_full": None, "vc_full": None, "ks_full": None, "kw_full": np.zeros((4, 128, 4096), BF),
             "vs_aug_full": np.zeros((4, 128, 32, 65), BF), "vw_aug_full": np.zeros((4, 128, 32, 65), BF)}
    m1 = layer1b_inputs(inp, np.zeros((128, 96), np.float32), np.zeros((1, 1), np.float32), core, dummy)
    vec1 = np.zeros((128, NV), np.float32)
    vec1[:, 0:280] = m1["vec"][:, 0:280]
    wcat = np.concatenate([inp["w_ada"][0], inp["w_ada"][1]], axis=1)
    bcat = np.concatenate([inp["b_ada"][0], inp["b_ada"][1]], axis=0)
    c = inp["c"][b].astype(np.float32)
    cT = np.ascontiguousarray(np.stack([c, c], axis=1).reshape(16, 128, 2).transpose(1, 0, 2))
    oneh = np.zeros((128, 8), np.float32)
    if r > 0:
        oneh[:, r - 1] = 1.0
    oneh[:, 4 + r] = 1.0
    return {"xin": m0["xin"], "pos": m0["pos"], "vec0": vec0, "vec1": vec1, "ident_f": m0["ident_f"], "masks": m0["masks"],
            "rows0": m0["rows"], "rows1": m1["rows"], "cT": cT,
            "wada": np.ascontiguousarray(wcat[:, r * 6144:(r + 1) * 6144]), "bada": np.ascontiguousarray(bcat[None, r * 6144:(r + 1) * 6144]),
            "w_qkv": inp["swa_w_qkv"][0], "w_o0": inp["swa_w_o"][0], "wr0": m0["wr"], "wr1": m1["wr"],
            "w1": inp["moe_w1"], "w3": inp["moe_w3"], "w2": inp["moe_w2"],
            "w_in": inp["nsa_w_in"][0], "w_o1": inp["nsa_w_o"][0], "phi_k1": inp["nsa_phi_k1"][0], "phi_v1": inp["nsa_phi_v1"][0],
            "phi2": m1["phi2"], "peT": m1["peT"], "causal": m1["causal"], "cvalid": m1["cvalid"], "sbias": m1["sbias"], "ovl": m1["ovl"],
            "oneh": oneh}


def kernel(**inp):
    inp = {kk: np.asarray(vv) for kk, vv in inp.items()}
    cores = list(range(8))
    in_maps = [fused_inputs(inp, cc) for cc in cores]
    res = run_bass_kernel_spmd(build_fused(), in_maps, core_ids=cores)
    x2 = np.stack([np.asarray(r["xout"]) for r in res.results])
    return np.ascontiguousarray(x2.reshape(2, 4096, 2048)).astype(np.float32)
```
